# Optimizing a Trainium2 kernel written in Bass

```python
import jax
import jax.numpy as jnp
from jax import lax
import numpy as np

D_MODEL = 1024
BATCH = 32
SEQ = 2048
DEPTH = 2

HEAD_DIM = 64
N_HEADS = 4
GROUP_W = N_HEADS * HEAD_DIM
N_GROUPS = 4
D_MIX = N_GROUPS * GROUP_W
ROPE_THETA = 10000.0
ATT_BLOCK = 128
NEG = -1e30
LN_EPS = 1e-5
A_KV_HEADS = 2
A_WINDOW = 128
B_CONV = 4
B_CHUNK = 128
B_FBIAS_LO = 3.0
B_FBIAS_HI = 6.0
C_CHUNK = 64
C_EPS = 1e-6
CMP_LEN = 32
CMP_STRIDE = 16
SLC_LEN = 64
N_SEL = 8
NSA_WINDOW = 512
SLC_QBLOCK = 32
FORCE_SCORE = 1e6
DN_ALPHA = (2.0 * DEPTH) ** 0.25
DN_BETA = (8.0 * DEPTH) ** -0.25

SPLITS = (
    ('a_q', GROUP_W), ('a_k', A_KV_HEADS * HEAD_DIM), ('a_v', A_KV_HEADS * HEAD_DIM), ('a_z', GROUP_W),
    ('b_qk', 2 * GROUP_W), ('b_v', GROUP_W), ('b_if', 2 * N_HEADS), ('b_o', GROUP_W), ('b_z', GROUP_W),
    ('c_q', GROUP_W), ('c_f', GROUP_W), ('c_i', GROUP_W), ('c_z', GROUP_W),
    ('d_q', GROUP_W), ('d_kv', 6 * HEAD_DIM), ('d_g', 3 * N_HEADS), ('d_z', GROUP_W),
)
N_COLS = sum(size for _, size in SPLITS)

kernel_name = 'hybrid_swa_mlstm_hgrn2_nsa_deepnorm'


def _col_offset(name):
    off = 0
    for n, size in SPLITS:
        if n == name:
            return off
        off += size
    raise KeyError(name)


def split_cols(h):
    cols = {}
    off = 0
    for name, size in SPLITS:
        cols[name] = h[..., off:off + size]
        off += size
    return cols


def layer_norm(x, g, b):
    xf = x.astype(jnp.float32)
    mu = jnp.mean(xf, axis=-1, keepdims=True)
    var = jnp.mean(jnp.square(xf - mu), axis=-1, keepdims=True)
    y = (xf - mu) * lax.rsqrt(var + LN_EPS) * g.astype(jnp.float32) + b.astype(jnp.float32)
    return y.astype(x.dtype)


def rope(x, pos):
    half = x.shape[-1] // 2
    inv = ROPE_THETA ** (-jnp.arange(half, dtype=jnp.float32) / half)
    ang = pos.astype(jnp.float32)[:, None] * inv[None, :]
    cos = jnp.cos(ang)[None, :, None, :]
    sin = jnp.sin(ang)[None, :, None, :]
    xf = x.astype(jnp.float32)
    x1, x2 = xf[..., :half], xf[..., half:]
    return jnp.concatenate([x1 * cos - x2 * sin, x1 * sin + x2 * cos], axis=-1).astype(x.dtype)


def causal_conv(x, w, b):
    K, C = w.shape
    y = lax.conv_general_dilated(x, w[:, None, :].astype(x.dtype), window_strides=(1,), padding=[(K - 1, 0)],
                                 dimension_numbers=('NWC', 'WIO', 'NWC'), feature_group_count=C)
    return y + b.astype(x.dtype)


def banded_attention(q, k, v, window, sinks=None):
    B, T, H, d = q.shape
    G = k.shape[2]
    R = H // G
    nb = T // ATT_BLOCK
    pad = window
    span = pad + ATT_BLOCK
    kp = jnp.pad(k, ((0, 0), (pad, 0), (0, 0), (0, 0)))
    vp = jnp.pad(v, ((0, 0), (pad, 0), (0, 0), (0, 0)))
    qb = q.reshape(B, nb, ATT_BLOCK, G, R, d).transpose(1, 0, 2, 3, 4, 5)
    qi = jnp.arange(ATT_BLOCK)[:, None]
    kj = jnp.arange(span)[None, :] - pad
    rel = qi - kj
    band = (rel >= 0) & (rel < window)
    scale = d ** -0.5

    def block(args):
        n, qblk = args
        start = n * ATT_BLOCK
        kb = lax.dynamic_slice_in_dim(kp, start, span, axis=1)
        vb = lax.dynamic_slice_in_dim(vp, start, span, axis=1)
        s = jnp.einsum('bqgrd,bkgd->bgrqk', qblk, kb).astype(jnp.float32) * scale
        s = jnp.where(band & (kj + start >= 0), s, NEG)
        if sinks is None:
            p = jax.nn.softmax(s, axis=-1)
        else:
            sink = jnp.broadcast_to(sinks.astype(jnp.float32).reshape(1, G, R, 1, 1), s.shape[:-1] + (1,))
            p = jax.nn.softmax(jnp.concatenate([s, sink], axis=-1), axis=-1)[..., :span]
        return jnp.einsum('bgrqk,bkgd->bqgrd', p.astype(v.dtype), vb)

    out = lax.map(block, (jnp.arange(nb), qb))
    return out.transpose(1, 0, 2, 3, 4, 5).reshape(B, T, H, d)


def mlstm_chunkwise(q, k, v, i_pre, f_pre):
    B, T, H, d = q.shape
    L = B_CHUNK
    nc = T // L
    f32 = jnp.float32
    to_chunks = lambda t: t.astype(f32).reshape(B, nc, L, H, -1).transpose(1, 0, 3, 2, 4)
    qc = to_chunks(q) * d ** -0.5
    kc = to_chunks(k)
    vc = to_chunks(v)
    ic = to_chunks(i_pre[..., None])[..., 0]
    fc = jax.nn.log_sigmoid(to_chunks(f_pre[..., None])[..., 0])
    causal = jnp.tril(jnp.ones((L, L), bool))

    def step(carry, inp):
        C, n, m = carry
        q_, k_, v_, i_, lf = inp
        b = jnp.cumsum(lf, axis=-1)
        D = jnp.where(causal, b[..., :, None] - b[..., None, :] + i_[..., None, :], -jnp.inf)
        inter = b + m[..., None]
        m_t = jnp.maximum(jnp.max(D, axis=-1), inter)
        w_inter = jnp.exp(inter - m_t)
        S = jnp.einsum('bhtd,bhsd->bhts', q_, k_) * jnp.exp(D - m_t[..., None])
        num = jnp.einsum('bhts,bhsv->bhtv', S, v_) + w_inter[..., None] * jnp.einsum('bhtk,bhvk->bhtv', q_, C)
        den = jnp.sum(S, axis=-1) + w_inter * jnp.einsum('bhtk,bhk->bht', q_, n)
        h = num / jnp.maximum(jnp.abs(den), jnp.exp(-m_t))[..., None]
        b_last = b[..., -1]
        d_s = b_last[..., None] - b + i_
        m_new = jnp.maximum(b_last + m, jnp.max(d_s, axis=-1))
        w_s = jnp.exp(d_s - m_new[..., None])
        decay = jnp.exp(b_last + m - m_new)
        C_new = decay[..., None, None] * C + jnp.einsum('bhs,bhsv,bhsk->bhvk', w_s, v_, k_)
        n_new = decay[..., None] * n + jnp.einsum('bhs,bhsk->bhk', w_s, k_)
        return (C_new, n_new, m_new), h

    init = (jnp.zeros((B, H, d, d), f32), jnp.zeros((B, H, d), f32), jnp.zeros((B, H), f32))
    _, hs = lax.scan(step, init, (qc, kc, vc, ic, fc))
    return hs.transpose(1, 0, 3, 2, 4).reshape(B, T, H, d).astype(q.dtype)


def hgrn2_chunkwise(q, log_f, k, v):
    B, T, H, dk = q.shape
    dv = v.shape[-1]
    L = C_CHUNK
    nc = T // L
    f32 = jnp.float32
    to_chunks = lambda t: t.astype(f32).reshape(B, nc, L, H, -1).transpose(1, 0, 3, 2, 4)
    causal = jnp.tril(jnp.ones((L, L), bool))[..., None]

    def step(S, inp):
        q_, g_, k_, v_ = inp
        a = jnp.cumsum(g_, axis=2)
        decay = jnp.exp(jnp.where(causal, a[:, :, :, None, :] - a[:, :, None, :, :], -jnp.inf))
        A = jnp.einsum('bhtd,bhtsd,bhsd->bhts', q_, decay, k_)
        o = jnp.einsum('bhts,bhsv->bhtv', A, v_) + jnp.einsum('bhtk,bhkv->bhtv', q_ * jnp.exp(a), S)
        a_last = a[:, :, -1:, :]
        S_new = jnp.exp(a_last[:, :, 0, :])[..., None] * S + jnp.einsum('bhsk,bhsv->bhkv', k_ * jnp.exp(a_last - a), v_)
        return S_new, o

    _, os_ = lax.scan(step, jnp.zeros((B, H, dk, dv), f32), (to_chunks(q), to_chunks(log_f), to_chunks(k), to_chunks(v)))
    return os_.transpose(1, 0, 3, 2, 4).reshape(B, T, H, dv)


def nsa_attention(q, k_cmp_in, v_cmp_in, k_slc, v_slc, k_win, v_win, gates, cmp_pe, cmp_w1, cmp_w2):
    B, T, H, d = q.shape
    pos = jnp.arange(T)
    scale = d ** -0.5
    n_cmp = (T - CMP_LEN) // CMP_STRIDE + 1
    starts = jnp.arange(n_cmp) * CMP_STRIDE
    ends = starts + CMP_LEN - 1
    idx = starts[:, None] + jnp.arange(CMP_LEN)[None, :]

    def compress(t, pe, w1, w2):
        blk = (t[:, idx] + pe).reshape(B, n_cmp, CMP_LEN * d)
        return jax.nn.silu(blk @ w1) @ w2

    k_cmp = rope(compress(k_cmp_in, cmp_pe[0], cmp_w1[0], cmp_w2[0])[:, :, None, :], ends)[:, :, 0, :]
    v_cmp = compress(v_cmp_in, cmp_pe[1], cmp_w1[1], cmp_w2[1])
    cmp_valid = ends[None, :] <= pos[:, None]
    s = jnp.einsum('bthd,bnd->bhtn', q, k_cmp).astype(jnp.float32) * scale
    p_cmp = jnp.where(cmp_valid, jax.nn.softmax(jnp.where(cmp_valid, s, NEG), axis=-1), 0.0)
    o_cmp = jnp.einsum('bhtn,bnd->bthd', p_cmp.astype(v_cmp.dtype), v_cmp)
    n_slc = T // SLC_LEN
    blk_id = jnp.arange(n_slc)
    overlap = ((starts[:, None] < (blk_id[None, :] + 1) * SLC_LEN)
               & (starts[:, None] + CMP_LEN > blk_id[None, :] * SLC_LEN)).astype(jnp.float32)
    imp = jnp.einsum('bhtn,nj->btj', p_cmp, overlap)
    cur = (pos // SLC_LEN)[:, None]
    forced = (blk_id[None, :] == 0) | (blk_id[None, :] == cur) | (blk_id[None, :] == cur - 1)
    imp = jnp.where(forced, FORCE_SCORE, imp)
    imp = jnp.where(blk_id[None, :] <= cur, imp, NEG)
    k_sel = min(N_SEL, n_slc)
    _, sel = lax.top_k(imp, k_sel)
    k_blocks = k_slc.reshape(B, n_slc, SLC_LEN, d)
    v_blocks = v_slc.reshape(B, n_slc, SLC_LEN, d)
    nq = T // SLC_QBLOCK
    q_ch = q.reshape(B, nq, SLC_QBLOCK, H, d).transpose(1, 0, 2, 3, 4)
    sel_ch = sel.reshape(B, nq, SLC_QBLOCK, k_sel).transpose(1, 0, 2, 3)
    offs = jnp.arange(SLC_LEN)

    def sel_block(args):
        t0, qc, ic = args
        kg = jax.vmap(lambda kb_, i_: kb_[i_])(k_blocks, ic).reshape(B, SLC_QBLOCK, k_sel * SLC_LEN, d)
        vg = jax.vmap(lambda vb_, i_: vb_[i_])(v_blocks, ic).reshape(B, SLC_QBLOCK, k_sel * SLC_LEN, d)
        tok = (ic[..., None] * SLC_LEN + offs).reshape(B, SLC_QBLOCK, k_sel * SLC_LEN)
        valid = tok <= (t0 + jnp.arange(SLC_QBLOCK))[None, :, None]
        s_ = jnp.einsum('bqhd,bqkd->bhqk', qc, kg).astype(jnp.float32) * scale
        p_ = jax.nn.softmax(jnp.where(valid[:, None], s_, NEG), axis=-1)
        return jnp.einsum('bhqk,bqkd->bqhd', p_.astype(vg.dtype), vg)

    o_slc = lax.map(sel_block, (jnp.arange(nq) * SLC_QBLOCK, q_ch, sel_ch))
    o_slc = o_slc.transpose(1, 0, 2, 3, 4).reshape(B, T, H, d)
    o_win = banded_attention(q, k_win[:, :, None, :], v_win[:, :, None, :], NSA_WINDOW)
    g = jax.nn.sigmoid(gates.astype(jnp.float32)).astype(q.dtype)
    return g[..., 0:1] * o_cmp + g[..., 1:2] * o_slc + g[..., 2:3] * o_win


def hybrid_mixer(x, w_in, b_in, a_sinks, b_conv_w, b_conv_b, lb, c_norm_g, d_cmp_pe, d_cmp_w1, d_cmp_w2, w_out):
    B, T, _ = x.shape
    pos = jnp.arange(T)
    f32 = jnp.float32
    silu = jax.nn.silu
    cols = split_cols(jnp.einsum('btd,dn->btn', x, w_in) + b_in)
    heads = lambda t: t.reshape(B, T, -1, HEAD_DIM)
    flat = lambda t: t.reshape(B, T, GROUP_W)
    q_a = rope(heads(cols['a_q']), pos)
    k_a = rope(heads(cols['a_k']), pos)
    y_a = flat(banded_attention(q_a, k_a, heads(cols['a_v']), A_WINDOW, a_sinks)) * silu(cols['a_z'])
    qk_b = silu(causal_conv(cols['b_qk'], b_conv_w, b_conv_b))
    h_b = mlstm_chunkwise(heads(qk_b[..., :GROUP_W]), heads(qk_b[..., GROUP_W:]), heads(cols['b_v']),
                          cols['b_if'][..., :N_HEADS], cols['b_if'][..., N_HEADS:])
    y_b = jax.nn.sigmoid(cols['b_o']) * flat(h_b) * silu(cols['b_z'])
    f_c = lb + (1.0 - lb) * jax.nn.sigmoid(cols['c_f'].astype(f32))
    o_c = hgrn2_chunkwise(heads(silu(cols['c_q'])), heads(jnp.log(f_c)), heads(1.0 - f_c), heads(cols['c_i']))
    o_c = o_c * lax.rsqrt(jnp.mean(jnp.square(o_c), axis=-1, keepdims=True) + C_EPS) * c_norm_g.astype(f32).reshape(N_HEADS, HEAD_DIM)
    y_c = flat(o_c).astype(x.dtype) * silu(cols['c_z'])
    q_d = rope(heads(cols['d_q']), pos)
    kv = cols['d_kv'].reshape(B, T, 6, HEAD_DIM)
    k_s = rope(kv[:, :, 2:3], pos)[:, :, 0]
    k_w = rope(kv[:, :, 4:5], pos)[:, :, 0]
    o_d = nsa_attention(q_d, kv[:, :, 0], kv[:, :, 1], k_s, kv[:, :, 3], k_w, kv[:, :, 5],
                        cols['d_g'].reshape(B, T, N_HEADS, 3), d_cmp_pe, d_cmp_w1, d_cmp_w2)
    y_d = flat(o_d) * silu(cols['d_z'])
    y = jnp.concatenate([y_a, y_b, y_c, y_d], axis=-1)
    return jnp.einsum('btm,md->btd', y, w_out)


def setup_inputs(seed: int = 0) -> dict:
    key = jax.random.key(seed)
    ks = jax.random.split(key, 17)
    f32 = jnp.float32
    nrm = lambda k, shape, s: s * jax.random.normal(k, shape, f32)
    x = jax.random.normal(ks[0], (BATCH, SEQ, D_MODEL), f32)
    ln0_g = 1.0 + nrm(ks[1], (D_MODEL,), 0.02)
    ln0_b = nrm(ks[2], (D_MODEL,), 0.02)
    w_in = nrm(ks[3], (DEPTH, D_MODEL, N_COLS), D_MODEL ** -0.5)
    off_f = _col_offset('b_if') + N_HEADS
    b_in = nrm(ks[4], (DEPTH, N_COLS), 0.02)
    b_in = b_in.at[:, off_f:off_f + N_HEADS].add(jnp.linspace(B_FBIAS_LO, B_FBIAS_HI, N_HEADS, dtype=f32))
    a_sinks = nrm(ks[5], (DEPTH, N_HEADS), 0.5)
    b_conv_w = nrm(ks[6], (DEPTH, B_CONV, 2 * GROUP_W), B_CONV ** -0.5)
    b_conv_b = nrm(ks[7], (DEPTH, 2 * GROUP_W), 0.02)
    c_lb = nrm(ks[8], (DEPTH, GROUP_W), 0.5)
    c_norm_g = 1.0 + nrm(ks[9], (DEPTH, GROUP_W), 0.02)
    d_cmp_pe = nrm(ks[10], (DEPTH, 2, CMP_LEN, HEAD_DIM), 0.02)
    d_cmp_w1 = nrm(ks[11], (DEPTH, 2, CMP_LEN * HEAD_DIM, HEAD_DIM), (CMP_LEN * HEAD_DIM) ** -0.5)
    d_cmp_w2 = nrm(ks[12], (DEPTH, 2, HEAD_DIM, HEAD_DIM), HEAD_DIM ** -0.5)
    w_out = nrm(ks[13], (DEPTH, D_MIX, D_MODEL), DN_BETA * D_MIX ** -0.5)
    ln_g = 1.0 + nrm(ks[14], (DEPTH, D_MODEL), 0.02)
    ln_b = nrm(ks[15], (DEPTH, D_MODEL), 0.02)
    return {'x': x, 'ln0_g': ln0_g, 'ln0_b': ln0_b, 'w_in': w_in, 'b_in': b_in, 'a_sinks': a_sinks,
            'b_conv_w': b_conv_w, 'b_conv_b': b_conv_b, 'c_lb': c_lb, 'c_norm_g': c_norm_g,
            'd_cmp_pe': d_cmp_pe, 'd_cmp_w1': d_cmp_w1, 'd_cmp_w2': d_cmp_w2, 'w_out': w_out,
            'ln_g': ln_g, 'ln_b': ln_b}


def reference(x, ln0_g, ln0_b, w_in, b_in, a_sinks, b_conv_w, b_conv_b, c_lb, c_norm_g, d_cmp_pe, d_cmp_w1, d_cmp_w2, w_out, ln_g, ln_b):
    lb = jnp.cumsum(jax.nn.softmax(c_lb.astype(jnp.float32), axis=0), axis=0)
    lb = lb - lb[0]
    h = layer_norm(x, ln0_g, ln0_b)
    for l in range(DEPTH):
        y = hybrid_mixer(h, w_in[l], b_in[l], a_sinks[l], b_conv_w[l], b_conv_b[l], lb[l], c_norm_g[l],
                         d_cmp_pe[l], d_cmp_w1[l], d_cmp_w2[l], w_out[l])
        h = layer_norm(DN_ALPHA * h + y, ln_g[l], ln_b[l])
    return h
```

```python
import contextlib
import numpy as np
import concourse.bass as bass
import concourse.mybir as mybir

F32 = mybir.dt.float32
BF16 = mybir.dt.bfloat16
AF = mybir.ActivationFunctionType
ALU = mybir.AluOpType
AX = mybir.AxisListType


class Buf:
    def __init__(self, name, t, slots=1):
        self.name = name
        self.t = t
        self.slots = slots

    def keys(self, slot=None):
        if slot is None:
            return [(self.name, s) for s in range(self.slots)]
        if isinstance(slot, (list, tuple, range)):
            return [(self.name, s) for s in slot]
        return [(self.name, slot)]

    def __getitem__(self, idx):
        return self.t[idx]


class Prog:
    NDMA = 4

    def __init__(self, nc, nepoch=1):
        self.nc = nc
        self.es = contextlib.ExitStack()
        self.eng = {"pe": nc.tensor, "act": nc.scalar, "dve": nc.vector, "pool": nc.gpsimd, "sp": nc.sync}
        self.esem = {}
        self.ecnt = {}
        self.seen = {}
        self.last_tok = {}
        self.finals = []
        self.esem_sets = []
        self.dsem_sets = []
        for ep in range(nepoch):
            self.esem_sets.append({k: self.es.enter_context(nc.semaphore("sem_%s_%d" % (k, ep))) for k in ("pe", "act", "dve", "pool")})
            self.dsem_sets.append([self.es.enter_context(nc.semaphore("dsem_sp%d_%d" % (i, ep))) for i in range(self.NDMA)])
        for k in self.eng:
            self.ecnt[k] = 0
            self.seen[k] = {}
            self.last_tok[k] = None
        self.esem = dict(self.esem_sets[0])
        self.dsem = {"sp": self.dsem_sets[0]}
        self.dval = {"sp": [0] * self.NDMA}
        self.dnext = {"sp": 0}
        self.dsem["pool"] = [self.es.enter_context(nc.semaphore("dsem_pool%d" % i)) for i in range(self.NDMA)]
        self.dval["pool"] = [0] * self.NDMA
        self.dnext["pool"] = 0
        self.cur_epoch = 0
        self.state = {}
        self.nbuf = 0
        self.excl = set()
        self.stop_ops = None
        self.ninstr = 0

    def epoch(self, ep):
        if ep == self.cur_epoch:
            return
        for k in ("pe", "act", "dve", "pool"):
            if self.ecnt[k] > 0:
                self.finals.append((self.esem[k], self.ecnt[k], k))
            self.esem[k] = self.esem_sets[ep][k]
            self.ecnt[k] = 0
        for j in range(self.NDMA):
            if self.dval["sp"][j] > 0:
                self.finals.append((self.dsem["sp"][j], self.dval["sp"][j], "dma"))
        self.dsem["sp"] = self.dsem_sets[ep]
        self.dval["sp"] = [0] * self.NDMA
        self.dnext["sp"] = 0
        self.cur_epoch = ep

    def sbuf(self, name, shape, dtype=F32, slots=1):
        t = self.es.enter_context(self.nc.sbuf_tensor(name, list(shape), dtype))
        return Buf(name, t, slots)

    def psum(self, name, shape, dtype=F32, slots=1):
        t = self.es.enter_context(self.nc.psum_tensor(name, list(shape), dtype))
        self.excl.add(name)
        return Buf(name, t, slots)

    def dram(self, name, shape, dtype=F32, kind="Internal", slots=1):
        t = self.nc.dram_tensor(name, list(shape), dtype, kind=kind)
        return Buf(name, t.ap(), slots)

    def _keys(self, lst):
        out = []
        for x in lst:
            if isinstance(x, Buf):
                out += x.keys()
            elif isinstance(x, tuple) and isinstance(x[0], Buf):
                out += x[0].keys(x[1])
            else:
                raise TypeError(x)
        return out

    def _wait(self, e, tok):
        sem, val, owner = tok
        sid = id(sem)
        if self.seen[e].get(sid, 0) >= val:
            return
        self.eng[e].wait_ge(sem, val)
        self.seen[e][sid] = val

    def _deps(self, e, R, W):
        toks = []
        for k in R:
            st = self.state.get(k)
            if st and st[0] is not None:
                toks.append(st[0])
        for k in W:
            st = self.state.get(k)
            if st:
                if st[0] is not None and (st[0][2] != e or e != 'pe'):
                    toks.append(st[0])
                for re_, tok in st[1].items():
                    if re_ != e or e != 'pe':
                        toks.append(tok)
        for tok in toks:
            self._wait(e, tok)

    def _commit(self, e, tok, R, W):
        for k in R:
            st = self.state.setdefault(k, [None, {}])
            st[1][e] = tok
        for k in W:
            self.state[k] = [tok, {}]

    def op(self, e, fn, R=(), W=(), fence=False):
        Rk, Wk = self._keys(R), self._keys(W)
        Wk = Wk + [k_ for k_ in Rk if k_[0] in self.excl]
        Rk = [k_ for k_ in Rk if k_[0] not in self.excl]
        self._deps(e, Rk, Wk)
        if fence and self.last_tok[e] is not None:
            self._wait(e, self.last_tok[e])
        if self.stop_ops is not None and self.ninstr >= self.stop_ops:
            raise StopIteration("stop_ops")
        ins = fn(self.eng[e])
        self.ecnt[e] += 1
        ins.then_inc(self.esem[e], 1)
        tok = (self.esem[e], self.ecnt[e], e)
        self.last_tok[e] = tok
        self._commit(e, tok, Rk, Wk)
        self.ninstr += 1
        return tok

    def dma(self, q, out, in_, R=(), W=(), **kw):
        Rk, Wk = self._keys(R), self._keys(W)
        self._deps(q, Rk, Wk)
        j = self.dnext[q]
        self.dnext[q] = (j + 1) % self.NDMA
        sem = self.dsem[q][j]
        if self.dval[q][j] > 0:
            self._wait(q, (sem, self.dval[q][j], "dma"))
        ins = self.eng[q].dma_start(out=out, in_=in_, **kw)
        self.dval[q][j] += 16
        ins.then_inc(sem, 16)
        tok = (sem, self.dval[q][j], "dma_" + q)
        self._commit("dma_" + q, tok, Rk, Wk)
        self.ninstr += 1
        return tok

    def wait_tok(self, e, tok):
        self._wait(e, tok)

    def finish(self, toks):
        for tok in toks:
            self._wait("sp", tok)
        for tok in self.finals:
            self._wait("sp", tok)
        for e in ("pe", "act", "dve", "pool"):
            if self.ecnt[e] > 0:
                self._wait("sp", (self.esem[e], self.ecnt[e], e))
        for q in ("sp", "pool"):
            for j in range(self.NDMA):
                if self.dval[q][j] > 0:
                    self._wait("sp", (self.dsem[q][j], self.dval[q][j], "dma"))


import numpy as np
import concourse.bass as bass
import concourse.mybir as mybir

D = 1024
HD = 64
LN_EPS = 1e-5
DN_ALPHA = (2.0 * 2) ** 0.25

OFF = {}
_o = 0
for _n, _s in (('a_q', 256), ('a_k', 128), ('a_v', 128), ('a_z', 256), ('b_qk', 512), ('b_v', 256), ('b_if', 8),
               ('b_o', 256), ('b_z', 256), ('c_q', 256), ('c_f', 256), ('c_i', 256), ('c_z', 256), ('d_q', 256),
               ('d_kv', 384), ('d_g', 12), ('d_z', 256)):
    OFF[_n] = _o
    _o += _s
NCOLS = _o


def _head(base, h):
    return np.arange(base + h * 64, base + (h + 1) * 64)


def _swap(c):
    return np.concatenate([c[32:], c[:32]])


def col_layout():
    L = {}
    idx = []

    def add(name, cols):
        L[name] = (sum(len(x) for x in idx), len(cols))
        idx.append(np.asarray(cols))

    aq, ak = OFF['a_q'], OFF['a_k']
    c = np.concatenate([_head(aq, 0), _head(aq, 2)]); add('AQ0', c); add('AQ0s', np.concatenate([_swap(_head(aq, 0)), _swap(_head(aq, 2))]))
    add('AQ1', np.concatenate([_head(aq, 1), _head(aq, 3)])); add('AQ1s', np.concatenate([_swap(_head(aq, 1)), _swap(_head(aq, 3))]))
    add('AK', np.concatenate([_head(ak, 0), _head(ak, 1)])); add('AKs', np.concatenate([_swap(_head(ak, 0)), _swap(_head(ak, 1))]))
    add('ATM', np.concatenate([np.arange(OFF['a_v'], OFF['a_v'] + 128), np.arange(OFF['a_z'], OFF['a_z'] + 256)]))
    bq = OFF['b_qk']
    add('BQ0', np.arange(bq, bq + 128)); add('BQ1', np.arange(bq + 128, bq + 256))
    add('BK0', np.arange(bq + 256, bq + 384)); add('BK1', np.arange(bq + 384, bq + 512))
    add('BI', np.arange(OFF['b_if'], OFF['b_if'] + 4)); add('BF', np.arange(OFF['b_if'] + 4, OFF['b_if'] + 8))
    add('BTM', np.concatenate([np.arange(OFF['b_v'], OFF['b_v'] + 256), np.arange(OFF['b_o'], OFF['b_o'] + 256),
                               np.arange(OFF['b_z'], OFF['b_z'] + 256)]))
    add('CQ0', np.arange(OFF['c_q'], OFF['c_q'] + 128)); add('CQ1', np.arange(OFF['c_q'] + 128, OFF['c_q'] + 256))
    add('CF0', np.arange(OFF['c_f'], OFF['c_f'] + 128)); add('CF1', np.arange(OFF['c_f'] + 128, OFF['c_f'] + 256))
    add('CTM', np.concatenate([np.arange(OFF['c_i'], OFF['c_i'] + 256), np.arange(OFF['c_z'], OFF['c_z'] + 256)]))
    dq, dk = OFF['d_q'], OFF['d_kv']
    add('DQ0', np.concatenate([_head(dq, 0), _head(dq, 1)])); add('DQ0s', np.concatenate([_swap(_head(dq, 0)), _swap(_head(dq, 1))]))
    add('DQ1', np.concatenate([_head(dq, 2), _head(dq, 3)])); add('DQ1s', np.concatenate([_swap(_head(dq, 2)), _swap(_head(dq, 3))]))
    add('DKCV', np.concatenate([_head(dk, 0), _head(dk, 1)]))
    add('DKS', np.concatenate([_head(dk, 2), _head(dk, 2)])); add('DKSs', np.concatenate([_swap(_head(dk, 2)), _swap(_head(dk, 2))]))
    add('DKW', np.concatenate([_head(dk, 4), _head(dk, 4)])); add('DKWs', np.concatenate([_swap(_head(dk, 4)), _swap(_head(dk, 4))]))
    add('DTM', np.concatenate([_head(dk, 3), _head(dk, 5), np.arange(OFF['d_g'], OFF['d_g'] + 12),
                               np.arange(OFF['d_z'], OFF['d_z'] + 256)]))
    return L, np.concatenate(idx)


LAY, GIDX = col_layout()
NCR = len(GIDX)
GROUPS = {
    'A': ('AQ0', 'ATM'),
    'B': ('BQ0', 'BTM'),
    'C': ('CQ0', 'CTM'),
    'D': ('DQ0', 'DTM'),
}


def grange(g):
    a, b = GROUPS[g]
    s = LAY[a][0]
    e = LAY[b][0] + LAY[b][1]
    return s, e


FM = ['AQ0', 'AQ0s', 'AQ1', 'AQ1s', 'AK', 'AKs', 'BQ0', 'BQ1', 'BK0', 'BK1', 'CQ0', 'CQ1', 'CF0', 'CF1',
      'DQ0', 'DQ0s', 'DQ1', 'DQ1s', 'DKCV', 'DKS', 'DKSs', 'DKW', 'DKWs']
FMI = {n: i for i, n in enumerate(FM)}
TM = ['ATM', 'BTM', 'CTM', 'DTM']
TMO = {}
_o = 0
for _n in TM:
    TMO[_n] = _o
    _o += LAY[_n][1]
NTM = _o
GL = ['A', 'B', 'C', 'D']
WMAX = max(grange(g)[1] - grange(g)[0] for g in GL)


def host_weights(w_in, b_in):
    w_r = np.ascontiguousarray(w_in[:, :, GIDX])
    b_r = b_in[:, GIDX]
    NL = w_in.shape[0]
    b_fm = np.zeros((NL, 128, len(FM)), np.float32)
    for i, n in enumerate(FM):
        s0, c = LAY[n]
        b_fm[:, :, i] = b_r[:, s0:s0 + 128]
    b_tm = np.concatenate([b_r[:, LAY[n][0]:LAY[n][0] + LAY[n][1]] for n in TM], axis=1)
    b_if = np.stack([b_r[:, LAY['BI'][0]:LAY['BI'][0] + 4], b_r[:, LAY['BF'][0]:LAY['BF'][0] + 4]], axis=-1)
    return w_r, np.ascontiguousarray(b_fm), np.ascontiguousarray(b_tm), np.ascontiguousarray(b_if)


def host_consts(T):
    half = 32
    inv = (10000.0 ** (-np.arange(half, dtype=np.float32) / half)).astype(np.float32)
    pos = np.arange(T, dtype=np.float32)
    ang = pos[None, :] * inv[:, None]
    cos = np.cos(ang).astype(np.float32)
    sin = np.sin(ang).astype(np.float32)
    cos64 = np.concatenate([cos, cos], 0)
    sin64 = np.concatenate([-sin, sin], 0)
    cosT = np.concatenate([cos64, cos64], 0)
    sinT = np.concatenate([sin64, sin64], 0)
    ident = np.eye(128, dtype=np.float32)
    p = np.arange(128)[:, None]
    f = np.arange(128)[None, :]
    tri_ge = (f >= p).astype(np.float32)
    tri_lt = (f < p).astype(np.float32)
    tri_ge = np.tile(tri_ge, (1, 4)); tri_lt = np.tile(tri_lt, (1, 4))
    NCMP = (T - 32) // 16 + 1
    cosE = np.zeros((128, 128), np.float32); sinE = np.zeros((128, 128), np.float32)
    cosE[:, :NCMP] = cosT[:, 31::16][:, :NCMP]; sinE[:, :NCMP] = sinT[:, 31::16][:, :NCMP]
    return dict(cosT=cosT, sinT=sinT, ident=ident, tri_ge=tri_ge, tri_lt=tri_lt, cosE=cosE, sinE=sinE)


def extra_inputs(full, T):
    NL = full['w_in'].shape[0]
    cw = full['b_conv_w'].reshape(NL, 4, 4, 128).transpose(0, 3, 2, 1)
    cb = full['b_conv_b'].reshape(NL, 4, 128).transpose(0, 2, 1)
    sel = np.zeros((4, 4, 128), np.float32)
    for h in range(4):
        sel[h, h, :] = 1.0
    clb = full['c_lb'].reshape(NL, 2, 128).transpose(2, 1, 0)
    rmask = np.ones((128, T), np.float32); rmask[:, ::64] = 0.0
    NCMP = (T - 32) // 16 + 1
    NSL = T // 64
    NT = T // 128
    w1 = full['d_cmp_w1'].reshape(NL, 2, 32, 64, 64).transpose(0, 1, 3, 2, 4).reshape(NL, 128, 32 * 64)
    w2 = full['d_cmp_w2']
    w2s = np.zeros((NL, 128, 320), np.float32)
    sw = np.concatenate([np.arange(32, 64), np.arange(0, 32)])
    w2s[:, 0:64, 0:64] = w2[:, 0]; w2s[:, 0:64, 64:128] = w2[:, 0]
    w2s[:, 0:64, 128:192] = w2[:, 0][:, :, sw]; w2s[:, 0:64, 192:256] = w2[:, 0][:, :, sw]
    w2s[:, 64:128, 256:320] = w2[:, 1]
    peT = full['d_cmp_pe'].transpose(0, 1, 3, 2).reshape(NL, 128, 32)
    starts = np.arange(NCMP) * 16
    blk = np.arange(NSL)
    overlap = ((starts[:, None] < (blk[None, :] + 1) * 64) & (starts[:, None] + 32 > blk[None, :] * 64)).astype(np.float32)
    vca0 = np.zeros((128, 65 + NSL), np.float32)
    vca0[:NCMP, 64] = 1.0
    vca0[:NCMP, 65:] = overlap
    t = np.arange(T)
    maskC = np.zeros((128, T), np.float32)
    maskC[:NCMP] = ((starts + 31)[:, None] <= t[None, :]).astype(np.float32)
    cur = (t // 64)[:, None]
    forced = (blk[None, :] == 0) | (blk[None, :] == cur) | (blk[None, :] == cur - 1)
    valid = blk[None, :] <= cur
    keep = (valid & ~forced).astype(np.float32)
    add = np.where(valid, np.where(forced, 1e6, 0.0), -1e30).astype(np.float32)
    keep = keep.reshape(NT, 128, NSL).transpose(1, 0, 2)
    add = add.reshape(NT, 128, NSL).transpose(1, 0, 2)
    return dict(cw=np.ascontiguousarray(cw), cb=np.ascontiguousarray(cb), sel=sel, clb=np.ascontiguousarray(clb), rmask=rmask,
                c_norm_g=full['c_norm_g'], w1s=np.ascontiguousarray(w1), w2s=w2s, peT=np.ascontiguousarray(peT), vca0=vca0, maskC=maskC,
                ikeep=np.ascontiguousarray(keep), iadd=np.ascontiguousarray(add))


class StopBuild(Exception):
    pass


class K:
    stop = None

    def chk(self, tag):
        if self.stop == tag:
            raise StopBuild(tag)


    def __init__(self, T=2048, NB=4, NL=2, dbg=(), mixers="ABCD"):
        self.mixers = mixers
        self.T, self.NB, self.NL = T, NB, NL
        self.NT = T // 128
        self.dbg = set(dbg)
        self.dbg_out = {}
        nc = bass.Bass("TRN2", target_bir_lowering=False)
        self.nc = nc
        self.P = Prog(nc, nepoch=NB * NL)
        self.out_toks = []
        self.in_names = []
        self.last_kb = None
        self.ctoks = []

    def const_barrier(self):
        for e in ("pe", "act", "dve", "pool"):
            for tok in self.ctoks:
                self.P.wait_tok(e, tok)
        self.ctoks = []

    def pe_seq(self, specs, R, W):
        P = self.P
        groups = []
        for kb, fn in specs:
            if groups and (kb is None or groups[-1][0] is None or groups[-1][0] == kb):
                if groups[-1][0] is None:
                    groups[-1][0] = kb
                groups[-1][1].append(fn)
            else:
                groups.append([kb, [fn]])
        for gi, (kb, fns) in enumerate(groups):
            def f(e, fns=fns):
                ins = None
                for fn in fns:
                    ins = fn(e)
                return ins
            fence = (kb is not None and self.last_kb is not None and kb != self.last_kb)
            P.op("pe", f, R=R, W=W, fence=fence)
            if kb is not None:
                self.last_kb = kb

    def din(self, name, shape, dtype=F32):
        self.in_names.append(name)
        return self.P.dram(name, shape, dtype, kind="ExternalInput")

    def dump(self, name, buf, ap, shape, R=None):
        if name not in self.dbg:
            return
        P = self.P
        d = P.dram("dbg_" + name, shape, F32, kind="ExternalOutput")
        tok = P.dma("pool", d[:], ap, R=[buf] if R is None else R, W=[d])
        self.out_toks.append(tok)

    def build(self):
        P, nc, T, NT = self.P, self.nc, self.T, self.NT
        NB, NL = self.NB, self.NL
        x = self.din("x", [NB, T, D])
        out = P.dram("out", [NB, T, D], F32, kind="ExternalOutput")
        w_in = self.din("w_in", [NL, D, NCR])
        ln0 = self.din("ln0", [2, D])
        lng = self.din("lng", [NL, 2, D])
        cosT_d = self.din("cosT", [128, T]); sinT_d = self.din("sinT", [128, T])
        ident_d = self.din("ident", [128, 128]); trige_d = self.din("tri_ge", [128, 512]); trilt_d = self.din("tri_lt", [128, 512])
        b_fm_d = self.din("b_fm", [NL, 128, len(FM)]); b_tm_d = self.din("b_tm", [NL, NTM]); b_if_d = self.din("b_if", [NL, 4, 2])
        self.w_bf = P.dram("w_bf", [NL, D, NCR], BF16, slots=NL * 4)
        sinks_d = self.din("a_sinks", [NL, 4])
        hbuf = P.dram("hbuf", [T, D], F32, slots=NT)
        self.x, self.out, self.w_in, self.hbuf = x, out, w_in, hbuf
        self.sinks_d = sinks_d
        self.cosT_d, self.sinT_d = cosT_d, sinT_d
        cosE_d = self.din("cosE", [128, 128]); sinE_d = self.din("sinE", [128, 128])
        self.cosE = P.sbuf("cosE_s", [128, 128]); self.sinE = P.sbuf("sinE_s", [128, 128])
        self.cst = [P.sbuf("cst%d" % i, [128, 2, 512]) for i in range(2)]
        self.ident = P.sbuf("ident_s", [128, 128]); self.tri_ge = P.sbuf("trige_s", [128, 512]); self.tri_lt = P.sbuf("trilt_s", [128, 512])
        for sb, dr in ((self.cosE, cosE_d), (self.sinE, sinE_d), (self.ident, ident_d), (self.tri_ge, trige_d), (self.tri_lt, trilt_d)):
            self.ctoks.append(P.dma("sp", sb[:], dr[:], R=[dr], W=[sb]))
        self.ln0_d = ln0; self.lng_d = lng
        self.b_fm = P.sbuf("b_fm_s", [128, NL, len(FM)])
        self.ctoks.append(P.dma("sp", self.b_fm[:], b_fm_d[:].rearrange("l p f -> p l f"), R=[b_fm_d], W=[self.b_fm]))
        self.b_tm_d = b_tm_d
        self.b_tm_s = P.sbuf("b_tm_s", [128, 768])
        self.b_if = P.sbuf("b_if_s", [4, NL, 2])
        self.ctoks.append(P.dma("sp", self.b_if[:], b_if_d[:].rearrange("l p f -> p l f"), R=[b_if_d], W=[self.b_if]))
        cw_d = self.din("cw", [NL, 128, 4, 4]); cb_d = self.din("cb", [NL, 128, 4]); sel_d = self.din("sel", [4, 4, 128])
        self.cw = P.sbuf("cw_s", [128, NL, 4, 4]); self.cb = P.sbuf("cb_s", [128, NL, 4]); self.sel = P.sbuf("sel_s", [4, 4, 128])
        self.ctoks.append(P.dma("sp", self.cw[:], cw_d[:].rearrange("l p j t -> p l j t"), R=[cw_d], W=[self.cw]))
        self.ctoks.append(P.dma("sp", self.cb[:], cb_d[:].rearrange("l p j -> p l j"), R=[cb_d], W=[self.cb]))
        self.ctoks.append(P.dma("sp", self.sel[:], sel_d[:], R=[sel_d], W=[self.sel]))
        clb_d = self.din("clb", [128, 2, NL]); cng_d = self.din("c_norm_g", [NL, 256])
        self.clb = P.sbuf("clb_s", [128, 2, NL]); self.gbc = P.sbuf("gbc", [128, NL, 256])
        self.ctoks.append(P.dma("sp", self.clb[:], clb_d[:], R=[clb_d], W=[self.clb]))
        self.ctoks.append(P.dma("sp", self.gbc[:], cng_d[:].partition_broadcast(128), R=[cng_d], W=[self.gbc]))
        self.NCMP = (T - 32) // 16 + 1
        self.NSL = T // 64
        NSL = self.NSL
        self.w1_d = self.din("w1s", [NL, 128, 2048]); w2_d = self.din("w2s", [NL, 128, 320]); pe_d = self.din("peT", [NL, 128, 32])
        vca_d = self.din("vca0", [128, 65 + NSL]); mc_d = self.din("maskC", [128, T]); ik_d = self.din("ikeep", [128, NT, NSL]); ia_d = self.din("iadd", [128, NT, NSL])
        self.w2s = P.sbuf("w2s_s", [128, NL, 320]); self.peT = P.sbuf("peT_s", [128, NL, 32]); self.vca = P.sbuf("vca", [128, 65 + NSL])
        self.maskC_d = mc_d; self.mct = [P.sbuf("mct%d" % i, [128, 128]) for i in range(2)]; self.ikeep = P.sbuf("ikeep_s", [128, NT, NSL]); self.iadd = P.sbuf("iadd_s", [128, NT, NSL])
        self.ctoks.append(P.dma("sp", self.w2s[:], w2_d[:].rearrange("l p f -> p l f"), R=[w2_d], W=[self.w2s]))
        self.ctoks.append(P.dma("sp", self.peT[:], pe_d[:].rearrange("l p f -> p l f"), R=[pe_d], W=[self.peT]))
        for sb, dr in ((self.vca, vca_d), (self.ikeep, ik_d), (self.iadd, ia_d)):
            self.ctoks.append(P.dma("sp", sb[:], dr[:], R=[dr], W=[sb]))
        self.esink = P.sbuf("esink", [128, NL, 4])
        self.ctoks.append(P.dma("sp", self.esink[:], sinks_d[:].partition_broadcast(128), R=[sinks_d], W=[self.esink]))
        for l in range(NL):
            for gi, g in enumerate(GL):
                s0, e0 = grange(g)
                P.dma("pool", self.w_bf[l, :, s0:e0], w_in[l, :, s0:e0], R=[w_in], W=[(self.w_bf, l * 4 + gi)])
        self.wt = [P.sbuf("wt%d" % i, [128, 8, 1152], BF16) for i in range(1)]
        wout = self.din("w_out", [NL, D, D])
        self.wout_bf = P.dram("wout_bf", [NL, D, D], BF16, slots=NL)
        for l in range(NL):
            P.dma("pool", self.wout_bf[l], wout[l], R=[wout], W=[(self.wout_bf, l)])
        self.wsel = 0
        self.hT = P.sbuf("hT", [128, 8, T], BF16, slots=NT)
        self.ybuf = P.dram("ybuf", [T, D], F32, slots=NT * 4)
        self.ps = [P.psum("ps%d" % i, [128, 512]) for i in range(8)]
        self.stat = [P.sbuf("stat%d" % i, [128, 2, 6]) for i in range(2)]
        self.mv = [P.sbuf("mv%d" % i, [128, 4]) for i in range(2)]
        self.const_barrier()
        try:
            for b in range(NB):
                self.seq(b)
        except (StopBuild, StopIteration):
            pass
        P.finish(self.out_toks)

    def layernorm(self, xt, i):
        P = self.P
        st, mv = self.stat[i], self.mv[i]
        xv = xt[:, 0:D]
        for c in range(2):
            P.op("dve", lambda e, c=c: e.bn_stats(out=st[:, c, :], in_=xt[:, c * 512:(c + 1) * 512]), R=[xt], W=[st])
        P.op("dve", lambda e: e.bn_aggr(out=mv[:, 0:2], in_=st[:]), R=[st], W=[mv])
        P.op("dve", lambda e: e.tensor_scalar(out=mv[:, 2:3], in0=mv[:, 1:2], scalar1=LN_EPS, scalar2=None, op0=ALU.add), R=[mv], W=[mv])
        P.op("act", lambda e: e.activation(out=mv[:, 2:3], in_=mv[:, 2:3], func=AF.Sqrt), R=[mv], W=[mv])
        P.op("dve", lambda e: e.reciprocal(out=mv[:, 3:4], in_=mv[:, 2:3]), R=[mv], W=[mv])
        P.op("dve", lambda e: e.tensor_scalar(out=xv, in0=xv, scalar1=mv[:, 0:1], scalar2=mv[:, 3:4], op0=ALU.subtract, op1=ALU.mult), R=[xt, mv], W=[xt])
        P.op("pool", lambda e: e.tensor_tensor(out=xv, in0=xv, in1=self.lnbc[:, 0:D], op=ALU.mult), R=[xt, self.lnbc], W=[xt])
        P.op("pool", lambda e: e.tensor_tensor(out=xv, in0=xv, in1=self.lnbc[:, D:2 * D], op=ALU.add), R=[xt, self.lnbc], W=[xt])

    def to_hT(self, xt, n):
        P = self.P
        for half in range(2):
            ps = self.ps[half]
            def f(e, half=half, ps=ps):
                ins = None
                for j in range(4):
                    dc = half * 4 + j
                    ins = e.transpose(out=ps[:, j * 128:(j + 1) * 128], in_=xt[:, dc * 128:(dc + 1) * 128], identity=self.ident[:])
                return ins
            P.op("pe", f, R=[xt, self.ident], W=[ps])
            P.op("act", lambda e, half=half, ps=ps: e.activation(
                out=self.hT[:, half * 4:(half + 1) * 4, n * 128:(n + 1) * 128],
                in_=ps[:].rearrange("p (j t) -> p j t", j=4), func=AF.Copy), R=[ps], W=[(self.hT, n)])

    def publish(self, tok):
        for e in ("pe", "act", "dve", "pool"):
            self.P.wait_tok(e, tok)

    def seq(self, b):
        P, T, NT = self.P, self.T, self.NT
        if b == 0:
            tok = P.op("act", lambda e: e.activation(out=self.esink[:], in_=self.esink[:], func=AF.Exp), W=[self.esink])
            self.publish(tok)
            self.alloc_work()
            self.xt = [self.big[3], self.big[4]]
            self.yt = [self.big[0], self.big[1]]
            self.lnbc = self.big[2]
        P.dma("sp", self.lnbc[:, 0:2 * D].rearrange("p (a d) -> p a d", a=2), self.ln0_d[:].partition_broadcast(128), R=[self.ln0_d], W=[self.lnbc])
        for n in range(NT):
            xt = self.xt[n % 2]
            P.dma("sp", xt[:, 0:D], self.x[b, n * 128:(n + 1) * 128, :], R=[self.x], W=[xt])
            self.layernorm(xt, n % 2)
            P.dma("sp", self.hbuf[n * 128:(n + 1) * 128, :], xt[:, 0:D], R=[xt], W=[(self.hbuf, n)])
            self.to_hT(xt, n)
        if b == 0:
            self.dump("hT", self.hT, self.hT[:, :, :], [128, 8, T])
        for l in range(self.NL):
            self.layer(b, l)

    def load_w(self, l, g, part):
        P = self.P
        wt = self.wt[0]
        s0, e0 = grange(g)
        tm0 = LAY[g + 'TM'][0]
        if part == 'fm':
            a, b_ = s0, tm0
        else:
            a, b_ = tm0, e0
            nt = e0 - tm0
            P.dma("sp", self.b_tm_s[:, 0:nt], self.b_tm_d[l, TMO[g + 'TM']:TMO[g + 'TM'] + nt].partition_broadcast(128), R=[self.b_tm_d], W=[self.b_tm_s])
        self.wbase = a
        gi = GL.index(g)
        P.dma("sp", wt[:, :, 0:b_ - a], self.w_bf[l, :, a:b_].rearrange("(dc p) c -> p dc c", p=128),
              R=[(self.w_bf, l * 4 + gi)], W=[wt])
        return wt

    def wcols(self, g, name):
        a, c = LAY[name]
        return a - self.wbase, c

    def proj_fm(self, ps, wt, g, name, c0, cw, mrows=128):
        a, c = self.wcols(g, name)
        n0, n1 = c0 // 128, (c0 + cw - 1) // 128
        def f(e):
            ins = None
            for dc in range(8):
                ins = e.matmul(ps[0:c, 0:cw], lhsT=wt[:, dc, a:a + c], rhs=self.hT[:, dc, c0:c0 + cw], start=(dc == 0), stop=(dc == 7))
            return ins
        self.P.op("pe", f, R=[wt, (self.hT, range(n0, n1 + 1))], W=[ps])

    def proj_tm(self, pss, wt, g, name, n):
        a, c = self.wcols(g, name)
        def f(e):
            ins = None
            for j, ps in enumerate(pss):
                cc = min(512, c - j * 512)
                for dc in range(8):
                    ins = e.matmul(ps[:, 0:cc], lhsT=self.hT[:, dc, n * 128:(n + 1) * 128], rhs=wt[:, dc, a + j * 512:a + j * 512 + cc],
                                   start=(dc == 0), stop=(dc == 7))
            return ins
        self.P.op("pe", f, R=[wt, (self.hT, n)], W=list(pss))

    def alloc_work(self):
        P, T, NT = self.P, self.T, self.NT
        BW = max(T + 128, 2176)
        self.big = [P.sbuf("big%d" % i, [128, BW]) for i in range(6)]
        self.fmb = self.big[0:4]
        self.t1 = [P.sbuf("t1_%d" % i, [128, 512]) for i in range(2)]
        self.vaug = P.sbuf("vaug", [128, NT, 4, 65], slots=NT)
        P.op("pool", lambda e: e.memset(self.vaug[:], 1.0), W=[self.vaug])
        self.zt = [P.sbuf("zt%d" % i, [128, 768]) for i in range(2)]
        self.pt = [P.sbuf("pt%d" % i, [128, 512]) for i in range(3)]
        self.t2 = [self.pt[0], self.pt[1]]
        self.ya = [P.sbuf("ya%d" % i, [128, 256]) for i in range(2)]
        self.sm = [P.sbuf("sm%d" % i, [128, 16]) for i in range(2)]
        self.cnt = 0
        self.sc = self.big[4:6]
        self.gbuf = P.dram("gbuf", [4, 3, T], F32)
        self.gsl = [P.sbuf("gsl%d" % i, [4, 3, 128]) for i in range(2)]
        self.EAc = P.sbuf("EAc", [128, T // 64, 2])
        self.z4 = P.sbuf("z4", [4, 2])
        self.publish(P.op("pool", lambda e: e.memset(self.z4[:], 0.0), W=[self.z4]))
        self.nbif = P.sbuf("nbif", [4, self.NL, 2])
        self.publish(P.op("dve", lambda e: e.tensor_scalar(out=self.nbif[:], in0=self.b_if[:], scalar1=-1.0, scalar2=None, op0=ALU.mult), W=[self.nbif]))
        self.smb = [P.sbuf("smb%d" % i, [128, 32]) for i in range(2)]
        self.w1 = [self.pt[0], self.pt[1]]
        self.wib = [P.sbuf("wib%d" % i, [128, 512]) for i in range(2)]
        self.qtl = [P.sbuf("qtl%d" % i, [128, 2, 128]) for i in range(2)]
        self.kw = [P.sbuf("kw%d" % i, [128, 256]) for i in range(2)]
        self.caug = P.sbuf("caug", [128, 2, 65])
        self.hb = [P.sbuf("hb%d" % i, [128, 256]) for i in range(2)]
        self.lbt = P.sbuf("lbt", [128, self.NL, 2, 2])
        P.op("pool", lambda e: e.memset(self.lbt[:, :, :, 0], 0.0), W=[self.lbt])
        P.op("pool", lambda e: e.memset(self.lbt[:, :, :, 1], 1.0), W=[self.lbt])
        if self.NL > 1:
            P.op("dve", lambda e: e.tensor_tensor(out=self.lbt[:, 1, :, 0], in0=self.clb[:, :, 1], in1=self.clb[:, :, 0], op=ALU.subtract), W=[self.lbt])
            P.op("act", lambda e: e.activation(out=self.lbt[:, 1, :, 0], in_=self.lbt[:, 1, :, 0], func=AF.Sigmoid), R=[self.lbt], W=[self.lbt])
            P.op("dve", lambda e: e.tensor_scalar(out=self.lbt[:, 1, :, 1], in0=self.lbt[:, 1, :, 0], scalar1=-1.0, scalar2=1.0, op0=ALU.mult, op1=ALU.add), R=[self.lbt], W=[self.lbt])
        self.publish(P.op("dve", lambda e: e.tensor_copy(out=self.lbt[:, 0, :, 0], in_=self.lbt[:, 0, :, 0]), R=[self.lbt], W=[self.lbt]))
        self.S2 = P.sbuf("S2", [128, 64]); self.st = [P.sbuf("st%d" % i, [128, 64]) for i in range(2)]
        self.at = [P.sbuf("at%d" % i, [128, 128]) for i in range(2)]
        self.kh = [P.sbuf("kh%d" % i, [128, 128]) for i in range(2)]
        self.oc = [P.sbuf("oc%d" % i, [128, 128]) for i in range(2)]
        self.osq = [P.sbuf("osq%d" % i, [128, 128]) for i in range(2)]
        self.vz = [P.sbuf("vz%d" % i, [128, 256]) for i in range(2)]
        self.cbias = P.sbuf("cbias", [128, 2])
        self.h1 = P.sbuf("h1", [128, 128]); self.kcr = P.sbuf("kcr", [128, 128]); self.kct = P.sbuf("kct", [128, 128])
        self.pc = self.wib
        self.ocmp = [P.sbuf("ocmp%d" % i, [128, 256]) for i in range(2)]
        self.imp = [P.sbuf("imp%d" % i, [128, 64]) for i in range(2)]
        self.selb = [P.sbuf("selb%d" % i, [128, 32]) for i in range(2)]
        self.gs = [P.sbuf("gs%d" % i, [128, 16]) for i in range(2)]
        self.yd = [P.sbuf("yd%d" % i, [128, 256]) for i in range(2)]
        self.yd2 = [P.sbuf("yd2_%d" % i, [128, 256]) for i in range(2)]
        self.zero256 = P.sbuf("zero256", [128, 256])
        self.publish(P.op("pool", lambda e: e.memset(self.zero256[:], 0.0), W=[self.zero256]))
        self.yTt = [P.sbuf("yTt%d" % i, [128, 8, 128], BF16) for i in range(2)]

    def rope_block(self, wt, g, blk, blks, dst, l):
        P, T = self.P, self.T
        SW = min(512, T)
        for s in range(T // SW):
            c0 = s * SW
            i = self.cnt % 2; self.cnt += 1
            pa, pb = self.ps[0 + i], self.ps[2 + i]
            t1, t2 = self.t1[i], self.t2[i]
            cst = self.cst[i]
            P.dma("sp", cst[:, 0, 0:SW], self.cosT_d[:, c0:c0 + SW], R=[self.cosT_d], W=[cst])
            P.dma("sp", cst[:, 1, 0:SW], self.sinT_d[:, c0:c0 + SW], R=[self.sinT_d], W=[cst])
            self.proj_fm(pa, wt, g, blk, c0, SW)
            self.proj_fm(pb, wt, g, blks, c0, SW)
            P.op("act", lambda e: e.activation(out=t1[:, 0:SW], in_=pa[:, 0:SW], func=AF.Identity, bias=self.b_fm[:, l, FMI[blk]:FMI[blk] + 1]), R=[pa], W=[t1])
            P.op("act", lambda e: e.activation(out=t2[:, 0:SW], in_=pb[:, 0:SW], func=AF.Identity, bias=self.b_fm[:, l, FMI[blks]:FMI[blks] + 1]), R=[pb], W=[t2])
            P.op("dve", lambda e: e.tensor_tensor(out=t1[:, 0:SW], in0=t1[:, 0:SW], in1=cst[:, 0, 0:SW], op=ALU.mult), R=[t1, cst], W=[t1])
            P.op("pool", lambda e: e.tensor_tensor(out=t2[:, 0:SW], in0=t2[:, 0:SW], in1=cst[:, 1, 0:SW], op=ALU.mult), R=[t2, cst], W=[t2])
            P.op("dve", lambda e: e.tensor_tensor(out=dst[:, c0:c0 + SW], in0=t1[:, 0:SW], in1=t2[:, 0:SW], op=ALU.add), R=[t1, t2], W=[dst])

    def emit_y(self, ya, n, m0):
        mi = m0 // 2
        self.P.dma("sp", self.ybuf[n * 128:(n + 1) * 128, m0 * 128:m0 * 128 + 256], ya[:], R=[ya], W=[(self.ybuf, n * 4 + mi)])

    def mixer_A(self, b, l, wt):
        P, T, NT = self.P, self.T, self.NT
        g = 'A'
        qr = [self.fmb[0], self.fmb[1]]
        kr = self.fmb[2]
        self.chk("A0")
        self.rope_block(wt, g, 'AQ0', 'AQ0s', qr[0], l)
        self.chk("A1")
        self.rope_block(wt, g, 'AQ1', 'AQ1s', qr[1], l)
        self.rope_block(wt, g, 'AK', 'AKs', kr, l)
        self.chk("A1c")
        if b == 0 and l == 0:
            self.dump("A_q0", qr[0], qr[0][:], [128, T]); self.dump("A_k", kr, kr[:], [128, T])
        tmo = 0
        self.load_w(l, g, 'tm')
        self.chk("A1d")
        for n in range(NT):
            i = n % 2
            ptm = self.ps[6]
            zt, ya, sm = self.zt[i], self.ya[i], self.sm[i]
            self.proj_tm([ptm], wt, g, 'ATM', n)
            self.chk("A1e")
            P.op("dve", lambda e: e.tensor_tensor(out=self.vaug[:, n, 0:2, 0:64], in0=ptm[:, 0:128].rearrange("p (g d) -> p g d", g=2),
                                                  in1=self.b_tm_s[:, tmo:tmo + 128].rearrange("p (g d) -> p g d", g=2), op=ALU.add),
                 R=[ptm, self.b_tm_s], W=[(self.vaug, n)])
            self.chk("A2a")
            P.op("dve", lambda e: e.tensor_tensor(out=zt[:, 0:256], in0=ptm[:, 128:384], in1=self.b_tm_s[:, tmo + 128:tmo + 384], op=ALU.add), R=[ptm, self.b_tm_s], W=[zt])
            self.chk("A2b")
            P.op("act", lambda e: e.activation(out=zt[:, 0:256], in_=zt[:, 0:256], func=AF.Silu), R=[zt], W=[zt])
            self.chk("A2")
            kts = [kt for kt in (n - 1, n) if kt >= 0]
            po = self.ps[7]
            pts = []
            for kt in kts:
                psn = self.ps[2 + (self.cnt % 2)]; pt = self.pt[self.cnt % 3]; self.cnt += 1
                specs = []
                for gg in range(2):
                    for r in range(2):
                        specs.append((gg * 64, lambda e, gg=gg, r=r, kt=kt, psn=psn: e.matmul(
                            psn[:, (gg * 2 + r) * 128:(gg * 2 + r + 1) * 128], lhsT=kr[gg * 64:(gg + 1) * 64, kt * 128:(kt + 1) * 128],
                            rhs=qr[r][gg * 64:(gg + 1) * 64, n * 128:(n + 1) * 128], start=True, stop=True)))
                self.pe_seq(specs, R=[kr, qr[0], qr[1]], W=[psn])
                self.chk("A3a")
                P.op("act", lambda e, psn=psn, pt=pt: e.activation(out=pt[:], in_=psn[:], func=AF.Exp, scale=0.125), R=[psn], W=[pt])
                self.chk("A3b")
                mask = self.tri_ge if kt == n else self.tri_lt
                P.op("dve", lambda e, pt=pt, mask=mask: e.tensor_tensor(out=pt[:], in0=pt[:], in1=mask[:], op=ALU.mult), R=[pt], W=[pt])
                pts.append((kt, pt))
            self.chk("A3")
            def f(e):
                ins = None
                for gg in range(2):
                    for r in range(2):
                        h = gg * 2 + r
                        for j, (kt, pt) in enumerate(pts):
                            ins = e.matmul(po[:, h * 65:(h + 1) * 65], lhsT=pt[:, h * 128:(h + 1) * 128], rhs=self.vaug[:, kt, gg, :],
                                           start=(j == 0), stop=(j == len(pts) - 1))
                return ins
            P.op("pe", f, R=[p for _, p in pts] + [(self.vaug, kts)], W=[po])
            pov = po[:, 0:260].rearrange("p (h c) -> p h c", h=4)
            P.op("dve", lambda e: e.tensor_tensor(out=sm[:, 0:4], in0=pov[:, :, 64], in1=self.esink[:, l, :], op=ALU.add), R=[po], W=[sm])
            P.op("dve", lambda e: e.reciprocal(out=sm[:, 4:8], in_=sm[:, 0:4]), R=[sm], W=[sm])
            yav = ya[:].rearrange("p (h d) -> p h d", h=4)
            P.op("dve", lambda e: e.tensor_tensor(out=yav, in0=pov[:, :, 0:64], in1=sm[:, 4:8].unsqueeze(2).to_broadcast([128, 4, 64]), op=ALU.mult), R=[po, sm], W=[ya])
            P.op("pool", lambda e: e.tensor_tensor(out=ya[:], in0=ya[:], in1=zt[:, 0:256], op=ALU.mult), R=[ya, zt], W=[ya])
            self.chk("A4")
            self.emit_y(ya, n, 0)

    def mixer_B(self, b, l, wt):
        P, T, NT = self.P, self.T, self.NT
        g = 'B'
        SW = min(512, T)
        raw = self.sc[0]
        X1, X2, X3, X4 = self.big[0:4]
        for s_ in range(T // SW):
            c0 = s_ * SW
            pi, pf = self.ps[0], self.ps[1]
            self.proj_fm(pi, wt, g, 'BI', c0, SW)
            self.proj_fm(pf, wt, g, 'BF', c0, SW)
            P.op("act", lambda e, c0=c0: e.activation(out=X1[0:4, c0:c0 + SW], in_=pi[0:4, 0:SW], func=AF.Identity, bias=self.b_if[:, l, 0:1]), R=[pi], W=[X1])
            P.op("act", lambda e, c0=c0: e.activation(out=X4[0:4, c0:c0 + SW], in_=pf[0:4, 0:SW], func=AF.Exp, scale=-1.0, bias=self.nbif[:, l, 1:2]), R=[pf], W=[X4])
        P.op("act", lambda e: e.activation(out=X4[0:4, 0:T], in_=X4[0:4, 0:T], func=AF.Ln, bias=1.0), R=[X4], W=[X4])
        P.op("dve", lambda e: e.tensor_tensor_scan(out=X3[0:4, 0:T], data0=X4[0:4, 0:T], data1=self.z4[:, 0:1].to_broadcast([4, T]), initial=0.0, op0=ALU.add, op1=ALU.add), R=[X4], W=[X3])
        P.op("dve", lambda e: e.tensor_tensor(out=X1[0:4, 0:T], in0=X1[0:4, 0:T], in1=X3[0:4, 0:T], op=ALU.add), R=[X1, X3], W=[X1])
        P.op("pool", lambda e: e.memset(X2[0:4, 0:128], 0.0), W=[X2])
        P.op("dve", lambda e: e.tensor_tensor_scan(out=X2[0:4, 128:128 + T], data0=X1[0:4, 0:T], data1=self.z4[:, 0:1].to_broadcast([4, T]), initial=0.0, op0=ALU.max, op1=ALU.add), R=[X1], W=[X2])
        P.op("dve", lambda e: e.tensor_tensor(out=X3[0:4, 0:T], in0=X3[0:4, 0:T], in1=X2[0:4, 128:128 + T], op=ALU.subtract), R=[X3, X2], W=[X3])
        P.op("act", lambda e: e.activation(out=X3[0:4, 0:T], in_=X3[0:4, 0:T], func=AF.Exp), R=[X3], W=[X3])
        gprev = X2[0:4, 0:T].rearrange("p (c t) -> p c t", t=128)[:, :, 127:128].to_broadcast([4, NT, 128])
        P.op("dve", lambda e: e.tensor_tensor(out=X4[0:4, 0:T].rearrange("p (c t) -> p c t", t=128), in0=X2[0:4, 128:128 + T].rearrange("p (c t) -> p c t", t=128),
                                              in1=gprev, op=ALU.subtract), R=[X2], W=[X4])
        P.op("dve", lambda e: e.tensor_tensor(out=X1[0:4, 0:T].rearrange("p (c t) -> p c t", t=128), in0=X1[0:4, 0:T].rearrange("p (c t) -> p c t", t=128),
                                              in1=gprev, op=ALU.subtract), R=[X1, X2], W=[X1])
        P.dma("sp", self.gbuf[:, 0, :], X4[0:4, 0:T], R=[X4], W=[self.gbuf])
        P.dma("sp", self.gbuf[:, 1, :], X1[0:4, 0:T], R=[X1], W=[self.gbuf])
        P.dma("sp", self.gbuf[:, 2, :], X3[0:4, 0:T], R=[X3], W=[self.gbuf])
        for j, blk in enumerate(['BQ0', 'BQ1', 'BK0', 'BK1']):
            dst = self.fmb[j]
            P.op("pool", lambda e: e.memset(raw[:, 0:3], 0.0), W=[raw])
            for s_ in range(T // SW):
                c0 = s_ * SW
                ps = self.ps[self.cnt % 2]; self.cnt += 1
                self.proj_fm(ps, wt, g, blk, c0, SW)
                P.op("act", lambda e, ps=ps, c0=c0: e.activation(out=raw[:, 3 + c0:3 + c0 + SW], in_=ps[:, 0:SW], func=AF.Identity,
                                                                 bias=self.b_fm[:, l, FMI[blk]:FMI[blk] + 1]), R=[ps], W=[raw])
            P.op("dve", lambda e: e.tensor_scalar(out=dst[:, 0:T], in0=raw[:, 3:3 + T], scalar1=self.cw[:, l, j, 3:4], scalar2=self.cb[:, l, j:j + 1],
                                                  op0=ALU.mult, op1=ALU.add), R=[raw], W=[dst])
            for tap in (2, 1, 0):
                P.op("dve", lambda e, tap=tap: e.scalar_tensor_tensor(out=dst[:, 0:T], in0=raw[:, tap:tap + T], scalar=self.cw[:, l, j, tap:tap + 1],
                                                                      in1=dst[:, 0:T], op0=ALU.mult, op1=ALU.add), R=[raw, dst], W=[dst])
            P.op("act", lambda e: e.activation(out=dst[:, 0:T], in_=dst[:, 0:T], func=AF.Silu), R=[dst], W=[dst])
        qT = [self.fmb[0], self.fmb[1]]
        kT = [self.fmb[2], self.fmb[3]]
        self.load_w(l, g, 'tm')
        tmo = 0
        caug = self.caug
        for c in range(NT):
            i = c % 2
            cs_ = slice(c * 128, (c + 1) * 128)
            zt, hb, smb, w1, wib, qtl, kw = self.zt[i], self.hb[i], self.smb[i], self.w1[i], self.wib[i], self.qtl[i], self.kw[i]
            ptm = [self.ps[6], self.ps[7]]
            self.proj_tm(ptm, wt, g, 'BTM', c)
            P.op("dve", lambda e: e.tensor_tensor(out=self.vaug[:, c, :, 0:64], in0=ptm[0][:, 0:256].rearrange("p (h d) -> p h d", h=4),
                                                  in1=self.b_tm_s[:, tmo:tmo + 256].rearrange("p (h d) -> p h d", h=4), op=ALU.add),
                 R=[ptm[0], self.b_tm_s], W=[(self.vaug, c)])
            P.op("dve", lambda e: e.tensor_tensor(out=zt[:, 0:256], in0=ptm[0][:, 256:512], in1=self.b_tm_s[:, tmo + 256:tmo + 512], op=ALU.add), R=[ptm[0], self.b_tm_s], W=[zt])
            P.op("dve", lambda e: e.tensor_tensor(out=zt[:, 256:512], in0=ptm[1][:, 0:256], in1=self.b_tm_s[:, tmo + 512:tmo + 768], op=ALU.add), R=[ptm[1], self.b_tm_s], W=[zt])
            P.op("act", lambda e: e.activation(out=zt[:, 0:256], in_=zt[:, 0:256], func=AF.Sigmoid), R=[zt], W=[zt])
            P.op("act", lambda e: e.activation(out=zt[:, 256:512], in_=zt[:, 256:512], func=AF.Silu), R=[zt], W=[zt])
            gsl = self.gsl[i]
            P.dma("sp", gsl[:], self.gbuf[:, :, cs_], R=[self.gbuf], W=[gsl])
            psT = self.ps[0]
            def f(e):
                e.transpose(out=psT[:, 0:4], in_=gsl[0:4, 1, :], identity=self.ident[0:4, 0:4])
                return e.transpose(out=psT[:, 4:8], in_=gsl[0:4, 2, :], identity=self.ident[0:4, 0:4])
            self.pe_seq([(0, f)], R=[gsl], W=[psT])
            P.op("dve", lambda e: e.tensor_copy(out=smb[:, 0:8], in_=psT[:, 0:8]), R=[psT], W=[smb])
            psG = self.ps[1]
            def f(e):
                ins = None
                for h in range(4):
                    ins = e.matmul(psG[:, h * 128:(h + 1) * 128], lhsT=self.sel[0:4, h, :], rhs=gsl[0:4, 0, :], start=True, stop=True)
                return ins
            self.pe_seq([(0, f)], R=[gsl], W=[psG])
            for h in range(4):
                P.op("act", lambda e, h=h: e.activation(out=w1[:, h * 128:(h + 1) * 128], in_=psG[:, h * 128:(h + 1) * 128], func=AF.Exp, scale=-1.0,
                                                        bias=smb[:, h:h + 1]), R=[psG, smb], W=[w1])
            P.op("act", lambda e: e.activation(out=wib[:], in_=psG[:], func=AF.Exp, scale=-1.0), R=[psG], W=[wib])
            P.op("pool", lambda e: e.tensor_tensor(out=w1[:], in0=w1[:], in1=self.tri_ge[:], op=ALU.mult), R=[w1], W=[w1])
            P.op("dve", lambda e: e.tensor_tensor(out=smb[:, 8:12], in0=smb[:, 0:4], in1=psG[:].rearrange("p (h t) -> p h t", h=4)[:, :, 127], op=ALU.subtract),
                 R=[smb, psG], W=[smb])
            P.op("act", lambda e: e.activation(out=smb[:, 8:12], in_=smb[:, 8:12], func=AF.Exp), R=[smb], W=[smb])
            psS = self.ps[2]
            specs = []
            for h in (0, 2, 1, 3):
                j, base = h // 2, (h % 2) * 64
                specs.append((base, lambda e, h=h, j=j, base=base: e.matmul(psS[:, h * 128:(h + 1) * 128], lhsT=kT[j][base:base + 64, cs_],
                                                                          rhs=qT[j][base:base + 64, cs_], start=True, stop=True)))
            self.pe_seq(specs, R=[kT[0], kT[1], qT[0], qT[1]], W=[psS])
            P.op("dve", lambda e: e.scalar_tensor_tensor(out=w1[:], in0=psS[:], scalar=0.125, in1=w1[:], op0=ALU.mult, op1=ALU.mult), R=[psS, w1], W=[w1])
            if c > 0:
                for h in range(4):
                    j, base = h // 2, (h % 2) * 64
                    P.op("dve", lambda e, h=h, j=j, base=base: e.scalar_tensor_tensor(
                        out=qtl[base:base + 64, j, :], in0=qT[j][base:base + 64, cs_], scalar=0.125, in1=wib[base:base + 64, h * 128:(h + 1) * 128],
                        op0=ALU.mult, op1=ALU.mult), R=[qT[j], wib], W=[qtl])
            po = self.ps[3]
            specs = []
            for h in (0, 2, 1, 3):
                j, base = h // 2, (h % 2) * 64
                specs.append((None, lambda e, h=h: e.matmul(po[:, h * 65:(h + 1) * 65], lhsT=w1[:, h * 128:(h + 1) * 128], rhs=self.vaug[:, c, h, :], start=True, stop=(c == 0))))
                if c > 0:
                    specs.append((base, lambda e, h=h, j=j, base=base: e.matmul(po[:, h * 65:(h + 1) * 65], lhsT=qtl[base:base + 64, j, :], rhs=caug[base:base + 64, j, :],
                                                                              start=False, stop=True)))
            self.pe_seq(specs, R=[w1, (self.vaug, c)] + ([qtl, caug] if c > 0 else []), W=[po])
            pov = po[:, 0:260].rearrange("p (h c) -> p h c", h=4)
            P.op("act", lambda e: e.activation(out=smb[:, 12:16], in_=pov[:, :, 64], func=AF.Abs), R=[po], W=[smb])
            P.op("dve", lambda e: e.tensor_tensor(out=smb[:, 12:16], in0=smb[:, 12:16], in1=smb[:, 4:8], op=ALU.max), R=[smb], W=[smb])
            P.op("dve", lambda e: e.reciprocal(out=smb[:, 16:20], in_=smb[:, 12:16]), R=[smb], W=[smb])
            hbv = hb[:].rearrange("p (h d) -> p h d", h=4)
            P.op("dve", lambda e: e.tensor_tensor(out=hbv, in0=pov[:, :, 0:64], in1=smb[:, 16:20].unsqueeze(2).to_broadcast([128, 4, 64]), op=ALU.mult), R=[po, smb], W=[hb])
            P.op("pool", lambda e: e.tensor_tensor(out=hb[:], in0=hb[:], in1=zt[:, 0:256], op=ALU.mult), R=[hb, zt], W=[hb])
            P.op("pool", lambda e: e.tensor_tensor(out=hb[:], in0=hb[:], in1=zt[:, 256:512], op=ALU.mult), R=[hb, zt], W=[hb])
            self.emit_y(hb, c, 2)
            if c < NT - 1:
                pk = self.ps[4]
                def f(e):
                    e.transpose(out=pk[:, 0:128], in_=kT[0][:, cs_], identity=self.ident[:])
                    return e.transpose(out=pk[:, 128:256], in_=kT[1][:, cs_], identity=self.ident[:])
                P.op("pe", f, R=[kT[0], kT[1]], W=[pk])
                P.op("dve", lambda e: e.tensor_tensor(out=kw[:].rearrange("p (h d) -> p h d", h=4), in0=pk[:, 0:256].rearrange("p (h d) -> p h d", h=4),
                                                      in1=smb[:, 8:12].unsqueeze(2).to_broadcast([128, 4, 64]), op=ALU.mult), R=[pk, smb], W=[kw])
                pu = self.ps[5]
                def f(e):
                    ins = None
                    for j in range(2):
                        ins = e.matmul(pu[:, j * 130:(j + 1) * 130], lhsT=kw[:, j * 128:(j + 1) * 128],
                                       rhs=self.vaug[:, c, 2 * j:2 * j + 2, :], start=True, stop=True)
                    return ins
                P.op("pe", f, R=[kw, (self.vaug, c)], W=[pu])
                for h in range(4):
                    j, base = h // 2, (h % 2) * 64
                    src = pu[base:base + 64, j * 130 + (h % 2) * 65:j * 130 + (h % 2) * 65 + 65]
                    if c == 0:
                        P.op("dve", lambda e, src=src, j=j, base=base: e.tensor_copy(out=caug[base:base + 64, j, :], in_=src), R=[pu], W=[caug])
                    else:
                        P.op("dve", lambda e, src=src, j=j, base=base, h=h: e.scalar_tensor_tensor(
                            out=caug[base:base + 64, j, :], in0=caug[base:base + 64, j, :], scalar=wib[base:base + 64, h * 128 + 127:h * 128 + 128], in1=src,
                            op0=ALU.mult, op1=ALU.add), R=[pu, caug, wib], W=[caug])

    def mixer_C(self, b, l, wt):
        P, T, NT = self.P, self.T, self.NT
        g = 'C'
        SW = min(512, T)
        NC = T // 64
        qs, fk, lfd, aa, tmp, kt_ = self.big
        qt_, kh_, EAc = qs, fk, self.EAc
        v3 = lambda t: t[:, 0:T].rearrange("p (c t) -> p c t", t=64)
        tmo = 0
        for j in range(2):
            bq, bf = 'CQ%d' % j, 'CF%d' % j
            if j > 0:
                wt = self.load_w(l, g, 'fm')
            for s_ in range(T // SW):
                c0 = s_ * SW
                pa, pb_ = self.ps[0], self.ps[1]
                self.proj_fm(pa, wt, g, bq, c0, SW)
                self.proj_fm(pb_, wt, g, bf, c0, SW)
                P.op("act", lambda e, c0=c0: e.activation(out=qs[:, c0:c0 + SW], in_=pa[:, 0:SW], func=AF.Silu, bias=self.b_fm[:, l, FMI[bq]:FMI[bq] + 1]), R=[pa], W=[qs])
                P.op("act", lambda e, c0=c0: e.activation(out=fk[:, c0:c0 + SW], in_=pb_[:, 0:SW], func=AF.Sigmoid, bias=self.b_fm[:, l, FMI[bf]:FMI[bf] + 1]), R=[pb_], W=[fk])
            P.op("dve", lambda e: e.tensor_scalar(out=fk[:, 0:T], in0=fk[:, 0:T], scalar1=self.lbt[:, l, j, 1:2], scalar2=self.lbt[:, l, j, 0:1], op0=ALU.mult, op1=ALU.add), R=[fk], W=[fk])
            P.op("act", lambda e: e.activation(out=lfd[:, 0:T], in_=fk[:, 0:T], func=AF.Ln), R=[fk], W=[lfd])
            P.op("dve", lambda e: e.tensor_scalar(out=fk[:, 0:T], in0=fk[:, 0:T], scalar1=-1.0, scalar2=1.0, op0=ALU.mult, op1=ALU.add), R=[fk, lfd], W=[fk])
            P.op("pool", lambda e: e.memset(kt_[:, 0:T], 1.0), W=[kt_])
            P.op("pool", lambda e: e.memset(v3(kt_)[:, :, 0:1], 0.0), W=[kt_])
            P.op("dve", lambda e: e.tensor_tensor_scan(out=aa[:, 0:T], data0=kt_[:, 0:T], data1=lfd[:, 0:T], initial=0.0, op0=ALU.mult, op1=ALU.add), R=[kt_, lfd], W=[aa])
            P.op("act", lambda e: e.activation(out=EAc[:, :, 0], in_=v3(aa)[:, :, 31], func=AF.Exp), R=[aa], W=[EAc])
            P.op("act", lambda e: e.activation(out=EAc[:, :, 1], in_=v3(aa)[:, :, 63], func=AF.Exp), R=[aa], W=[EAc])
            P.op("dve", lambda e: e.tensor_tensor(out=v3(lfd), in0=v3(aa), in1=v3(aa)[:, :, 31:32].to_broadcast([128, NC, 64]), op=ALU.subtract), R=[aa], W=[lfd])
            P.op("act", lambda e: e.activation(out=tmp[:, 0:T], in_=lfd[:, 0:T], func=AF.Exp), R=[lfd], W=[tmp])
            P.op("dve", lambda e: e.tensor_tensor(out=qt_[:, 0:T], in0=qs[:, 0:T], in1=tmp[:, 0:T], op=ALU.mult), R=[qs, tmp], W=[qt_])
            P.op("act", lambda e: e.activation(out=tmp[:, 0:T], in_=lfd[:, 0:T], func=AF.Exp, scale=-1.0), R=[lfd, qt_], W=[tmp])
            P.op("pool", lambda e: e.tensor_tensor(out=kt_[:, 0:T], in0=fk[:, 0:T], in1=tmp[:, 0:T], op=ALU.mult), R=[fk, tmp], W=[kt_])
            P.op("dve", lambda e: e.tensor_tensor(out=v3(tmp), in0=v3(lfd)[:, :, 63:64].to_broadcast([128, NC, 64]), in1=v3(lfd), op=ALU.subtract), R=[lfd, kt_], W=[tmp])
            P.op("act", lambda e: e.activation(out=tmp[:, 0:T], in_=tmp[:, 0:T], func=AF.Exp), R=[tmp], W=[tmp])
            P.op("pool", lambda e: e.tensor_tensor(out=kh_[:, 0:T], in0=fk[:, 0:T], in1=tmp[:, 0:T], op=ALU.mult), R=[fk, tmp], W=[kh_])
            S2 = self.S2
            self.load_w(l, g, 'tm')
            aC, cC = self.wcols(g, 'CTM')
            for n in range(NT):
                i = n % 2
                vz, oc, osq, sm = self.vz[i], self.oc[i], self.osq[i], self.sm[i]
                ptm = self.ps[6]
                def f(e):
                    ins = None
                    for r, off in enumerate((j * 128, 256 + j * 128)):
                        for dc in range(8):
                            ins = e.matmul(ptm[:, r * 128:(r + 1) * 128], lhsT=self.hT[:, dc, n * 128:(n + 1) * 128], rhs=wt[:, dc, aC + off:aC + off + 128],
                                           start=(dc == 0), stop=(dc == 7))
                    return ins
                P.op("pe", f, R=[wt, (self.hT, n)], W=[ptm])
                P.op("dve", lambda e: e.tensor_tensor(out=vz[:, 0:128], in0=ptm[:, 0:128], in1=self.b_tm_s[:, tmo + j * 128:tmo + (j + 1) * 128], op=ALU.add), R=[ptm, self.b_tm_s], W=[vz])
                P.op("dve", lambda e: e.tensor_tensor(out=vz[:, 128:256], in0=ptm[:, 128:256], in1=self.b_tm_s[:, tmo + 256 + j * 128:tmo + 256 + (j + 1) * 128], op=ALU.add), R=[ptm, self.b_tm_s], W=[vz])
                P.op("act", lambda e: e.activation(out=vz[:, 128:256], in_=vz[:, 128:256], func=AF.Silu), R=[vz], W=[vz])
                po = self.ps[4 + i]
                khb = self.kh[i]
                if n * 2 < NC - 1:
                    pk = self.ps[0]
                    P.op("pe", lambda e, pk=pk: e.transpose(out=pk[:, 0:128], in_=kh_[:, n * 128:(n + 1) * 128], identity=self.ident[:]), R=[kh_], W=[pk])
                    P.op("act", lambda e, pk=pk: e.activation(out=khb[:], in_=pk[:, 0:128], func=AF.Copy), R=[pk], W=[khb])
                for cp in range(2):
                    c = n * 2 + cp
                    pb = cp * 64
                    cs_ = slice(c * 64, (c + 1) * 64)
                    at, kh, st = self.at[cp], khb, self.st[cp]
                    psA = self.ps[2 + cp]
                    specs = []
                    for hh in range(2):
                        base = hh * 64
                        specs.append((base, lambda e, hh=hh, base=base, psA=psA, pb=pb, cs_=cs_: e.matmul(
                            psA[pb:pb + 64, hh * 64:(hh + 1) * 64], lhsT=kt_[base:base + 64, cs_], rhs=qt_[base:base + 64, cs_], start=True, stop=True)))
                    self.pe_seq(specs, R=[kt_, qt_], W=[psA])
                    pU = self.ps[1] if cp == 0 else self.ps[7]
                    if c < NC - 1:
                        self.pe_seq([(pb, lambda e, pU=pU, kh=kh, pb=pb: e.matmul(pU[:, 0:128], lhsT=kh[pb:pb + 64, :], rhs=vz[pb:pb + 64, 0:128], start=True, stop=True))],
                                    R=[kh, vz], W=[pU])
                    mask = self.tri_ge[pb:pb + 64, :].rearrange("p (r f) -> p r f", f=128)[:, 0:2, pb:pb + 64]
                    P.op("dve", lambda e, at=at, psA=psA, mask=mask, pb=pb: e.tensor_tensor(out=at[pb:pb + 64, :].rearrange("p (r f) -> p r f", f=64),
                                                                                          in0=psA[pb:pb + 64, 0:128].rearrange("p (r f) -> p r f", f=64), in1=mask, op=ALU.mult),
                         R=[psA], W=[at])
                    if c > 0:
                        P.op("dve", lambda e, st=st, c=c: e.tensor_scalar(out=st[:], in0=S2[:], scalar1=EAc[:, c, 0:1], scalar2=None, op0=ALU.mult), R=[S2, EAc], W=[st])
                    specs = []
                    for hh in range(2):
                        base = hh * 64
                        specs.append((pb, lambda e, hh=hh, at=at, pb=pb, c=c: e.matmul(
                            po[pb:pb + 64, hh * 64:(hh + 1) * 64], lhsT=at[pb:pb + 64, hh * 64:(hh + 1) * 64], rhs=vz[pb:pb + 64, hh * 64:(hh + 1) * 64],
                            start=True, stop=(c == 0))))
                        if c > 0:
                            specs.append((base, lambda e, hh=hh, base=base, st=st, pb=pb, cs_=cs_: e.matmul(
                                po[pb:pb + 64, hh * 64:(hh + 1) * 64], lhsT=qt_[base:base + 64, cs_], rhs=st[base:base + 64, :], start=False, stop=True)))
                    self.pe_seq(specs, R=[at, vz, qt_] + ([st] if c > 0 else []), W=[po])
                    if c < NC - 1:
                        for hh in range(2):
                            base = hh * 64
                            if c == 0:
                                P.op("dve", lambda e, base=base, hh=hh, pU=pU: e.tensor_copy(out=S2[base:base + 64, :], in_=pU[base:base + 64, hh * 64:(hh + 1) * 64]), R=[pU], W=[S2])
                            else:
                                P.op("dve", lambda e, base=base, hh=hh, pU=pU, c=c: e.scalar_tensor_tensor(
                                    out=S2[base:base + 64, :], in0=S2[base:base + 64, :], scalar=EAc[base:base + 64, c, 1:2],
                                    in1=pU[base:base + 64, hh * 64:(hh + 1) * 64], op0=ALU.mult, op1=ALU.add), R=[pU, S2, EAc], W=[S2])
                P.op("act", lambda e: e.activation(out=oc[:], in_=po[:, 0:128], func=AF.Copy), R=[po], W=[oc])
                P.op("dve", lambda e: e.tensor_tensor(out=osq[:], in0=oc[:], in1=oc[:], op=ALU.mult), R=[oc], W=[osq])
                P.op("dve", lambda e: e.tensor_reduce(out=sm[:, 0:2], in_=osq[:].rearrange("p (h d) -> p h d", h=2), axis=AX.X, op=ALU.add), R=[osq], W=[sm])
                P.op("dve", lambda e: e.tensor_scalar(out=sm[:, 2:4], in0=sm[:, 0:2], scalar1=1.0 / 64, scalar2=1e-6, op0=ALU.mult, op1=ALU.add), R=[sm], W=[sm])
                P.op("act", lambda e: e.activation(out=sm[:, 2:4], in_=sm[:, 2:4], func=AF.Sqrt), R=[sm], W=[sm])
                P.op("dve", lambda e: e.reciprocal(out=sm[:, 4:6], in_=sm[:, 2:4]), R=[sm], W=[sm])
                P.op("dve", lambda e: e.tensor_tensor(out=oc[:].rearrange("p (h d) -> p h d", h=2), in0=oc[:].rearrange("p (h d) -> p h d", h=2),
                                                      in1=sm[:, 4:6].unsqueeze(2).to_broadcast([128, 2, 64]), op=ALU.mult), R=[oc, sm], W=[oc])
                P.op("pool", lambda e: e.tensor_tensor(out=oc[:], in0=oc[:], in1=self.gbc[:, l, j * 128:(j + 1) * 128], op=ALU.mult), R=[oc], W=[oc])
                P.op("pool", lambda e: e.tensor_tensor(out=oc[:], in0=oc[:], in1=vz[:, 128:256], op=ALU.mult), R=[oc, vz], W=[oc])
                P.dma("sp", self.ybuf[n * 128:(n + 1) * 128, 512 + j * 128:512 + (j + 1) * 128], oc[:], R=[oc], W=[(self.ybuf, n * 4 + 2)])

    def mixer_D(self, b, l, wt):
        P, T, NT = self.P, self.T, self.NT
        g = 'D'
        SW = min(512, T)
        NCMP, NSL = self.NCMP, self.NSL
        DQ = [self.fmb[0], self.fmb[1]]
        KS, KW, KCV = self.fmb[2], self.fmb[3], self.sc[0]
        w1b = self.big[5]
        w1v = w1b[:, 0:2048].rearrange("p (j e) -> p j e", e=64)
        P.dma("sp", w1v, self.w1_d[l].rearrange("p (j e) -> p j e", e=64), R=[self.w1_d], W=[w1b])
        self.rope_block(wt, g, 'DQ0', 'DQ0s', DQ[0], l)
        self.rope_block(wt, g, 'DQ1', 'DQ1s', DQ[1], l)
        self.rope_block(wt, g, 'DKS', 'DKSs', KS, l)
        self.rope_block(wt, g, 'DKW', 'DKWs', KW, l)
        for s_ in range(T // SW):
            c0 = s_ * SW
            ps = self.ps[self.cnt % 2]; self.cnt += 1
            self.proj_fm(ps, wt, g, 'DKCV', c0, SW)
            P.op("act", lambda e, ps=ps, c0=c0: e.activation(out=KCV[:, c0:c0 + SW], in_=ps[:, 0:SW], func=AF.Identity,
                                                             bias=self.b_fm[:, l, FMI['DKCV']:FMI['DKCV'] + 1]), R=[ps], W=[KCV])
        pb_ = self.ps[5]
        specs = []
        for half in range(2):
            r = slice(half * 64, half * 64 + 64)
            for j in range(32):
                specs.append((half * 64, lambda e, r=r, j=j: e.matmul(pb_[r, 0:1], lhsT=w1v[r, j, :], rhs=self.peT[r, l, j:j + 1], start=(j == 0), stop=(j == 31))))
        self.pe_seq(specs, R=[w1b], W=[pb_])
        P.op("dve", lambda e: e.tensor_copy(out=self.cbias[:, 0:1], in_=pb_[:, 0:1]), R=[pb_], W=[self.cbias])
        pH = self.ps[6]
        span = 16 * (NCMP - 1) + 1
        specs = []
        for half in range(2):
            r = slice(half * 64, half * 64 + 64)
            for j in range(32):
                specs.append((half * 64, lambda e, r=r, j=j: e.matmul(pH[r, 0:NCMP], lhsT=w1v[r, j, :], rhs=KCV[r, j:j + span:16], start=(j == 0), stop=(j == 31))))
        self.pe_seq(specs, R=[w1b, KCV], W=[pH])
        h1, kcr, kct = self.h1, self.kcr, self.kct
        P.op("act", lambda e: e.activation(out=h1[:, 0:NCMP], in_=pH[:, 0:NCMP], func=AF.Silu, bias=self.cbias[:, 0:1]), R=[pH, self.cbias], W=[h1])
        pK = self.ps[7]
        self.pe_seq([
            (0, lambda e: e.matmul(pK[:, 0:NCMP], lhsT=self.w2s[0:64, l, 0:128], rhs=h1[0:64, 0:NCMP], start=True, stop=True)),
            (0, lambda e: e.matmul(pK[:, 128:128 + NCMP], lhsT=self.w2s[0:64, l, 128:256], rhs=h1[0:64, 0:NCMP], start=True, stop=True)),
            (64, lambda e: e.matmul(pK[0:NCMP, 256:320], lhsT=h1[64:128, 0:NCMP], rhs=self.w2s[64:128, l, 256:320], start=True, stop=True))], R=[h1], W=[pK])
        ce = self.cosE[:, 0:NCMP]
        se = self.sinE[:, 0:NCMP]
        P.op("dve", lambda e: e.tensor_tensor(out=kcr[:, 0:NCMP], in0=pK[:, 0:NCMP], in1=ce, op=ALU.mult), R=[pK], W=[kcr])
        P.op("dve", lambda e: e.tensor_tensor(out=kct[:, 0:NCMP], in0=pK[:, 128:128 + NCMP], in1=se, op=ALU.mult), R=[pK], W=[kct])
        P.op("dve", lambda e: e.tensor_tensor(out=kcr[:, 0:NCMP], in0=kcr[:, 0:NCMP], in1=kct[:, 0:NCMP], op=ALU.add), R=[kcr, kct], W=[kcr])
        P.op("act", lambda e: e.activation(out=self.vca[0:NCMP, 0:64], in_=pK[0:NCMP, 256:320], func=AF.Copy), R=[pK], W=[self.vca])
        tmo = 0
        self.load_w(l, g, 'tm')
        W97 = 65 + NSL
        for n in range(NT):
            i = n % 2
            qs_ = slice(n * 128, (n + 1) * 128)
            zt, pc, ocmp, imp, selb, gs, yd, yd2, sm = self.zt[i], self.pc[i], self.ocmp[i], self.imp[i], self.selb[i], self.gs[i], self.yd[i], self.yd2[i], self.smb[i]
            ptm = self.ps[0]
            self.proj_tm([ptm], wt, g, 'DTM', n)
            P.op("dve", lambda e: e.tensor_tensor(out=self.vaug[:, n, 0:2, 0:64], in0=ptm[:, 0:128].rearrange("p (h d) -> p h d", h=2),
                                                  in1=self.b_tm_s[:, tmo:tmo + 128].rearrange("p (h d) -> p h d", h=2), op=ALU.add),
                 R=[ptm, self.b_tm_s], W=[(self.vaug, n)])
            P.op("dve", lambda e: e.tensor_tensor(out=gs[:, 0:12], in0=ptm[:, 128:140], in1=self.b_tm_s[:, tmo + 128:tmo + 140], op=ALU.add), R=[ptm, self.b_tm_s], W=[gs])
            P.op("act", lambda e: e.activation(out=gs[:, 0:12], in_=gs[:, 0:12], func=AF.Sigmoid), R=[gs], W=[gs])
            P.op("dve", lambda e: e.tensor_tensor(out=zt[:, 0:256], in0=ptm[:, 140:396], in1=self.b_tm_s[:, tmo + 140:tmo + 396], op=ALU.add), R=[ptm, self.b_tm_s], W=[zt])
            P.op("act", lambda e: e.activation(out=zt[:, 0:256], in_=zt[:, 0:256], func=AF.Silu), R=[zt], W=[zt])
            mct = self.mct[i]
            P.dma("sp", mct[:], self.maskC_d[:, qs_], R=[self.maskC_d], W=[mct])
            pC = self.ps[1]
            specs = []
            for h in (0, 2, 1, 3):
                base = (h % 2) * 64
                specs.append((base, lambda e, h=h, base=base: e.matmul(pC[0:NCMP, h * 128:(h + 1) * 128], lhsT=kcr[base:base + 64, 0:NCMP],
                                                                     rhs=DQ[h // 2][base:base + 64, qs_], start=True, stop=True)))
            self.pe_seq(specs, R=[kcr, DQ[0], DQ[1]], W=[pC])
            P.op("act", lambda e: e.activation(out=pc[0:NCMP, :], in_=pC[0:NCMP, :], func=AF.Exp, scale=0.125), R=[pC], W=[pc])
            P.op("dve", lambda e: e.tensor_tensor(out=pc[0:NCMP, :].rearrange("p (h t) -> p h t", h=4), in0=pc[0:NCMP, :].rearrange("p (h t) -> p h t", h=4),
                                                  in1=mct[0:NCMP, :].unsqueeze(1).to_broadcast([NCMP, 4, 128]), op=ALU.mult), R=[pc, mct], W=[pc])
            pO = self.ps[2]
            def f(e):
                ins = None
                for h in range(4):
                    ins = e.matmul(pO[:, h * W97:(h + 1) * W97], lhsT=pc[0:NCMP, h * 128:(h + 1) * 128], rhs=self.vca[0:NCMP, 0:W97], start=True, stop=True)
                return ins
            self.pe_seq([(0, f)], R=[pc, self.vca], W=[pO])
            pOv = pO[:, 0:4 * W97].rearrange("p (h c) -> p h c", h=4)
            P.op("dve", lambda e: e.tensor_scalar(out=sm[:, 0:4], in0=pOv[:, :, 64], scalar1=1e-30, scalar2=None, op0=ALU.max), R=[pO], W=[sm])
            P.op("dve", lambda e: e.reciprocal(out=sm[:, 4:8], in_=sm[:, 0:4]), R=[sm], W=[sm])
            for h in range(4):
                if h == 0:
                    P.op("dve", lambda e: e.tensor_scalar(out=imp[:, 0:NSL], in0=pOv[:, 0, 65:W97], scalar1=sm[:, 4:5], scalar2=None, op0=ALU.mult), R=[pO, sm], W=[imp])
                else:
                    P.op("dve", lambda e, h=h: e.scalar_tensor_tensor(out=imp[:, 0:NSL], in0=pOv[:, h, 65:W97], scalar=sm[:, 4 + h:5 + h], in1=imp[:, 0:NSL],
                                                                      op0=ALU.mult, op1=ALU.add), R=[pO, sm, imp], W=[imp])
            P.op("dve", lambda e: e.tensor_tensor(out=imp[:, 0:NSL], in0=imp[:, 0:NSL], in1=self.ikeep[:, n, :], op=ALU.mult), R=[imp], W=[imp])
            P.op("dve", lambda e: e.tensor_tensor(out=imp[:, 0:NSL], in0=imp[:, 0:NSL], in1=self.iadd[:, n, :], op=ALU.add), R=[imp], W=[imp])
            P.op("dve", lambda e: e.max(out=selb[:, 0:8], in_=imp[:, 0:NSL]), R=[imp], W=[selb])
            P.op("dve", lambda e: e.tensor_reduce(out=selb[:, 8:9], in_=selb[:, 0:8], axis=AX.X, op=ALU.min), R=[selb], W=[selb])
            P.op("dve", lambda e: e.tensor_scalar(out=imp[:, 32:32 + NSL], in0=imp[:, 0:NSL], scalar1=selb[:, 8:9], scalar2=None, op0=ALU.is_ge), R=[imp, selb], W=[imp])
            gv = gs[:, 0:12].rearrange("p (h c) -> p h c", c=3)
            P.op("dve", lambda e: e.tensor_tensor(out=sm[:, 8:12], in0=sm[:, 4:8], in1=gv[:, :, 0], op=ALU.mult), R=[sm, gs], W=[sm])
            ydv = yd[:].rearrange("p (h d) -> p h d", h=4)
            yd2v = yd2[:].rearrange("p (h d) -> p h d", h=4)
            P.op("dve", lambda e: e.tensor_tensor(out=ydv, in0=pOv[:, :, 0:64], in1=sm[:, 8:12].unsqueeze(2).to_broadcast([128, 4, 64]), op=ALU.mult), R=[pO, sm], W=[yd])
            pOs = self.ps[6]
            selx = self.sc[1]
            nb_ = 2 * (n + 1)
            P.op("pool", lambda e: e.tensor_copy(out=selx[:, 0:nb_ * 64].rearrange("p (j s) -> p j s", s=64),
                                                 in_=imp[:, 32:32 + nb_].unsqueeze(2).to_broadcast([128, nb_, 64])), R=[imp], W=[selx])
            for kt in range(n + 1):
                ks_ = slice(kt * 128, (kt + 1) * 128)
                pS = self.ps[3 + (self.cnt % 2)]; pt = self.pt[self.cnt % 3]; self.cnt += 1
                pM = self.ps[5]
                specs = []
                for h in (0, 2, 1, 3):
                    base = (h % 2) * 64
                    specs.append((base, lambda e, h=h, base=base, pS=pS, ks_=ks_: e.matmul(pS[:, h * 128:(h + 1) * 128], lhsT=KS[base:base + 64, ks_],
                                                                                         rhs=DQ[h // 2][base:base + 64, qs_], start=True, stop=True)))
                self.pe_seq(specs, R=[KS, DQ[0], DQ[1]], W=[pS])
                P.op("pe", lambda e, kt=kt: e.matmul(pM[:, 0:128], lhsT=selx[:, kt * 128:(kt + 1) * 128], rhs=self.ident[:],
                                                     start=True, stop=True), R=[selx], W=[pM])
                P.op("act", lambda e, pS=pS, pt=pt: e.activation(out=pt[:], in_=pS[:], func=AF.Exp, scale=0.125), R=[pS], W=[pt])
                P.op("dve", lambda e, pt=pt: e.tensor_tensor(out=pt[:].rearrange("p (h t) -> p h t", h=4), in0=pt[:].rearrange("p (h t) -> p h t", h=4),
                                                             in1=pM[:, 0:128].unsqueeze(1).to_broadcast([128, 4, 128]), op=ALU.mult), R=[pt, pM], W=[pt])
                if kt == n:
                    P.op("pool", lambda e, pt=pt: e.tensor_tensor(out=pt[:], in0=pt[:], in1=self.tri_ge[:], op=ALU.mult), R=[pt], W=[pt])
                def f(e, pt=pt, kt=kt):
                    ins = None
                    for h in range(4):
                        ins = e.matmul(pOs[:, h * 65:(h + 1) * 65], lhsT=pt[:, h * 128:(h + 1) * 128], rhs=self.vaug[:, kt, 0, :], start=(kt == 0 and h == 0), stop=(kt == n and h == 3))
                    return ins
                P.op("pe", f, R=[pt, (self.vaug, kt)], W=[pOs])
            pOsv = pOs[:, 0:260].rearrange("p (h c) -> p h c", h=4)
            P.op("dve", lambda e: e.reciprocal(out=sm[:, 12:16], in_=pOsv[:, :, 64]), R=[pOs], W=[sm])
            P.op("dve", lambda e: e.tensor_tensor(out=sm[:, 12:16], in0=sm[:, 12:16], in1=gv[:, :, 1], op=ALU.mult), R=[sm, gs], W=[sm])
            P.op("dve", lambda e: e.tensor_tensor(out=yd2v, in0=pOsv[:, :, 0:64], in1=sm[:, 12:16].unsqueeze(2).to_broadcast([128, 4, 64]), op=ALU.mult), R=[pOs, sm], W=[yd2])
            P.op("pool", lambda e: e.tensor_tensor(out=yd[:], in0=yd[:], in1=yd2[:], op=ALU.add), R=[yd, yd2], W=[yd])
            pOw = self.ps[7]
            kts = [kt for kt in range(n - 4, n + 1) if kt >= 0]
            for kt in kts:
                ks_ = slice(kt * 128, (kt + 1) * 128)
                pS = self.ps[3 + (self.cnt % 2)]; pt = self.pt[self.cnt % 3]; self.cnt += 1
                specs = []
                for h in (0, 2, 1, 3):
                    base = (h % 2) * 64
                    specs.append((base, lambda e, h=h, base=base, pS=pS, ks_=ks_: e.matmul(pS[:, h * 128:(h + 1) * 128], lhsT=KW[base:base + 64, ks_],
                                                                                         rhs=DQ[h // 2][base:base + 64, qs_], start=True, stop=True)))
                self.pe_seq(specs, R=[KW, DQ[0], DQ[1]], W=[pS])
                P.op("act", lambda e, pS=pS, pt=pt: e.activation(out=pt[:], in_=pS[:], func=AF.Exp, scale=0.125), R=[pS], W=[pt])
                if kt == n:
                    P.op("pool", lambda e, pt=pt: e.tensor_tensor(out=pt[:], in0=pt[:], in1=self.tri_ge[:], op=ALU.mult), R=[pt], W=[pt])
                elif kt == n - 4:
                    P.op("pool", lambda e, pt=pt: e.tensor_tensor(out=pt[:], in0=pt[:], in1=self.tri_lt[:], op=ALU.mult), R=[pt], W=[pt])
                def f(e, pt=pt, kt=kt):
                    ins = None
                    for h in range(4):
                        ins = e.matmul(pOw[:, h * 65:(h + 1) * 65], lhsT=pt[:, h * 128:(h + 1) * 128], rhs=self.vaug[:, kt, 1, :], start=(kt == kts[0] and h == 0), stop=(kt == kts[-1] and h == 3))
                    return ins
                P.op("pe", f, R=[pt, (self.vaug, kt)], W=[pOw])
            pOwv = pOw[:, 0:260].rearrange("p (h c) -> p h c", h=4)
            P.op("dve", lambda e: e.reciprocal(out=sm[:, 16:20], in_=pOwv[:, :, 64]), R=[pOw], W=[sm])
            P.op("dve", lambda e: e.tensor_tensor(out=sm[:, 16:20], in0=sm[:, 16:20], in1=gv[:, :, 2], op=ALU.mult), R=[sm, gs], W=[sm])
            P.op("dve", lambda e: e.tensor_tensor(out=yd2v, in0=pOwv[:, :, 0:64], in1=sm[:, 16:20].unsqueeze(2).to_broadcast([128, 4, 64]), op=ALU.mult), R=[pOw, sm], W=[yd2])
            P.op("pool", lambda e: e.tensor_tensor(out=yd[:], in0=yd[:], in1=yd2[:], op=ALU.add), R=[yd, yd2], W=[yd])
            P.op("pool", lambda e: e.tensor_tensor(out=yd[:], in0=yd[:], in1=zt[:, 0:256], op=ALU.mult), R=[yd, zt], W=[yd])
            self.emit_y(yd, n, 6)

    def layer(self, b, l):
        P, T, NT = self.P, self.T, self.NT
        P.epoch(b * self.NL + l)
        for g in GL:
            if g in self.mixers:
                w = self.load_w(l, g, 'fm')
                getattr(self, "mixer_" + g)(b, l, w)
            else:
                gi = GL.index(g)
                for n in range(NT):
                    P.dma("sp", self.ybuf[n * 128:(n + 1) * 128, gi * 256:(gi + 1) * 256], self.zero256[:], W=[(self.ybuf, n * 4 + gi)])
        self.outproj(b, l)

    def outproj(self, b, l):
        P, T, NT = self.P, self.T, self.NT
        wt = self.wt[0]
        P.dma("sp", wt[:, :, 0:D], self.wout_bf[l].rearrange("(mc p) c -> p mc c", p=128), R=[(self.wout_bf, l)], W=[wt])
        P.dma("sp", self.lnbc[:, 0:2 * D].rearrange("p (a d) -> p a d", a=2), self.lng_d[l].partition_broadcast(128), R=[self.lng_d], W=[self.lnbc])
        last = (l == self.NL - 1)
        for n in range(NT):
            i = n % 2
            yt, xt = self.yt[i], self.xt[i]
            P.dma("sp", yt[:, 0:D], self.ybuf[n * 128:(n + 1) * 128, :], R=[(self.ybuf, range(n * 4, n * 4 + 4))], W=[yt])
            P.dma("sp", xt[:, 0:D], self.hbuf[n * 128:(n + 1) * 128, :], R=[(self.hbuf, n)], W=[xt])
            yTt = self.yTt[i]
            for half in range(2):
                ps = self.ps[half]
                def f(e, half=half, ps=ps):
                    ins = None
                    for j in range(4):
                        mc = half * 4 + j
                        ins = e.transpose(out=ps[:, j * 128:(j + 1) * 128], in_=yt[:, mc * 128:(mc + 1) * 128], identity=self.ident[:])
                    return ins
                P.op("pe", f, R=[yt], W=[ps])
                P.op("act", lambda e, half=half, ps=ps: e.activation(out=yTt[:, half * 4:(half + 1) * 4, :], in_=ps[:].rearrange("p (j t) -> p j t", j=4), func=AF.Copy),
                     R=[ps], W=[yTt])
            for half in range(2):
                ps = self.ps[2 + half]
                def f(e, half=half, ps=ps):
                    ins = None
                    for mc in range(8):
                        ins = e.matmul(ps[:], lhsT=yTt[:, mc, :], rhs=wt[:, mc, half * 512:(half + 1) * 512], start=(mc == 0), stop=(mc == 7))
                    return ins
                P.op("pe", f, R=[yTt, wt], W=[ps])
                P.op("dve", lambda e, half=half, ps=ps: e.scalar_tensor_tensor(out=xt[:, half * 512:(half + 1) * 512], in0=xt[:, half * 512:(half + 1) * 512],
                                                                              scalar=DN_ALPHA, in1=ps[:], op0=ALU.mult, op1=ALU.add), R=[xt, ps], W=[xt])
            self.layernorm(xt, i)
            if last:
                tok = P.dma("sp", self.out[b, n * 128:(n + 1) * 128, :], xt[:, 0:D], R=[xt], W=[self.out])
                self.out_toks.append(tok)
            else:
                P.dma("sp", self.hbuf[n * 128:(n + 1) * 128, :], xt[:, 0:D], R=[xt], W=[(self.hbuf, n)])
                self.to_hT(xt, n)


_CACHE = {}


def kernel(x, ln0_g, ln0_b, w_in, b_in, a_sinks, b_conv_w, b_conv_b, c_lb, c_norm_g, d_cmp_pe, d_cmp_w1, d_cmp_w2, w_out, ln_g, ln_b):
    from concourse.bass_utils import run_bass_kernel_spmd
    f32 = np.float32
    full = dict(x=np.asarray(x, f32), ln0_g=np.asarray(ln0_g, f32), ln0_b=np.asarray(ln0_b, f32), w_in=np.asarray(w_in, f32), b_in=np.asarray(b_in, f32),
                a_sinks=np.asarray(a_sinks, f32), b_conv_w=np.asarray(b_conv_w, f32), b_conv_b=np.asarray(b_conv_b, f32), c_lb=np.asarray(c_lb, f32),
                c_norm_g=np.asarray(c_norm_g, f32), d_cmp_pe=np.asarray(d_cmp_pe, f32), d_cmp_w1=np.asarray(d_cmp_w1, f32), d_cmp_w2=np.asarray(d_cmp_w2, f32),
                w_out=np.asarray(w_out, f32), ln_g=np.asarray(ln_g, f32), ln_b=np.asarray(ln_b, f32))
    B, T, _ = full['x'].shape
    NCORE = 8
    NB = B // NCORE
    NL = full['w_in'].shape[0]
    hc = host_consts(T)
    w_r, b_fm, b_tm, b_if = host_weights(full['w_in'], full['b_in'])
    common = dict(ln0=np.stack([full['ln0_g'], full['ln0_b']]), lng=np.stack([full['ln_g'], full['ln_b']], 1),
                  w_in=w_r, b_fm=b_fm, b_tm=b_tm, b_if=b_if, a_sinks=full['a_sinks'], w_out=full['w_out'], **hc)
    common.update(extra_inputs(full, T))
    k = K(T=T, NB=NB, NL=NL)
    k.build()
    common = {n: np.ascontiguousarray(common[n]) for n in k.in_names if n != 'x'}
    in_maps = []
    for c in range(NCORE):
        m = dict(common)
        m['x'] = np.ascontiguousarray(full['x'][c * NB:(c + 1) * NB])
        in_maps.append(m)
    res = run_bass_kernel_spmd(k.nc, in_maps, core_ids=list(range(NCORE)))
    out = np.concatenate([np.asarray(r['out']) for r in res.results], axis=0)
    return out.astype(np.float32)
```

```python
import contextlib
import numpy as np
import concourse.bass as bass
import concourse.mybir as mybir

F32 = mybir.dt.float32
BF16 = mybir.dt.bfloat16
AF = mybir.ActivationFunctionType
ALU = mybir.AluOpType
AX = mybir.AxisListType


class Buf:
    def __init__(self, name, t, slots=1):
        self.name = name
        self.t = t
        self.slots = slots

    def keys(self, slot=None):
        if slot is None:
            return [(self.name, s) for s in range(self.slots)]
        if isinstance(slot, (list, tuple, range)):
            return [(self.name, s) for s in slot]
        return [(self.name, slot)]

    def __getitem__(self, idx):
        return self.t[idx]


class Prog:
    NDMA = 4

    def __init__(self, nc, nepoch=1):
        self.nc = nc
        self.es = contextlib.ExitStack()
        self.eng = {"pe": nc.tensor, "act": nc.scalar, "dve": nc.vector, "pool": nc.gpsimd, "sp": nc.sync}
        self.esem = {}
        self.ecnt = {}
        self.seen = {}
        self.last_tok = {}
        self.finals = []
        self.esem_sets = []
        self.dsem_sets = []
        for ep in range(nepoch):
            self.esem_sets.append({k: self.es.enter_context(nc.semaphore("sem_%s_%d" % (k, ep))) for k in ("pe", "act", "dve", "pool")})
            self.dsem_sets.append([self.es.enter_context(nc.semaphore("dsem_sp%d_%d" % (i, ep))) for i in range(self.NDMA)])
        for k in self.eng:
            self.ecnt[k] = 0
            self.seen[k] = {}
            self.last_tok[k] = None
        self.esem = dict(self.esem_sets[0])
        self.dsem = {"sp": self.dsem_sets[0]}
        self.dval = {"sp": [0] * self.NDMA}
        self.dnext = {"sp": 0}
        self.dsem["pool"] = [self.es.enter_context(nc.semaphore("dsem_pool%d" % i)) for i in range(self.NDMA)]
        self.dval["pool"] = [0] * self.NDMA
        self.dnext["pool"] = 0
        self.cur_epoch = 0
        self.state = {}
        self.nbuf = 0
        self.excl = set()
        self.stop_ops = None
        self.ninstr = 0

    def epoch(self, ep):
        if ep == self.cur_epoch:
            return
        for k in ("pe", "act", "dve", "pool"):
            if self.ecnt[k] > 0:
                self.finals.append((self.esem[k], self.ecnt[k], k))
            self.esem[k] = self.esem_sets[ep][k]
            self.ecnt[k] = 0
        for j in range(self.NDMA):
            if self.dval["sp"][j] > 0:
                self.finals.append((self.dsem["sp"][j], self.dval["sp"][j], "dma"))
        self.dsem["sp"] = self.dsem_sets[ep]
        self.dval["sp"] = [0] * self.NDMA
        self.dnext["sp"] = 0
        self.cur_epoch = ep

    def sbuf(self, name, shape, dtype=F32, slots=1):
        t = self.es.enter_context(self.nc.sbuf_tensor(name, list(shape), dtype))
        return Buf(name, t, slots)

    def psum(self, name, shape, dtype=F32, slots=1):
        t = self.es.enter_context(self.nc.psum_tensor(name, list(shape), dtype))
        self.excl.add(name)
        return Buf(name, t, slots)

    def dram(self, name, shape, dtype=F32, kind="Internal", slots=1):
        t = self.nc.dram_tensor(name, list(shape), dtype, kind=kind)
        return Buf(name, t.ap(), slots)

    def _keys(self, lst):
        out = []
        for x in lst:
            if isinstance(x, Buf):
                out += x.keys()
            elif isinstance(x, tuple) and isinstance(x[0], Buf):
                out += x[0].keys(x[1])
            else:
                raise TypeError(x)
        return out

    def _wait(self, e, tok):
        sem, val, owner = tok
        sid = id(sem)
        if self.seen[e].get(sid, 0) >= val:
            return
        self.eng[e].wait_ge(sem, val)
        self.seen[e][sid] = val

    def _deps(self, e, R, W):
        toks = []
        for k in R:
            st = self.state.get(k)
            if st and st[0] is not None:
                toks.append(st[0])
        for k in W:
            st = self.state.get(k)
            if st:
                if st[0] is not None and (st[0][2] != e or e != 'pe'):
                    toks.append(st[0])
                for re_, tok in st[1].items():
                    if re_ != e or e != 'pe':
                        toks.append(tok)
        for tok in toks:
            self._wait(e, tok)

    def _commit(self, e, tok, R, W):
        for k in R:
            st = self.state.setdefault(k, [None, {}])
            st[1][e] = tok
        for k in W:
            self.state[k] = [tok, {}]

    def op(self, e, fn, R=(), W=(), fence=False):
        Rk, Wk = self._keys(R), self._keys(W)
        Wk = Wk + [k_ for k_ in Rk if k_[0] in self.excl]
        Rk = [k_ for k_ in Rk if k_[0] not in self.excl]
        self._deps(e, Rk, Wk)
        if fence and self.last_tok[e] is not None:
            self._wait(e, self.last_tok[e])
        if self.stop_ops is not None and self.ninstr >= self.stop_ops:
            raise StopIteration("stop_ops")
        ins = fn(self.eng[e])
        self.ecnt[e] += 1
        ins.then_inc(self.esem[e], 1)
        tok = (self.esem[e], self.ecnt[e], e)
        self.last_tok[e] = tok
        self._commit(e, tok, Rk, Wk)
        self.ninstr += 1
        return tok

    def dma(self, q, out, in_, R=(), W=(), **kw):
        Rk, Wk = self._keys(R), self._keys(W)
        self._deps(q, Rk, Wk)
        j = self.dnext[q]
        self.dnext[q] = (j + 1) % self.NDMA
        sem = self.dsem[q][j]
        if self.dval[q][j] > 0:
            self._wait(q, (sem, self.dval[q][j], "dma"))
        ins = self.eng[q].dma_start(out=out, in_=in_, **kw)
        self.dval[q][j] += 16
        ins.then_inc(sem, 16)
        tok = (sem, self.dval[q][j], "dma_" + q)
        self._commit("dma_" + q, tok, Rk, Wk)
        self.ninstr += 1
        return tok

    def wait_tok(self, e, tok):
        self._wait(e, tok)

    def finish(self, toks):
        for tok in toks:
            self._wait("sp", tok)
        for tok in self.finals:
            self._wait("sp", tok)
        for e in ("pe", "act", "dve", "pool"):
            if self.ecnt[e] > 0:
                self._wait("sp", (self.esem[e], self.ecnt[e], e))
        for q in ("sp", "pool"):
            for j in range(self.NDMA):
                if self.dval[q][j] > 0:
                    self._wait("sp", (self.dsem[q][j], self.dval[q][j], "dma"))


import numpy as np
import concourse.bass as bass
import concourse.mybir as mybir

D = 1024
HD = 64
LN_EPS = 1e-5
DN_ALPHA = (2.0 * 2) ** 0.25

OFF = {}
_o = 0
for _n, _s in (('a_q', 256), ('a_k', 128), ('a_v', 128), ('a_z', 256), ('b_qk', 512), ('b_v', 256), ('b_if', 8),
               ('b_o', 256), ('b_z', 256), ('c_q', 256), ('c_f', 256), ('c_i', 256), ('c_z', 256), ('d_q', 256),
               ('d_kv', 384), ('d_g', 12), ('d_z', 256)):
    OFF[_n] = _o
    _o += _s
NCOLS = _o


def _head(base, h):
    return np.arange(base + h * 64, base + (h + 1) * 64)


def _swap(c):
    return np.concatenate([c[32:], c[:32]])


def col_layout():
    L = {}
    idx = []

    def add(name, cols):
        L[name] = (sum(len(x) for x in idx), len(cols))
        idx.append(np.asarray(cols))

    aq, ak = OFF['a_q'], OFF['a_k']
    c = np.concatenate([_head(aq, 0), _head(aq, 2)]); add('AQ0', c); add('AQ0s', np.concatenate([_swap(_head(aq, 0)), _swap(_head(aq, 2))]))
    add('AQ1', np.concatenate([_head(aq, 1), _head(aq, 3)])); add('AQ1s', np.concatenate([_swap(_head(aq, 1)), _swap(_head(aq, 3))]))
    add('AK', np.concatenate([_head(ak, 0), _head(ak, 1)])); add('AKs', np.concatenate([_swap(_head(ak, 0)), _swap(_head(ak, 1))]))
    add('ATM', np.concatenate([np.arange(OFF['a_v'], OFF['a_v'] + 128), np.arange(OFF['a_z'], OFF['a_z'] + 256)]))
    bq = OFF['b_qk']
    add('BQ0', np.arange(bq, bq + 128)); add('BQ1', np.arange(bq + 128, bq + 256))
    add('BK0', np.arange(bq + 256, bq + 384)); add('BK1', np.arange(bq + 384, bq + 512))
    add('BI', np.arange(OFF['b_if'], OFF['b_if'] + 4)); add('BF', np.arange(OFF['b_if'] + 4, OFF['b_if'] + 8))
    add('BTM', np.concatenate([np.arange(OFF['b_v'], OFF['b_v'] + 256), np.arange(OFF['b_o'], OFF['b_o'] + 256),
                               np.arange(OFF['b_z'], OFF['b_z'] + 256)]))
    add('CQ0', np.arange(OFF['c_q'], OFF['c_q'] + 128)); add('CQ1', np.arange(OFF['c_q'] + 128, OFF['c_q'] + 256))
    add('CF0', np.arange(OFF['c_f'], OFF['c_f'] + 128)); add('CF1', np.arange(OFF['c_f'] + 128, OFF['c_f'] + 256))
    add('CTM', np.concatenate([np.arange(OFF['c_i'], OFF['c_i'] + 256), np.arange(OFF['c_z'], OFF['c_z'] + 256)]))
    dq, dk = OFF['d_q'], OFF['d_kv']
    add('DQ0', _head(dq, 0)); add('DQ0s', _swap(_head(dq, 0)))
    for h in range(1, 4):
        add('DH%d' % h, _head(dq, h)); add('DH%ds' % h, _swap(_head(dq, h)))
    add('DKCV', np.concatenate([_head(dk, 0), _head(dk, 1)]))
    add('DKS', _head(dk, 2)); add('DKSs', _swap(_head(dk, 2)))
    add('DKW', _head(dk, 4)); add('DKWs', _swap(_head(dk, 4)))
    add('DTM', np.concatenate([_head(dk, 3), _head(dk, 5), np.arange(OFF['d_g'], OFF['d_g'] + 12),
                               np.arange(OFF['d_z'], OFF['d_z'] + 256)]))
    return L, np.concatenate(idx)


LAY, GIDX = col_layout()
NCR = len(GIDX)
GROUPS = {
    'A': ('AQ0', 'ATM'),
    'B': ('BQ0', 'BTM'),
    'C': ('CQ0', 'CTM'),
    'D': ('DQ0', 'DTM'),
}


def grange(g):
    a, b = GROUPS[g]
    s = LAY[a][0]
    e = LAY[b][0] + LAY[b][1]
    return s, e


FM = ['AQ0', 'AQ0s', 'AQ1', 'AQ1s', 'AK', 'AKs', 'BQ0', 'BQ1', 'BK0', 'BK1', 'CQ0', 'CQ1', 'CF0', 'CF1',
      'DQ0', 'DQ0s', 'DH1', 'DH1s', 'DH2', 'DH2s', 'DH3', 'DH3s', 'DKCV', 'DKS', 'DKSs', 'DKW', 'DKWs']
FMI = {n: i for i, n in enumerate(FM)}
TM = ['ATM', 'BTM', 'CTM', 'DTM']
TMO = {}
_o = 0
for _n in TM:
    TMO[_n] = _o
    _o += LAY[_n][1]
NTM = _o
GL = ['A', 'B', 'C', 'D']
WMAX = max(grange(g)[1] - grange(g)[0] for g in GL)


def host_weights(w_in, b_in):
    w_r = np.ascontiguousarray(w_in[:, :, GIDX])
    b_r = b_in[:, GIDX]
    NL = w_in.shape[0]
    b_fm = np.zeros((NL, 128, len(FM)), np.float32)
    for i, n in enumerate(FM):
        s0, c = LAY[n]
        b_fm[:, :c, i] = b_r[:, s0:s0 + c]
    b_tm = np.concatenate([b_r[:, LAY[n][0]:LAY[n][0] + LAY[n][1]] for n in TM], axis=1)
    b_if = np.stack([b_r[:, LAY['BI'][0]:LAY['BI'][0] + 4], b_r[:, LAY['BF'][0]:LAY['BF'][0] + 4]], axis=-1)
    return w_r, np.ascontiguousarray(b_fm), np.ascontiguousarray(b_tm), np.ascontiguousarray(b_if)


def host_consts(T):
    half = 32
    inv = (10000.0 ** (-np.arange(half, dtype=np.float32) / half)).astype(np.float32)
    pos = np.arange(T, dtype=np.float32)
    ang = pos[None, :] * inv[:, None]
    cos = np.cos(ang).astype(np.float32)
    sin = np.sin(ang).astype(np.float32)
    cos64 = np.concatenate([cos, cos], 0)
    sin64 = np.concatenate([-sin, sin], 0)
    cosT = np.concatenate([cos64, cos64], 0)
    sinT = np.concatenate([sin64, sin64], 0)
    ident = np.eye(128, dtype=np.float32)
    p = np.arange(128)[:, None]
    f = np.arange(128)[None, :]
    tri_ge = (f >= p).astype(np.float32)
    tri_lt = (f < p).astype(np.float32)
    tri_ge = np.tile(tri_ge, (1, 4)); tri_lt = np.tile(tri_lt, (1, 4))
    NCMP = (T - 32) // 16 + 1
    cosE = np.zeros((128, 128), np.float32); sinE = np.zeros((128, 128), np.float32)
    cosE[:, :NCMP] = cosT[:, 31::16][:, :NCMP]; sinE[:, :NCMP] = sinT[:, 31::16][:, :NCMP]
    return dict(cosT=cosT, sinT=sinT, ident=ident, tri_ge=tri_ge, tri_lt=tri_lt, cosE=cosE, sinE=sinE)


def extra_inputs(full, T):
    NL = full['w_in'].shape[0]
    cw = full['b_conv_w'].reshape(NL, 4, 4, 128).transpose(0, 3, 2, 1)
    cb = full['b_conv_b'].reshape(NL, 4, 128).transpose(0, 2, 1)
    sel = np.zeros((4, 4, 128), np.float32)
    for h in range(4):
        sel[h, h, :] = 1.0
    clb = full['c_lb'].reshape(NL, 2, 128).transpose(2, 1, 0)
    rmask = np.ones((128, T), np.float32); rmask[:, ::64] = 0.0
    NCMP = (T - 32) // 16 + 1
    NSL = T // 64
    NT = T // 128
    w1 = full['d_cmp_w1'].reshape(NL, 2, 32, 64, 64).transpose(0, 1, 3, 2, 4).reshape(NL, 128, 32 * 64)
    w2 = full['d_cmp_w2']
    w2s = np.zeros((NL, 128, 320), np.float32)
    sw = np.concatenate([np.arange(32, 64), np.arange(0, 32)])
    w2s[:, 0:64, 0:64] = w2[:, 0]; w2s[:, 0:64, 64:128] = w2[:, 0]
    w2s[:, 0:64, 128:192] = w2[:, 0][:, :, sw]; w2s[:, 0:64, 192:256] = w2[:, 0][:, :, sw]
    w2s[:, 64:128, 256:320] = w2[:, 1]
    peT = full['d_cmp_pe'].transpose(0, 1, 3, 2).reshape(NL, 128, 32)
    starts = np.arange(NCMP) * 16
    blk = np.arange(NSL)
    overlap = ((starts[:, None] < (blk[None, :] + 1) * 64) & (starts[:, None] + 32 > blk[None, :] * 64)).astype(np.float32)
    vca0 = np.zeros((128, 65 + NSL), np.float32)
    vca0[:NCMP, 64] = 1.0
    vca0[:NCMP, 65:] = overlap
    t = np.arange(T)
    maskC = np.zeros((128, T), np.float32)
    maskC[:NCMP] = ((starts + 31)[:, None] <= t[None, :]).astype(np.float32)
    cur = (t // 64)[:, None]
    forced = (blk[None, :] == 0) | (blk[None, :] == cur) | (blk[None, :] == cur - 1)
    valid = blk[None, :] <= cur
    keep = (valid & ~forced).astype(np.float32)
    add = np.where(valid, np.where(forced, 1e6, 0.0), -1e30).astype(np.float32)
    keep = keep.reshape(NT, 128, NSL).transpose(1, 0, 2)
    add = add.reshape(NT, 128, NSL).transpose(1, 0, 2)
    return dict(cw=np.ascontiguousarray(cw), cb=np.ascontiguousarray(cb), sel=sel, clb=np.ascontiguousarray(clb), rmask=rmask,
                c_norm_g=full['c_norm_g'], w1s=np.ascontiguousarray(w1), w2s=w2s, peT=np.ascontiguousarray(peT), vca0=vca0, maskC=maskC,
                ikeep=np.ascontiguousarray(keep), iadd=np.ascontiguousarray(add))


class StopBuild(Exception):
    pass


class K:
    stop = None

    def chk(self, tag):
        if self.stop == tag:
            raise StopBuild(tag)


    def __init__(self, T=2048, NB=4, NL=2, dbg=(), mixers="ABCD"):
        self.mixers = mixers
        self.T, self.NB, self.NL = T, NB, NL
        self.NT = T // 128
        self.dbg = set(dbg)
        self.dbg_out = {}
        nc = bass.Bass("TRN2", target_bir_lowering=False)
        self.nc = nc
        self.P = Prog(nc, nepoch=NB * NL)
        self.out_toks = []
        self.in_names = []
        self.last_kb = None
        self.ctoks = []

    def const_barrier(self):
        for e in ("pe", "act", "dve", "pool"):
            for tok in self.ctoks:
                self.P.wait_tok(e, tok)
        self.ctoks = []

    def pe_seq(self, specs, R, W):
        P = self.P
        groups = []
        for kb, fn in specs:
            if groups and (kb is None or groups[-1][0] is None or groups[-1][0] == kb):
                if groups[-1][0] is None:
                    groups[-1][0] = kb
                groups[-1][1].append(fn)
            else:
                groups.append([kb, [fn]])
        for gi, (kb, fns) in enumerate(groups):
            def f(e, fns=fns):
                ins = None
                for fn in fns:
                    ins = fn(e)
                return ins
            fence = (kb is not None and self.last_kb is not None and kb != self.last_kb)
            P.op("pe", f, R=R, W=W, fence=fence)
            if kb is not None:
                self.last_kb = kb

    def din(self, name, shape, dtype=F32):
        self.in_names.append(name)
        return self.P.dram(name, shape, dtype, kind="ExternalInput")

    def dump(self, name, buf, ap, shape, R=None):
        if name not in self.dbg:
            return
        P = self.P
        d = P.dram("dbg_" + name, shape, F32, kind="ExternalOutput")
        tok = P.dma("pool", d[:], ap, R=[buf] if R is None else R, W=[d])
        self.out_toks.append(tok)

    def build(self):
        P, nc, T, NT = self.P, self.nc, self.T, self.NT
        NB, NL = self.NB, self.NL
        x = self.din("x", [NB, T, D])
        out = P.dram("out", [NB, T, D], F32, kind="ExternalOutput")
        w_in = self.din("w_in", [NL, D, NCR])
        ln0 = self.din("ln0", [2, D])
        lng = self.din("lng", [NL, 2, D])
        cosT_d = self.din("cosT", [128, T]); sinT_d = self.din("sinT", [128, T])
        ident_d = self.din("ident", [128, 128]); trige_d = self.din("tri_ge", [128, 512]); trilt_d = self.din("tri_lt", [128, 512])
        b_fm_d = self.din("b_fm", [NL, 128, len(FM)]); b_tm_d = self.din("b_tm", [NL, NTM]); b_if_d = self.din("b_if", [NL, 4, 2])
        self.w_bf = P.dram("w_bf", [NL, D, NCR], BF16, slots=NL * 4)
        sinks_d = self.din("a_sinks", [NL, 4])
        hbuf = P.dram("hbuf", [T, D], F32, slots=NT)
        self.x, self.out, self.w_in, self.hbuf = x, out, w_in, hbuf
        self.sinks_d = sinks_d
        self.cosT_d, self.sinT_d = cosT_d, sinT_d
        cosE_d = self.din("cosE", [128, 128]); sinE_d = self.din("sinE", [128, 128])
        self.cosE = P.sbuf("cosE_s", [128, 128]); self.sinE = P.sbuf("sinE_s", [128, 128])
        self.cst = [P.sbuf("cst%d" % i, [128, 2, 512]) for i in range(2)]
        self.ident = P.sbuf("ident_s", [128, 128]); self.tri_ge = P.sbuf("trige_s", [128, 512]); self.tri_lt = P.sbuf("trilt_s", [128, 512])
        for sb, dr in ((self.cosE, cosE_d), (self.sinE, sinE_d), (self.ident, ident_d), (self.tri_ge, trige_d), (self.tri_lt, trilt_d)):
            self.ctoks.append(P.dma("sp", sb[:], dr[:], R=[dr], W=[sb]))
        self.ln0_d = ln0; self.lng_d = lng
        self.b_fm = P.sbuf("b_fm_s", [128, NL, len(FM)])
        self.ctoks.append(P.dma("sp", self.b_fm[:], b_fm_d[:].rearrange("l p f -> p l f"), R=[b_fm_d], W=[self.b_fm]))
        self.b_tm_d = b_tm_d
        self.b_tm_s = P.sbuf("b_tm_s", [128, 768])
        self.b_if = P.sbuf("b_if_s", [4, NL, 2])
        self.ctoks.append(P.dma("sp", self.b_if[:], b_if_d[:].rearrange("l p f -> p l f"), R=[b_if_d], W=[self.b_if]))
        cw_d = self.din("cw", [NL, 128, 4, 4]); cb_d = self.din("cb", [NL, 128, 4]); sel_d = self.din("sel", [4, 4, 128])
        self.cw = P.sbuf("cw_s", [128, NL, 4, 4]); self.cb = P.sbuf("cb_s", [128, NL, 4]); self.sel = P.sbuf("sel_s", [4, 4, 128])
        self.ctoks.append(P.dma("sp", self.cw[:], cw_d[:].rearrange("l p j t -> p l j t"), R=[cw_d], W=[self.cw]))
        self.ctoks.append(P.dma("sp", self.cb[:], cb_d[:].rearrange("l p j -> p l j"), R=[cb_d], W=[self.cb]))
        self.ctoks.append(P.dma("sp", self.sel[:], sel_d[:], R=[sel_d], W=[self.sel]))
        clb_d = self.din("clb", [128, 2, NL]); cng_d = self.din("c_norm_g", [NL, 256])
        self.clb = P.sbuf("clb_s", [128, 2, NL]); self.gbc = P.sbuf("gbc", [128, NL, 256])
        self.ctoks.append(P.dma("sp", self.clb[:], clb_d[:], R=[clb_d], W=[self.clb]))
        self.ctoks.append(P.dma("sp", self.gbc[:], cng_d[:].partition_broadcast(128), R=[cng_d], W=[self.gbc]))
        self.NCMP = (T - 32) // 16 + 1
        self.NSL = T // 64
        NSL = self.NSL
        self.w1_d = self.din("w1s", [NL, 128, 2048]); w2_d = self.din("w2s", [NL, 128, 320]); pe_d = self.din("peT", [NL, 128, 32])
        vca_d = self.din("vca0", [128, 65 + NSL]); mc_d = self.din("maskC", [128, T]); ik_d = self.din("ikeep", [128, NT, NSL]); ia_d = self.din("iadd", [128, NT, NSL])
        self.w2s = P.sbuf("w2s_s", [128, NL, 320]); self.peT = P.sbuf("peT_s", [128, NL, 32]); self.vca = P.sbuf("vca", [128, 65 + NSL])
        self.maskC_d = mc_d; self.mct = [P.sbuf("mct%d" % i, [128, 128]) for i in range(2)]; self.ikeep = P.sbuf("ikeep_s", [128, NT, NSL]); self.iadd = P.sbuf("iadd_s", [128, NT, NSL])
        self.ctoks.append(P.dma("sp", self.w2s[:], w2_d[:].rearrange("l p f -> p l f"), R=[w2_d], W=[self.w2s]))
        self.ctoks.append(P.dma("sp", self.peT[:], pe_d[:].rearrange("l p f -> p l f"), R=[pe_d], W=[self.peT]))
        for sb, dr in ((self.vca, vca_d), (self.ikeep, ik_d), (self.iadd, ia_d)):
            self.ctoks.append(P.dma("sp", sb[:], dr[:], R=[dr], W=[sb]))
        self.esink = P.sbuf("esink", [128, NL, 4])
        self.ctoks.append(P.dma("sp", self.esink[:], sinks_d[:].partition_broadcast(128), R=[sinks_d], W=[self.esink]))
        for l in range(NL):
            for gi, g in enumerate(GL):
                s0, e0 = grange(g)
                P.dma("pool", self.w_bf[l, :, s0:e0], w_in[l, :, s0:e0], R=[w_in], W=[(self.w_bf, l * 4 + gi)])
        self.wt = [P.sbuf("wt%d" % i, [128, 8, 1152], BF16) for i in range(1)]
        wout = self.din("w_out", [NL, D, D])
        self.wout_bf = P.dram("wout_bf", [NL, D, D], BF16, slots=NL)
        for l in range(NL):
            P.dma("pool", self.wout_bf[l], wout[l], R=[wout], W=[(self.wout_bf, l)])
        self.wsel = 0
        self.hT = P.sbuf("hT", [128, 8, T], BF16, slots=NT)
        self.ybuf = P.dram("ybuf", [T, D], F32, slots=NT * 4)
        self.ps = [P.psum("ps%d" % i, [128, 512]) for i in range(8)]
        self.stat = [P.sbuf("stat%d" % i, [128, 2, 6]) for i in range(2)]
        self.mv = [P.sbuf("mv%d" % i, [128, 4]) for i in range(2)]
        self.const_barrier()
        try:
            for b in range(NB):
                self.seq(b)
        except (StopBuild, StopIteration):
            pass
        P.finish(self.out_toks)

    def layernorm(self, xt, i):
        P = self.P
        st, mv = self.stat[i], self.mv[i]
        xv = xt[:, 0:D]
        for c in range(2):
            P.op("dve", lambda e, c=c: e.bn_stats(out=st[:, c, :], in_=xt[:, c * 512:(c + 1) * 512]), R=[xt], W=[st])
        P.op("dve", lambda e: e.bn_aggr(out=mv[:, 0:2], in_=st[:]), R=[st], W=[mv])
        P.op("dve", lambda e: e.tensor_scalar(out=mv[:, 2:3], in0=mv[:, 1:2], scalar1=LN_EPS, scalar2=None, op0=ALU.add), R=[mv], W=[mv])
        P.op("act", lambda e: e.activation(out=mv[:, 2:3], in_=mv[:, 2:3], func=AF.Sqrt), R=[mv], W=[mv])
        P.op("dve", lambda e: e.reciprocal(out=mv[:, 3:4], in_=mv[:, 2:3]), R=[mv], W=[mv])
        P.op("dve", lambda e: e.tensor_scalar(out=xv, in0=xv, scalar1=mv[:, 0:1], scalar2=mv[:, 3:4], op0=ALU.subtract, op1=ALU.mult), R=[xt, mv], W=[xt])
        P.op("pool", lambda e: e.tensor_tensor(out=xv, in0=xv, in1=self.lnbc[:, 0:D], op=ALU.mult), R=[xt, self.lnbc], W=[xt])
        P.op("pool", lambda e: e.tensor_tensor(out=xv, in0=xv, in1=self.lnbc[:, D:2 * D], op=ALU.add), R=[xt, self.lnbc], W=[xt])

    def to_hT(self, xt, n):
        P = self.P
        for half in range(2):
            ps = self.ps[half]
            def f(e, half=half, ps=ps):
                ins = None
                for j in range(4):
                    dc = half * 4 + j
                    ins = e.transpose(out=ps[:, j * 128:(j + 1) * 128], in_=xt[:, dc * 128:(dc + 1) * 128], identity=self.ident[:])
                return ins
            P.op("pe", f, R=[xt, self.ident], W=[ps])
            P.op("act", lambda e, half=half, ps=ps: e.activation(
                out=self.hT[:, half * 4:(half + 1) * 4, n * 128:(n + 1) * 128],
                in_=ps[:].rearrange("p (j t) -> p j t", j=4), func=AF.Copy), R=[ps], W=[(self.hT, n)])

    def publish(self, tok):
        for e in ("pe", "act", "dve", "pool"):
            self.P.wait_tok(e, tok)

    def seq(self, b):
        P, T, NT = self.P, self.T, self.NT
        if b == 0:
            tok = P.op("act", lambda e: e.activation(out=self.esink[:], in_=self.esink[:], func=AF.Exp), W=[self.esink])
            self.publish(tok)
            self.alloc_work()
            self.xt = [self.big[3], self.big[4]]
            self.yt = [self.big[0], self.big[1]]
            self.lnbc = self.big[2]
        P.dma("sp", self.lnbc[:, 0:2 * D].rearrange("p (a d) -> p a d", a=2), self.ln0_d[:].partition_broadcast(128), R=[self.ln0_d], W=[self.lnbc])
        for n in range(NT):
            xt = self.xt[n % 2]
            P.dma("sp", xt[:, 0:D], self.x[b, n * 128:(n + 1) * 128, :], R=[self.x], W=[xt])
            self.layernorm(xt, n % 2)
            P.dma("sp", self.hbuf[n * 128:(n + 1) * 128, :], xt[:, 0:D], R=[xt], W=[(self.hbuf, n)])
            self.to_hT(xt, n)
        if b == 0:
            self.dump("hT", self.hT, self.hT[:, :, :], [128, 8, T])
        for l in range(self.NL):
            self.layer(b, l)

    def load_w(self, l, g, part):
        P = self.P
        wt = self.wt[0]
        s0, e0 = grange(g)
        tm0 = LAY[g + 'TM'][0]
        if part == 'fm':
            a, b_ = s0, tm0
        else:
            a, b_ = tm0, e0
            nt = e0 - tm0
            P.dma("sp", self.b_tm_s[:, 0:nt], self.b_tm_d[l, TMO[g + 'TM']:TMO[g + 'TM'] + nt].partition_broadcast(128), R=[self.b_tm_d], W=[self.b_tm_s])
        self.wbase = a
        gi = GL.index(g)
        P.dma("sp", wt[:, :, 0:b_ - a], self.w_bf[l, :, a:b_].rearrange("(dc p) c -> p dc c", p=128),
              R=[(self.w_bf, l * 4 + gi)], W=[wt])
        return wt

    def wcols(self, g, name):
        a, c = LAY[name]
        return a - self.wbase, c

    def proj_fm(self, ps, wt, g, name, c0, cw, mrows=128):
        a, c = self.wcols(g, name)
        n0, n1 = c0 // 128, (c0 + cw - 1) // 128
        def f(e):
            ins = None
            for dc in range(8):
                ins = e.matmul(ps[0:c, 0:cw], lhsT=wt[:, dc, a:a + c], rhs=self.hT[:, dc, c0:c0 + cw], start=(dc == 0), stop=(dc == 7))
            return ins
        self.P.op("pe", f, R=[wt, (self.hT, range(n0, n1 + 1))], W=[ps])

    def proj_tm(self, pss, wt, g, name, n):
        a, c = self.wcols(g, name)
        def f(e):
            ins = None
            for j, ps in enumerate(pss):
                cc = min(512, c - j * 512)
                for dc in range(8):
                    ins = e.matmul(ps[:, 0:cc], lhsT=self.hT[:, dc, n * 128:(n + 1) * 128], rhs=wt[:, dc, a + j * 512:a + j * 512 + cc],
                                   start=(dc == 0), stop=(dc == 7))
            return ins
        self.P.op("pe", f, R=[wt, (self.hT, n)], W=list(pss))

    def alloc_work(self):
        P, T, NT = self.P, self.T, self.NT
        BW = max(T + 128, 2176)
        self.big = [P.sbuf("big%d" % i, [128, BW]) for i in range(6)]
        self.fmb = self.big[0:4]
        self.t1 = [P.sbuf("t1_%d" % i, [128, 512]) for i in range(2)]
        self.vaug = P.sbuf("vaug", [128, NT, 4, 65], slots=NT)
        P.op("pool", lambda e: e.memset(self.vaug[:], 1.0), W=[self.vaug])
        self.zt = [P.sbuf("zt%d" % i, [128, 768]) for i in range(2)]
        self.pt = [P.sbuf("pt%d" % i, [128, 512]) for i in range(3)]
        self.t2 = [self.pt[0], self.pt[1]]
        self.ya = [P.sbuf("ya%d" % i, [128, 256]) for i in range(2)]
        self.sm = [P.sbuf("sm%d" % i, [128, 16]) for i in range(2)]
        self.cnt = 0
        self.sc = self.big[4:6]
        self.gbuf = P.dram("gbuf", [4, 3, T], F32)
        self.gsl = [P.sbuf("gsl%d" % i, [4, 3, 128]) for i in range(2)]
        self.EAc = P.sbuf("EAc", [128, T // 64, 2])
        self.z4 = P.sbuf("z4", [4, 2])
        self.publish(P.op("pool", lambda e: e.memset(self.z4[:], 0.0), W=[self.z4]))
        self.nbif = P.sbuf("nbif", [4, self.NL, 2])
        self.publish(P.op("dve", lambda e: e.tensor_scalar(out=self.nbif[:], in0=self.b_if[:], scalar1=-1.0, scalar2=None, op0=ALU.mult), W=[self.nbif]))
        self.smb = [P.sbuf("smb%d" % i, [128, 32]) for i in range(2)]
        self.w1 = [self.pt[0], self.pt[1]]
        self.wib = [P.sbuf("wib%d" % i, [128, 512]) for i in range(2)]
        self.qtl = [P.sbuf("qtl%d" % i, [128, 2, 128]) for i in range(2)]
        self.kw = [P.sbuf("kw%d" % i, [128, 256]) for i in range(2)]
        self.caug = P.sbuf("caug", [128, 2, 65])
        self.hb = [P.sbuf("hb%d" % i, [128, 256]) for i in range(2)]
        self.lbt = P.sbuf("lbt", [128, self.NL, 2, 2])
        P.op("pool", lambda e: e.memset(self.lbt[:, :, :, 0], 0.0), W=[self.lbt])
        P.op("pool", lambda e: e.memset(self.lbt[:, :, :, 1], 1.0), W=[self.lbt])
        if self.NL > 1:
            P.op("dve", lambda e: e.tensor_tensor(out=self.lbt[:, 1, :, 0], in0=self.clb[:, :, 1], in1=self.clb[:, :, 0], op=ALU.subtract), W=[self.lbt])
            P.op("act", lambda e: e.activation(out=self.lbt[:, 1, :, 0], in_=self.lbt[:, 1, :, 0], func=AF.Sigmoid), R=[self.lbt], W=[self.lbt])
            P.op("dve", lambda e: e.tensor_scalar(out=self.lbt[:, 1, :, 1], in0=self.lbt[:, 1, :, 0], scalar1=-1.0, scalar2=1.0, op0=ALU.mult, op1=ALU.add), R=[self.lbt], W=[self.lbt])
        self.publish(P.op("dve", lambda e: e.tensor_copy(out=self.lbt[:, 0, :, 0], in_=self.lbt[:, 0, :, 0]), R=[self.lbt], W=[self.lbt]))
        self.S2 = P.sbuf("S2", [128, 64]); self.st = [P.sbuf("st%d" % i, [128, 64]) for i in range(2)]
        self.at = [P.sbuf("at%d" % i, [128, 128]) for i in range(2)]
        self.kh = [P.sbuf("kh%d" % i, [128, 128]) for i in range(2)]
        self.oc = [P.sbuf("oc%d" % i, [128, 128]) for i in range(2)]
        self.osq = [P.sbuf("osq%d" % i, [128, 128]) for i in range(2)]
        self.vz = [P.sbuf("vz%d" % i, [128, 256]) for i in range(2)]
        self.cbias = P.sbuf("cbias", [128, 2])
        self.h1 = P.sbuf("h1", [128, 128]); self.kcr = P.sbuf("kcr", [128, 128]); self.kct = P.sbuf("kct", [128, 128])
        self.pc = self.wib
        self.ocmp = [P.sbuf("ocmp%d" % i, [128, 256]) for i in range(2)]
        self.imp = [P.sbuf("imp%d" % i, [128, 64]) for i in range(2)]
        self.selb = [P.sbuf("selb%d" % i, [128, 32]) for i in range(2)]
        self.gs = [P.sbuf("gs%d" % i, [128, 16]) for i in range(2)]
        self.yd = [P.sbuf("yd%d" % i, [128, 256]) for i in range(2)]
        self.yd2 = [P.sbuf("yd2_%d" % i, [128, 256]) for i in range(2)]
        self.zero256 = P.sbuf("zero256", [128, 256])
        self.publish(P.op("pool", lambda e: e.memset(self.zero256[:], 0.0), W=[self.zero256]))
        self.ident_bf = P.sbuf("ident_bf", [128, 128], BF16)
        self.publish(P.op("dve", lambda e: e.tensor_copy(out=self.ident_bf[:], in_=self.ident[:]), W=[self.ident_bf]))
        self.vaug_b = P.sbuf("vaug_b", [128, NT, 2, 65], BF16, slots=NT)
        self.publish(P.op("dve", lambda e: e.memset(self.vaug_b[:], 1.0), W=[self.vaug_b]))
        self.kcb = P.sbuf("kcb", [64, 128], BF16)
        self.oT = [P.sbuf("oT%d" % i, [65, 512]) for i in range(2)]
        self.yTt = [P.sbuf("yTt%d" % i, [128, 8, 128], BF16) for i in range(2)]

    def rope_block(self, wt, g, blk, blks, dst, l):
        P, T = self.P, self.T
        SW = min(512, T)
        for s in range(T // SW):
            c0 = s * SW
            i = self.cnt % 2; self.cnt += 1
            pa, pb = self.ps[0 + i], self.ps[2 + i]
            t1, t2 = self.t1[i], self.t2[i]
            cst = self.cst[i]
            P.dma("sp", cst[:, 0, 0:SW], self.cosT_d[:, c0:c0 + SW], R=[self.cosT_d], W=[cst])
            P.dma("sp", cst[:, 1, 0:SW], self.sinT_d[:, c0:c0 + SW], R=[self.sinT_d], W=[cst])
            self.proj_fm(pa, wt, g, blk, c0, SW)
            self.proj_fm(pb, wt, g, blks, c0, SW)
            P.op("act", lambda e: e.activation(out=t1[:, 0:SW], in_=pa[:, 0:SW], func=AF.Identity, bias=self.b_fm[:, l, FMI[blk]:FMI[blk] + 1]), R=[pa], W=[t1])
            P.op("act", lambda e: e.activation(out=t2[:, 0:SW], in_=pb[:, 0:SW], func=AF.Identity, bias=self.b_fm[:, l, FMI[blks]:FMI[blks] + 1]), R=[pb], W=[t2])
            P.op("dve", lambda e: e.tensor_tensor(out=t1[:, 0:SW], in0=t1[:, 0:SW], in1=cst[:, 0, 0:SW], op=ALU.mult), R=[t1, cst], W=[t1])
            P.op("pool", lambda e: e.tensor_tensor(out=t2[:, 0:SW], in0=t2[:, 0:SW], in1=cst[:, 1, 0:SW], op=ALU.mult), R=[t2, cst], W=[t2])
            P.op("dve", lambda e: e.tensor_tensor(out=dst[:, c0:c0 + SW], in0=t1[:, 0:SW], in1=t2[:, 0:SW], op=ALU.add), R=[t1, t2], W=[dst])

    def emit_y(self, ya, n, m0):
        mi = m0 // 2
        self.P.dma("sp", self.ybuf[n * 128:(n + 1) * 128, m0 * 128:m0 * 128 + 256], ya[:], R=[ya], W=[(self.ybuf, n * 4 + mi)])

    def mixer_A(self, b, l, wt):
        P, T, NT = self.P, self.T, self.NT
        g = 'A'
        qr = [self.fmb[0], self.fmb[1]]
        kr = self.fmb[2]
        self.chk("A0")
        self.rope_block(wt, g, 'AQ0', 'AQ0s', qr[0], l)
        self.chk("A1")
        self.rope_block(wt, g, 'AQ1', 'AQ1s', qr[1], l)
        self.rope_block(wt, g, 'AK', 'AKs', kr, l)
        self.chk("A1c")
        if b == 0 and l == 0:
            self.dump("A_q0", qr[0], qr[0][:], [128, T]); self.dump("A_k", kr, kr[:], [128, T])
        tmo = 0
        self.load_w(l, g, 'tm')
        self.chk("A1d")
        for n in range(NT):
            i = n % 2
            ptm = self.ps[6]
            zt, ya, sm = self.zt[i], self.ya[i], self.sm[i]
            self.proj_tm([ptm], wt, g, 'ATM', n)
            self.chk("A1e")
            P.op("dve", lambda e: e.tensor_tensor(out=self.vaug[:, n, 0:2, 0:64], in0=ptm[:, 0:128].rearrange("p (g d) -> p g d", g=2),
                                                  in1=self.b_tm_s[:, tmo:tmo + 128].rearrange("p (g d) -> p g d", g=2), op=ALU.add),
                 R=[ptm, self.b_tm_s], W=[(self.vaug, n)])
            self.chk("A2a")
            P.op("dve", lambda e: e.tensor_tensor(out=zt[:, 0:256], in0=ptm[:, 128:384], in1=self.b_tm_s[:, tmo + 128:tmo + 384], op=ALU.add), R=[ptm, self.b_tm_s], W=[zt])
            self.chk("A2b")
            P.op("act", lambda e: e.activation(out=zt[:, 0:256], in_=zt[:, 0:256], func=AF.Silu), R=[zt], W=[zt])
            self.chk("A2")
            kts = [kt for kt in (n - 1, n) if kt >= 0]
            po = self.ps[7]
            pts = []
            for kt in kts:
                psn = self.ps[2 + (self.cnt % 2)]; pt = self.pt[self.cnt % 3]; self.cnt += 1
                specs = []
                for gg in range(2):
                    for r in range(2):
                        specs.append((gg * 64, lambda e, gg=gg, r=r, kt=kt, psn=psn: e.matmul(
                            psn[:, (gg * 2 + r) * 128:(gg * 2 + r + 1) * 128], lhsT=kr[gg * 64:(gg + 1) * 64, kt * 128:(kt + 1) * 128],
                            rhs=qr[r][gg * 64:(gg + 1) * 64, n * 128:(n + 1) * 128], start=True, stop=True)))
                self.pe_seq(specs, R=[kr, qr[0], qr[1]], W=[psn])
                self.chk("A3a")
                P.op("act", lambda e, psn=psn, pt=pt: e.activation(out=pt[:], in_=psn[:], func=AF.Exp, scale=0.125), R=[psn], W=[pt])
                self.chk("A3b")
                mask = self.tri_ge if kt == n else self.tri_lt
                P.op("dve", lambda e, pt=pt, mask=mask: e.tensor_tensor(out=pt[:], in0=pt[:], in1=mask[:], op=ALU.mult), R=[pt], W=[pt])
                pts.append((kt, pt))
            self.chk("A3")
            def f(e):
                ins = None
                for gg in range(2):
                    for r in range(2):
                        h = gg * 2 + r
                        for j, (kt, pt) in enumerate(pts):
                            ins = e.matmul(po[:, h * 65:(h + 1) * 65], lhsT=pt[:, h * 128:(h + 1) * 128], rhs=self.vaug[:, kt, gg, :],
                                           start=(j == 0), stop=(j == len(pts) - 1))
                return ins
            P.op("pe", f, R=[p for _, p in pts] + [(self.vaug, kts)], W=[po])
            pov = po[:, 0:260].rearrange("p (h c) -> p h c", h=4)
            P.op("dve", lambda e: e.tensor_tensor(out=sm[:, 0:4], in0=pov[:, :, 64], in1=self.esink[:, l, :], op=ALU.add), R=[po], W=[sm])
            P.op("dve", lambda e: e.reciprocal(out=sm[:, 4:8], in_=sm[:, 0:4]), R=[sm], W=[sm])
            yav = ya[:].rearrange("p (h d) -> p h d", h=4)
            P.op("dve", lambda e: e.tensor_tensor(out=yav, in0=pov[:, :, 0:64], in1=sm[:, 4:8].unsqueeze(2).to_broadcast([128, 4, 64]), op=ALU.mult), R=[po, sm], W=[ya])
            P.op("pool", lambda e: e.tensor_tensor(out=ya[:], in0=ya[:], in1=zt[:, 0:256], op=ALU.mult), R=[ya, zt], W=[ya])
            self.chk("A4")
            self.emit_y(ya, n, 0)

    def mixer_B(self, b, l, wt):
        P, T, NT = self.P, self.T, self.NT
        g = 'B'
        SW = min(512, T)
        raw = self.sc[0]
        X1, X2, X3, X4 = self.big[0:4]
        for s_ in range(T // SW):
            c0 = s_ * SW
            pi, pf = self.ps[0], self.ps[1]
            self.proj_fm(pi, wt, g, 'BI', c0, SW)
            self.proj_fm(pf, wt, g, 'BF', c0, SW)
            P.op("act", lambda e, c0=c0: e.activation(out=X1[0:4, c0:c0 + SW], in_=pi[0:4, 0:SW], func=AF.Identity, bias=self.b_if[:, l, 0:1]), R=[pi], W=[X1])
            P.op("act", lambda e, c0=c0: e.activation(out=X4[0:4, c0:c0 + SW], in_=pf[0:4, 0:SW], func=AF.Exp, scale=-1.0, bias=self.nbif[:, l, 1:2]), R=[pf], W=[X4])
        P.op("act", lambda e: e.activation(out=X4[0:4, 0:T], in_=X4[0:4, 0:T], func=AF.Ln, bias=1.0), R=[X4], W=[X4])
        P.op("dve", lambda e: e.tensor_tensor_scan(out=X3[0:4, 0:T], data0=X4[0:4, 0:T], data1=self.z4[:, 0:1].to_broadcast([4, T]), initial=0.0, op0=ALU.add, op1=ALU.add), R=[X4], W=[X3])
        P.op("dve", lambda e: e.tensor_tensor(out=X1[0:4, 0:T], in0=X1[0:4, 0:T], in1=X3[0:4, 0:T], op=ALU.add), R=[X1, X3], W=[X1])
        P.op("pool", lambda e: e.memset(X2[0:4, 0:128], 0.0), W=[X2])
        P.op("dve", lambda e: e.tensor_tensor_scan(out=X2[0:4, 128:128 + T], data0=X1[0:4, 0:T], data1=self.z4[:, 0:1].to_broadcast([4, T]), initial=0.0, op0=ALU.max, op1=ALU.add), R=[X1], W=[X2])
        P.op("dve", lambda e: e.tensor_tensor(out=X3[0:4, 0:T], in0=X3[0:4, 0:T], in1=X2[0:4, 128:128 + T], op=ALU.subtract), R=[X3, X2], W=[X3])
        P.op("act", lambda e: e.activation(out=X3[0:4, 0:T], in_=X3[0:4, 0:T], func=AF.Exp), R=[X3], W=[X3])
        gprev = X2[0:4, 0:T].rearrange("p (c t) -> p c t", t=128)[:, :, 127:128].to_broadcast([4, NT, 128])
        P.op("dve", lambda e: e.tensor_tensor(out=X4[0:4, 0:T].rearrange("p (c t) -> p c t", t=128), in0=X2[0:4, 128:128 + T].rearrange("p (c t) -> p c t", t=128),
                                              in1=gprev, op=ALU.subtract), R=[X2], W=[X4])
        P.op("dve", lambda e: e.tensor_tensor(out=X1[0:4, 0:T].rearrange("p (c t) -> p c t", t=128), in0=X1[0:4, 0:T].rearrange("p (c t) -> p c t", t=128),
                                              in1=gprev, op=ALU.subtract), R=[X1, X2], W=[X1])
        P.dma("sp", self.gbuf[:, 0, :], X4[0:4, 0:T], R=[X4], W=[self.gbuf])
        P.dma("sp", self.gbuf[:, 1, :], X1[0:4, 0:T], R=[X1], W=[self.gbuf])
        P.dma("sp", self.gbuf[:, 2, :], X3[0:4, 0:T], R=[X3], W=[self.gbuf])
        for j, blk in enumerate(['BQ0', 'BQ1', 'BK0', 'BK1']):
            dst = self.fmb[j]
            P.op("pool", lambda e: e.memset(raw[:, 0:3], 0.0), W=[raw])
            for s_ in range(T // SW):
                c0 = s_ * SW
                ps = self.ps[self.cnt % 2]; self.cnt += 1
                self.proj_fm(ps, wt, g, blk, c0, SW)
                P.op("act", lambda e, ps=ps, c0=c0: e.activation(out=raw[:, 3 + c0:3 + c0 + SW], in_=ps[:, 0:SW], func=AF.Identity,
                                                                 bias=self.b_fm[:, l, FMI[blk]:FMI[blk] + 1]), R=[ps], W=[raw])
            P.op("dve", lambda e: e.tensor_scalar(out=dst[:, 0:T], in0=raw[:, 3:3 + T], scalar1=self.cw[:, l, j, 3:4], scalar2=self.cb[:, l, j:j + 1],
                                                  op0=ALU.mult, op1=ALU.add), R=[raw], W=[dst])
            for tap in (2, 1, 0):
                P.op("dve", lambda e, tap=tap: e.scalar_tensor_tensor(out=dst[:, 0:T], in0=raw[:, tap:tap + T], scalar=self.cw[:, l, j, tap:tap + 1],
                                                                      in1=dst[:, 0:T], op0=ALU.mult, op1=ALU.add), R=[raw, dst], W=[dst])
            P.op("act", lambda e: e.activation(out=dst[:, 0:T], in_=dst[:, 0:T], func=AF.Silu), R=[dst], W=[dst])
        qT = [self.fmb[0], self.fmb[1]]
        kT = [self.fmb[2], self.fmb[3]]
        self.load_w(l, g, 'tm')
        tmo = 0
        caug = self.caug
        for c in range(NT):
            i = c % 2
            cs_ = slice(c * 128, (c + 1) * 128)
            zt, hb, smb, w1, wib, qtl, kw = self.zt[i], self.hb[i], self.smb[i], self.w1[i], self.wib[i], self.qtl[i], self.kw[i]
            ptm = [self.ps[6], self.ps[7]]
            self.proj_tm(ptm, wt, g, 'BTM', c)
            P.op("dve", lambda e: e.tensor_tensor(out=self.vaug[:, c, :, 0:64], in0=ptm[0][:, 0:256].rearrange("p (h d) -> p h d", h=4),
                                                  in1=self.b_tm_s[:, tmo:tmo + 256].rearrange("p (h d) -> p h d", h=4), op=ALU.add),
                 R=[ptm[0], self.b_tm_s], W=[(self.vaug, c)])
            P.op("dve", lambda e: e.tensor_tensor(out=zt[:, 0:256], in0=ptm[0][:, 256:512], in1=self.b_tm_s[:, tmo + 256:tmo + 512], op=ALU.add), R=[ptm[0], self.b_tm_s], W=[zt])
            P.op("dve", lambda e: e.tensor_tensor(out=zt[:, 256:512], in0=ptm[1][:, 0:256], in1=self.b_tm_s[:, tmo + 512:tmo + 768], op=ALU.add), R=[ptm[1], self.b_tm_s], W=[zt])
            P.op("act", lambda e: e.activation(out=zt[:, 0:256], in_=zt[:, 0:256], func=AF.Sigmoid), R=[zt], W=[zt])
            P.op("act", lambda e: e.activation(out=zt[:, 256:512], in_=zt[:, 256:512], func=AF.Silu), R=[zt], W=[zt])
            gsl = self.gsl[i]
            P.dma("sp", gsl[:], self.gbuf[:, :, cs_], R=[self.gbuf], W=[gsl])
            psT = self.ps[0]
            def f(e):
                e.transpose(out=psT[:, 0:4], in_=gsl[0:4, 1, :], identity=self.ident[0:4, 0:4])
                return e.transpose(out=psT[:, 4:8], in_=gsl[0:4, 2, :], identity=self.ident[0:4, 0:4])
            self.pe_seq([(0, f)], R=[gsl], W=[psT])
            P.op("dve", lambda e: e.tensor_copy(out=smb[:, 0:8], in_=psT[:, 0:8]), R=[psT], W=[smb])
            psG = self.ps[1]
            def f(e):
                ins = None
                for h in range(4):
                    ins = e.matmul(psG[:, h * 128:(h + 1) * 128], lhsT=self.sel[0:4, h, :], rhs=gsl[0:4, 0, :], start=True, stop=True)
                return ins
            self.pe_seq([(0, f)], R=[gsl], W=[psG])
            for h in range(4):
                P.op("act", lambda e, h=h: e.activation(out=w1[:, h * 128:(h + 1) * 128], in_=psG[:, h * 128:(h + 1) * 128], func=AF.Exp, scale=-1.0,
                                                        bias=smb[:, h:h + 1]), R=[psG, smb], W=[w1])
            P.op("act", lambda e: e.activation(out=wib[:], in_=psG[:], func=AF.Exp, scale=-1.0), R=[psG], W=[wib])
            P.op("pool", lambda e: e.tensor_tensor(out=w1[:], in0=w1[:], in1=self.tri_ge[:], op=ALU.mult), R=[w1], W=[w1])
            P.op("dve", lambda e: e.tensor_tensor(out=smb[:, 8:12], in0=smb[:, 0:4], in1=psG[:].rearrange("p (h t) -> p h t", h=4)[:, :, 127], op=ALU.subtract),
                 R=[smb, psG], W=[smb])
            P.op("act", lambda e: e.activation(out=smb[:, 8:12], in_=smb[:, 8:12], func=AF.Exp), R=[smb], W=[smb])
            psS = self.ps[2]
            specs = []
            for h in (0, 2, 1, 3):
                j, base = h // 2, (h % 2) * 64
                specs.append((base, lambda e, h=h, j=j, base=base: e.matmul(psS[:, h * 128:(h + 1) * 128], lhsT=kT[j][base:base + 64, cs_],
                                                                          rhs=qT[j][base:base + 64, cs_], start=True, stop=True)))
            self.pe_seq(specs, R=[kT[0], kT[1], qT[0], qT[1]], W=[psS])
            P.op("dve", lambda e: e.scalar_tensor_tensor(out=w1[:], in0=psS[:], scalar=0.125, in1=w1[:], op0=ALU.mult, op1=ALU.mult), R=[psS, w1], W=[w1])
            if c > 0:
                for h in range(4):
                    j, base = h // 2, (h % 2) * 64
                    P.op("dve", lambda e, h=h, j=j, base=base: e.scalar_tensor_tensor(
                        out=qtl[base:base + 64, j, :], in0=qT[j][base:base + 64, cs_], scalar=0.125, in1=wib[base:base + 64, h * 128:(h + 1) * 128],
                        op0=ALU.mult, op1=ALU.mult), R=[qT[j], wib], W=[qtl])
            po = self.ps[3]
            specs = []
            for h in (0, 2, 1, 3):
                j, base = h // 2, (h % 2) * 64
                specs.append((None, lambda e, h=h: e.matmul(po[:, h * 65:(h + 1) * 65], lhsT=w1[:, h * 128:(h + 1) * 128], rhs=self.vaug[:, c, h, :], start=True, stop=(c == 0))))
                if c > 0:
                    specs.append((base, lambda e, h=h, j=j, base=base: e.matmul(po[:, h * 65:(h + 1) * 65], lhsT=qtl[base:base + 64, j, :], rhs=caug[base:base + 64, j, :],
                                                                              start=False, stop=True)))
            self.pe_seq(specs, R=[w1, (self.vaug, c)] + ([qtl, caug] if c > 0 else []), W=[po])
            pov = po[:, 0:260].rearrange("p (h c) -> p h c", h=4)
            P.op("act", lambda e: e.activation(out=smb[:, 12:16], in_=pov[:, :, 64], func=AF.Abs), R=[po], W=[smb])
            P.op("dve", lambda e: e.tensor_tensor(out=smb[:, 12:16], in0=smb[:, 12:16], in1=smb[:, 4:8], op=ALU.max), R=[smb], W=[smb])
            P.op("dve", lambda e: e.reciprocal(out=smb[:, 16:20], in_=smb[:, 12:16]), R=[smb], W=[smb])
            hbv = hb[:].rearrange("p (h d) -> p h d", h=4)
            P.op("dve", lambda e: e.tensor_tensor(out=hbv, in0=pov[:, :, 0:64], in1=smb[:, 16:20].unsqueeze(2).to_broadcast([128, 4, 64]), op=ALU.mult), R=[po, smb], W=[hb])
            P.op("pool", lambda e: e.tensor_tensor(out=hb[:], in0=hb[:], in1=zt[:, 0:256], op=ALU.mult), R=[hb, zt], W=[hb])
            P.op("pool", lambda e: e.tensor_tensor(out=hb[:], in0=hb[:], in1=zt[:, 256:512], op=ALU.mult), R=[hb, zt], W=[hb])
            self.emit_y(hb, c, 2)
            if c < NT - 1:
                pk = self.ps[4]
                def f(e):
                    e.transpose(out=pk[:, 0:128], in_=kT[0][:, cs_], identity=self.ident[:])
                    return e.transpose(out=pk[:, 128:256], in_=kT[1][:, cs_], identity=self.ident[:])
                P.op("pe", f, R=[kT[0], kT[1]], W=[pk])
                P.op("dve", lambda e: e.tensor_tensor(out=kw[:].rearrange("p (h d) -> p h d", h=4), in0=pk[:, 0:256].rearrange("p (h d) -> p h d", h=4),
                                                      in1=smb[:, 8:12].unsqueeze(2).to_broadcast([128, 4, 64]), op=ALU.mult), R=[pk, smb], W=[kw])
                pu = self.ps[5]
                def f(e):
                    ins = None
                    for j in range(2):
                        ins = e.matmul(pu[:, j * 130:(j + 1) * 130], lhsT=kw[:, j * 128:(j + 1) * 128],
                                       rhs=self.vaug[:, c, 2 * j:2 * j + 2, :], start=True, stop=True)
                    return ins
                P.op("pe", f, R=[kw, (self.vaug, c)], W=[pu])
                for h in range(4):
                    j, base = h // 2, (h % 2) * 64
                    src = pu[base:base + 64, j * 130 + (h % 2) * 65:j * 130 + (h % 2) * 65 + 65]
                    if c == 0:
                        P.op("dve", lambda e, src=src, j=j, base=base: e.tensor_copy(out=caug[base:base + 64, j, :], in_=src), R=[pu], W=[caug])
                    else:
                        P.op("dve", lambda e, src=src, j=j, base=base, h=h: e.scalar_tensor_tensor(
                            out=caug[base:base + 64, j, :], in0=caug[base:base + 64, j, :], scalar=wib[base:base + 64, h * 128 + 127:h * 128 + 128], in1=src,
                            op0=ALU.mult, op1=ALU.add), R=[pu, caug, wib], W=[caug])

    def mixer_C(self, b, l, wt):
        P, T, NT = self.P, self.T, self.NT
        g = 'C'
        SW = min(512, T)
        NC = T // 64
        qs, fk, lfd, aa, tmp, kt_ = self.big
        qt_, kh_, EAc = qs, fk, self.EAc
        v3 = lambda t: t[:, 0:T].rearrange("p (c t) -> p c t", t=64)
        tmo = 0
        for j in range(2):
            bq, bf = 'CQ%d' % j, 'CF%d' % j
            if j > 0:
                wt = self.load_w(l, g, 'fm')
            for s_ in range(T // SW):
                c0 = s_ * SW
                pa, pb_ = self.ps[0], self.ps[1]
                self.proj_fm(pa, wt, g, bq, c0, SW)
                self.proj_fm(pb_, wt, g, bf, c0, SW)
                P.op("act", lambda e, c0=c0: e.activation(out=qs[:, c0:c0 + SW], in_=pa[:, 0:SW], func=AF.Silu, bias=self.b_fm[:, l, FMI[bq]:FMI[bq] + 1]), R=[pa], W=[qs])
                P.op("act", lambda e, c0=c0: e.activation(out=fk[:, c0:c0 + SW], in_=pb_[:, 0:SW], func=AF.Sigmoid, bias=self.b_fm[:, l, FMI[bf]:FMI[bf] + 1]), R=[pb_], W=[fk])
            P.op("dve", lambda e: e.tensor_scalar(out=fk[:, 0:T], in0=fk[:, 0:T], scalar1=self.lbt[:, l, j, 1:2], scalar2=self.lbt[:, l, j, 0:1], op0=ALU.mult, op1=ALU.add), R=[fk], W=[fk])
            P.op("act", lambda e: e.activation(out=lfd[:, 0:T], in_=fk[:, 0:T], func=AF.Ln), R=[fk], W=[lfd])
            P.op("dve", lambda e: e.tensor_scalar(out=fk[:, 0:T], in0=fk[:, 0:T], scalar1=-1.0, scalar2=1.0, op0=ALU.mult, op1=ALU.add), R=[fk, lfd], W=[fk])
            P.op("pool", lambda e: e.memset(kt_[:, 0:T], 1.0), W=[kt_])
            P.op("pool", lambda e: e.memset(v3(kt_)[:, :, 0:1], 0.0), W=[kt_])
            P.op("dve", lambda e: e.tensor_tensor_scan(out=aa[:, 0:T], data0=kt_[:, 0:T], data1=lfd[:, 0:T], initial=0.0, op0=ALU.mult, op1=ALU.add), R=[kt_, lfd], W=[aa])
            P.op("act", lambda e: e.activation(out=EAc[:, :, 0], in_=v3(aa)[:, :, 31], func=AF.Exp), R=[aa], W=[EAc])
            P.op("act", lambda e: e.activation(out=EAc[:, :, 1], in_=v3(aa)[:, :, 63], func=AF.Exp), R=[aa], W=[EAc])
            P.op("dve", lambda e: e.tensor_tensor(out=v3(lfd), in0=v3(aa), in1=v3(aa)[:, :, 31:32].to_broadcast([128, NC, 64]), op=ALU.subtract), R=[aa], W=[lfd])
            P.op("act", lambda e: e.activation(out=tmp[:, 0:T], in_=lfd[:, 0:T], func=AF.Exp), R=[lfd], W=[tmp])
            P.op("dve", lambda e: e.tensor_tensor(out=qt_[:, 0:T], in0=qs[:, 0:T], in1=tmp[:, 0:T], op=ALU.mult), R=[qs, tmp], W=[qt_])
            P.op("act", lambda e: e.activation(out=tmp[:, 0:T], in_=lfd[:, 0:T], func=AF.Exp, scale=-1.0), R=[lfd, qt_], W=[tmp])
            P.op("pool", lambda e: e.tensor_tensor(out=kt_[:, 0:T], in0=fk[:, 0:T], in1=tmp[:, 0:T], op=ALU.mult), R=[fk, tmp], W=[kt_])
            P.op("dve", lambda e: e.tensor_tensor(out=v3(tmp), in0=v3(lfd)[:, :, 63:64].to_broadcast([128, NC, 64]), in1=v3(lfd), op=ALU.subtract), R=[lfd, kt_], W=[tmp])
            P.op("act", lambda e: e.activation(out=tmp[:, 0:T], in_=tmp[:, 0:T], func=AF.Exp), R=[tmp], W=[tmp])
            P.op("pool", lambda e: e.tensor_tensor(out=kh_[:, 0:T], in0=fk[:, 0:T], in1=tmp[:, 0:T], op=ALU.mult), R=[fk, tmp], W=[kh_])
            S2 = self.S2
            self.load_w(l, g, 'tm')
            aC, cC = self.wcols(g, 'CTM')
            for n in range(NT):
                i = n % 2
                vz, oc, osq, sm = self.vz[i], self.oc[i], self.osq[i], self.sm[i]
                ptm = self.ps[6]
                def f(e):
                    ins = None
                    for r, off in enumerate((j * 128, 256 + j * 128)):
                        for dc in range(8):
                            ins = e.matmul(ptm[:, r * 128:(r + 1) * 128], lhsT=self.hT[:, dc, n * 128:(n + 1) * 128], rhs=wt[:, dc, aC + off:aC + off + 128],
                                           start=(dc == 0), stop=(dc == 7))
                    return ins
                P.op("pe", f, R=[wt, (self.hT, n)], W=[ptm])
                P.op("dve", lambda e: e.tensor_tensor(out=vz[:, 0:128], in0=ptm[:, 0:128], in1=self.b_tm_s[:, tmo + j * 128:tmo + (j + 1) * 128], op=ALU.add), R=[ptm, self.b_tm_s], W=[vz])
                P.op("dve", lambda e: e.tensor_tensor(out=vz[:, 128:256], in0=ptm[:, 128:256], in1=self.b_tm_s[:, tmo + 256 + j * 128:tmo + 256 + (j + 1) * 128], op=ALU.add), R=[ptm, self.b_tm_s], W=[vz])
                P.op("act", lambda e: e.activation(out=vz[:, 128:256], in_=vz[:, 128:256], func=AF.Silu), R=[vz], W=[vz])
                po = self.ps[4 + i]
                khb = self.kh[i]
                if n * 2 < NC - 1:
                    pk = self.ps[0]
                    P.op("pe", lambda e, pk=pk: e.transpose(out=pk[:, 0:128], in_=kh_[:, n * 128:(n + 1) * 128], identity=self.ident[:]), R=[kh_], W=[pk])
                    P.op("act", lambda e, pk=pk: e.activation(out=khb[:], in_=pk[:, 0:128], func=AF.Copy), R=[pk], W=[khb])
                for cp in range(2):
                    c = n * 2 + cp
                    pb = cp * 64
                    cs_ = slice(c * 64, (c + 1) * 64)
                    at, kh, st = self.at[cp], khb, self.st[cp]
                    psA = self.ps[2 + cp]
                    specs = []
                    for hh in range(2):
                        base = hh * 64
                        specs.append((base, lambda e, hh=hh, base=base, psA=psA, pb=pb, cs_=cs_: e.matmul(
                            psA[pb:pb + 64, hh * 64:(hh + 1) * 64], lhsT=kt_[base:base + 64, cs_], rhs=qt_[base:base + 64, cs_], start=True, stop=True)))
                    self.pe_seq(specs, R=[kt_, qt_], W=[psA])
                    pU = self.ps[1] if cp == 0 else self.ps[7]
                    if c < NC - 1:
                        self.pe_seq([(pb, lambda e, pU=pU, kh=kh, pb=pb: e.matmul(pU[:, 0:128], lhsT=kh[pb:pb + 64, :], rhs=vz[pb:pb + 64, 0:128], start=True, stop=True))],
                                    R=[kh, vz], W=[pU])
                    mask = self.tri_ge[pb:pb + 64, :].rearrange("p (r f) -> p r f", f=128)[:, 0:2, pb:pb + 64]
                    P.op("dve", lambda e, at=at, psA=psA, mask=mask, pb=pb: e.tensor_tensor(out=at[pb:pb + 64, :].rearrange("p (r f) -> p r f", f=64),
                                                                                          in0=psA[pb:pb + 64, 0:128].rearrange("p (r f) -> p r f", f=64), in1=mask, op=ALU.mult),
                         R=[psA], W=[at])
                    if c > 0:
                        P.op("dve", lambda e, st=st, c=c: e.tensor_scalar(out=st[:], in0=S2[:], scalar1=EAc[:, c, 0:1], scalar2=None, op0=ALU.mult), R=[S2, EAc], W=[st])
                    specs = []
                    for hh in range(2):
                        base = hh * 64
                        specs.append((pb, lambda e, hh=hh, at=at, pb=pb, c=c: e.matmul(
                            po[pb:pb + 64, hh * 64:(hh + 1) * 64], lhsT=at[pb:pb + 64, hh * 64:(hh + 1) * 64], rhs=vz[pb:pb + 64, hh * 64:(hh + 1) * 64],
                            start=True, stop=(c == 0))))
                        if c > 0:
                            specs.append((base, lambda e, hh=hh, base=base, st=st, pb=pb, cs_=cs_: e.matmul(
                                po[pb:pb + 64, hh * 64:(hh + 1) * 64], lhsT=qt_[base:base + 64, cs_], rhs=st[base:base + 64, :], start=False, stop=True)))
                    self.pe_seq(specs, R=[at, vz, qt_] + ([st] if c > 0 else []), W=[po])
                    if c < NC - 1:
                        for hh in range(2):
                            base = hh * 64
                            if c == 0:
                                P.op("dve", lambda e, base=base, hh=hh, pU=pU: e.tensor_copy(out=S2[base:base + 64, :], in_=pU[base:base + 64, hh * 64:(hh + 1) * 64]), R=[pU], W=[S2])
                            else:
                                P.op("dve", lambda e, base=base, hh=hh, pU=pU, c=c: e.scalar_tensor_tensor(
                                    out=S2[base:base + 64, :], in0=S2[base:base + 64, :], scalar=EAc[base:base + 64, c, 1:2],
                                    in1=pU[base:base + 64, hh * 64:(hh + 1) * 64], op0=ALU.mult, op1=ALU.add), R=[pU, S2, EAc], W=[S2])
                P.op("act", lambda e: e.activation(out=oc[:], in_=po[:, 0:128], func=AF.Copy), R=[po], W=[oc])
                P.op("dve", lambda e: e.tensor_tensor(out=osq[:], in0=oc[:], in1=oc[:], op=ALU.mult), R=[oc], W=[osq])
                P.op("dve", lambda e: e.tensor_reduce(out=sm[:, 0:2], in_=osq[:].rearrange("p (h d) -> p h d", h=2), axis=AX.X, op=ALU.add), R=[osq], W=[sm])
                P.op("dve", lambda e: e.tensor_scalar(out=sm[:, 2:4], in0=sm[:, 0:2], scalar1=1.0 / 64, scalar2=1e-6, op0=ALU.mult, op1=ALU.add), R=[sm], W=[sm])
                P.op("act", lambda e: e.activation(out=sm[:, 2:4], in_=sm[:, 2:4], func=AF.Sqrt), R=[sm], W=[sm])
                P.op("dve", lambda e: e.reciprocal(out=sm[:, 4:6], in_=sm[:, 2:4]), R=[sm], W=[sm])
                P.op("dve", lambda e: e.tensor_tensor(out=oc[:].rearrange("p (h d) -> p h d", h=2), in0=oc[:].rearrange("p (h d) -> p h d", h=2),
                                                      in1=sm[:, 4:6].unsqueeze(2).to_broadcast([128, 2, 64]), op=ALU.mult), R=[oc, sm], W=[oc])
                P.op("pool", lambda e: e.tensor_tensor(out=oc[:], in0=oc[:], in1=self.gbc[:, l, j * 128:(j + 1) * 128], op=ALU.mult), R=[oc], W=[oc])
                P.op("pool", lambda e: e.tensor_tensor(out=oc[:], in0=oc[:], in1=vz[:, 128:256], op=ALU.mult), R=[oc, vz], W=[oc])
                P.dma("sp", self.ybuf[n * 128:(n + 1) * 128, 512 + j * 128:512 + (j + 1) * 128], oc[:], R=[oc], W=[(self.ybuf, n * 4 + 2)])

    def rope64(self, wt, g, blk, blks, l, dst_of):
        P, T = self.P, self.T
        SW = min(512, T)
        for s_ in range(T // SW):
            c0 = s_ * SW
            i = self.cnt % 2; self.cnt += 1
            pa, pb = self.ps[0 + i], self.ps[2 + i]
            t1, t2 = self.t1[i], self.t2[i]
            cst = self.cst[i]
            P.dma("sp", cst[0:64, 0, 0:SW], self.cosT_d[0:64, c0:c0 + SW], R=[self.cosT_d], W=[cst])
            P.dma("sp", cst[0:64, 1, 0:SW], self.sinT_d[0:64, c0:c0 + SW], R=[self.sinT_d], W=[cst])
            self.proj_fm(pa, wt, g, blk, c0, SW)
            self.proj_fm(pb, wt, g, blks, c0, SW)
            P.op("act", lambda e: e.activation(out=t1[0:64, 0:SW], in_=pa[0:64, 0:SW], func=AF.Identity, bias=self.b_fm[0:64, l, FMI[blk]:FMI[blk] + 1]), R=[pa], W=[t1])
            P.op("act", lambda e: e.activation(out=t2[0:64, 0:SW], in_=pb[0:64, 0:SW], func=AF.Identity, bias=self.b_fm[0:64, l, FMI[blks]:FMI[blks] + 1]), R=[pb], W=[t2])
            P.op("dve", lambda e: e.tensor_tensor(out=t1[0:64, 0:SW], in0=t1[0:64, 0:SW], in1=cst[0:64, 0, 0:SW], op=ALU.mult), R=[t1, cst], W=[t1])
            P.op("pool", lambda e: e.tensor_tensor(out=t2[0:64, 0:SW], in0=t2[0:64, 0:SW], in1=cst[0:64, 1, 0:SW], op=ALU.mult), R=[t2, cst], W=[t2])
            dbuf, dap, t1v, t2v = dst_of(c0, SW, t1, t2)
            P.op("dve", lambda e: e.tensor_tensor(out=dap, in0=t1v, in1=t2v, op=ALU.add), R=[t1, t2], W=[dbuf])

    def mixer_D(self, b, l, wt):
        P, T, NT = self.P, self.T, self.NT
        g = 'D'
        SW = min(512, T)
        NCMP, NSL = self.NCMP, self.NSL
        HT = NT if NT <= 8 else NT // 2
        qb = [self.big[0][:, :].bitcast(BF16), self.big[1][:, :].bitcast(BF16)]
        def qtile(n):
            return qb[n // HT][0:64, (n % HT) * 512:(n % HT + 1) * 512]
        kb = self.big[2][:, :].bitcast(BF16)
        KCV = self.big[4]
        w1b = self.big[5]
        w1v = w1b[:, 0:2048].rearrange("p (j e) -> p j e", e=64)
        P.dma("sp", w1v, self.w1_d[l].rearrange("p (j e) -> p j e", e=64), R=[self.w1_d], W=[w1b])
        for h in range(4):
            blk = 'DQ0' if h == 0 else 'DH%d' % h
            def dst_of(c0, SW_, t1, t2, h=h):
                n0 = c0 // 128
                nt_ = SW_ // 128
                buf = self.big[n0 // HT]
                base = (n0 % HT) * 512
                dap = qb[n0 // HT][0:64, base:base + nt_ * 512].rearrange("p (n hh t) -> p n hh t", hh=4, t=128)[:, :, h, :]
                return buf, dap, t1[0:64, 0:SW_].rearrange("p (n t) -> p n t", t=128), t2[0:64, 0:SW_].rearrange("p (n t) -> p n t", t=128)
            self.rope64(wt, g, blk, blk + 's', l, dst_of)
        for name, off in (('DKS', 0), ('DKW', T)):
            def dst_of(c0, SW_, t1, t2, off=off):
                return self.big[2], kb[0:64, off + c0:off + c0 + SW_], t1[0:64, 0:SW_], t2[0:64, 0:SW_]
            self.rope64(wt, g, name, name + 's', l, dst_of)
        for s_ in range(T // SW):
            c0 = s_ * SW
            ps = self.ps[self.cnt % 2]; self.cnt += 1
            self.proj_fm(ps, wt, g, 'DKCV', c0, SW)
            P.op("act", lambda e, ps=ps, c0=c0: e.activation(out=KCV[:, c0:c0 + SW], in_=ps[:, 0:SW], func=AF.Identity,
                                                             bias=self.b_fm[:, l, FMI['DKCV']:FMI['DKCV'] + 1]), R=[ps], W=[KCV])
        pb_ = self.ps[5]
        specs = []
        for half in range(2):
            r = slice(half * 64, half * 64 + 64)
            for j in range(32):
                specs.append((half * 64, lambda e, r=r, j=j: e.matmul(pb_[r, 0:1], lhsT=w1v[r, j, :], rhs=self.peT[r, l, j:j + 1], start=(j == 0), stop=(j == 31))))
        self.pe_seq(specs, R=[w1b], W=[pb_])
        P.op("dve", lambda e: e.tensor_copy(out=self.cbias[:, 0:1], in_=pb_[:, 0:1]), R=[pb_], W=[self.cbias])
        pH = self.ps[6]
        span = 16 * (NCMP - 1) + 1
        specs = []
        for half in range(2):
            r = slice(half * 64, half * 64 + 64)
            for j in range(32):
                specs.append((half * 64, lambda e, r=r, j=j: e.matmul(pH[r, 0:NCMP], lhsT=w1v[r, j, :], rhs=KCV[r, j:j + span:16], start=(j == 0), stop=(j == 31))))
        self.pe_seq(specs, R=[w1b, KCV], W=[pH])
        h1, kcr, kct, kcb = self.h1, self.kcr, self.kct, self.kcb
        P.op("act", lambda e: e.activation(out=h1[:, 0:NCMP], in_=pH[:, 0:NCMP], func=AF.Silu, bias=self.cbias[:, 0:1]), R=[pH, self.cbias], W=[h1])
        pK = self.ps[7]
        self.pe_seq([
            (0, lambda e: e.matmul(pK[:, 0:NCMP], lhsT=self.w2s[0:64, l, 0:128], rhs=h1[0:64, 0:NCMP], start=True, stop=True)),
            (0, lambda e: e.matmul(pK[:, 128:128 + NCMP], lhsT=self.w2s[0:64, l, 128:256], rhs=h1[0:64, 0:NCMP], start=True, stop=True)),
            (64, lambda e: e.matmul(pK[0:NCMP, 256:320], lhsT=h1[64:128, 0:NCMP], rhs=self.w2s[64:128, l, 256:320], start=True, stop=True))], R=[h1], W=[pK])
        ce = self.cosE[:, 0:NCMP]
        se = self.sinE[:, 0:NCMP]
        P.op("dve", lambda e: e.tensor_tensor(out=kcr[:, 0:NCMP], in0=pK[:, 0:NCMP], in1=ce, op=ALU.mult), R=[pK], W=[kcr])
        P.op("dve", lambda e: e.tensor_tensor(out=kct[:, 0:NCMP], in0=pK[:, 128:128 + NCMP], in1=se, op=ALU.mult), R=[pK], W=[kct])
        P.op("dve", lambda e: e.tensor_tensor(out=kcb[0:64, 0:NCMP], in0=kcr[0:64, 0:NCMP], in1=kct[0:64, 0:NCMP], op=ALU.add), R=[kcr, kct], W=[kcb])
        P.op("act", lambda e: e.activation(out=self.vca[0:NCMP, 0:64], in_=pK[0:NCMP, 256:320], func=AF.Copy), R=[pK], W=[self.vca])
        tmo = 0
        self.load_w(l, g, 'tm')
        W97 = 65 + NSL
        selx = self.big[5]
        selb_ = selx[:, :].bitcast(BF16)
        ptb = [p_[:, :].bitcast(BF16) for p_ in self.pt]
        for n in range(NT):
            i = n % 2
            qs_ = slice(n * 128, (n + 1) * 128)
            qt_n = qtile(n)
            qbuf = self.big[n // HT]
            zt, pc, imp, selb, gs, yd, yd2, sm, oT = self.zt[i], self.pc[i], self.imp[i], self.selb[i], self.gs[i], self.yd[i], self.yd2[i], self.smb[i], self.oT[i]
            ptm = self.ps[0]
            self.proj_tm([ptm], wt, g, 'DTM', n)
            P.op("dve", lambda e: e.tensor_tensor(out=self.vaug_b[:, n, 0:2, 0:64], in0=ptm[:, 0:128].rearrange("p (h d) -> p h d", h=2),
                                                  in1=self.b_tm_s[:, tmo:tmo + 128].rearrange("p (h d) -> p h d", h=2), op=ALU.add),
                 R=[ptm, self.b_tm_s], W=[(self.vaug_b, n)])
            P.op("dve", lambda e: e.tensor_tensor(out=gs[:, 0:12], in0=ptm[:, 128:140], in1=self.b_tm_s[:, tmo + 128:tmo + 140], op=ALU.add), R=[ptm, self.b_tm_s], W=[gs])
            P.op("act", lambda e: e.activation(out=gs[:, 0:12], in_=gs[:, 0:12], func=AF.Sigmoid), R=[gs], W=[gs])
            P.op("dve", lambda e: e.tensor_tensor(out=zt[:, 0:256], in0=ptm[:, 140:396], in1=self.b_tm_s[:, tmo + 140:tmo + 396], op=ALU.add), R=[ptm, self.b_tm_s], W=[zt])
            P.op("act", lambda e: e.activation(out=zt[:, 0:256], in_=zt[:, 0:256], func=AF.Silu), R=[zt], W=[zt])
            mct = self.mct[i]
            P.dma("sp", mct[:], self.maskC_d[:, qs_], R=[self.maskC_d], W=[mct])
            pC = self.ps[1]
            self.pe_seq([(0, lambda e: e.matmul(pC[0:NCMP, :], lhsT=kcb[0:64, 0:NCMP], rhs=qt_n, start=True, stop=True))], R=[kcb, qbuf], W=[pC])
            P.op("act", lambda e: e.activation(out=pc[0:NCMP, :], in_=pC[0:NCMP, :], func=AF.Exp, scale=0.125), R=[pC], W=[pc])
            P.op("dve", lambda e: e.tensor_tensor(out=pc[0:NCMP, :].rearrange("p (h t) -> p h t", h=4), in0=pc[0:NCMP, :].rearrange("p (h t) -> p h t", h=4),
                                                  in1=mct[0:NCMP, :].unsqueeze(1).to_broadcast([NCMP, 4, 128]), op=ALU.mult), R=[pc, mct], W=[pc])
            pO = self.ps[2]
            def f(e):
                ins = None
                for h in range(4):
                    ins = e.matmul(pO[:, h * W97:(h + 1) * W97], lhsT=pc[0:NCMP, h * 128:(h + 1) * 128], rhs=self.vca[0:NCMP, 0:W97], start=True, stop=True)
                return ins
            self.pe_seq([(0, f)], R=[pc, self.vca], W=[pO])
            pOv = pO[:, 0:4 * W97].rearrange("p (h c) -> p h c", h=4)
            P.op("dve", lambda e: e.tensor_scalar(out=sm[:, 0:4], in0=pOv[:, :, 64], scalar1=1e-30, scalar2=None, op0=ALU.max), R=[pO], W=[sm])
            P.op("dve", lambda e: e.reciprocal(out=sm[:, 4:8], in_=sm[:, 0:4]), R=[sm], W=[sm])
            for h in range(4):
                if h == 0:
                    P.op("dve", lambda e: e.tensor_scalar(out=imp[:, 0:NSL], in0=pOv[:, 0, 65:W97], scalar1=sm[:, 4:5], scalar2=None, op0=ALU.mult), R=[pO, sm], W=[imp])
                else:
                    P.op("dve", lambda e, h=h: e.scalar_tensor_tensor(out=imp[:, 0:NSL], in0=pOv[:, h, 65:W97], scalar=sm[:, 4 + h:5 + h], in1=imp[:, 0:NSL],
                                                                      op0=ALU.mult, op1=ALU.add), R=[pO, sm, imp], W=[imp])
            P.op("dve", lambda e: e.tensor_tensor(out=imp[:, 0:NSL], in0=imp[:, 0:NSL], in1=self.ikeep[:, n, :], op=ALU.mult), R=[imp], W=[imp])
            P.op("dve", lambda e: e.tensor_tensor(out=imp[:, 0:NSL], in0=imp[:, 0:NSL], in1=self.iadd[:, n, :], op=ALU.add), R=[imp], W=[imp])
            P.op("dve", lambda e: e.max(out=selb[:, 0:8], in_=imp[:, 0:NSL]), R=[imp], W=[selb])
            P.op("dve", lambda e: e.tensor_reduce(out=selb[:, 8:9], in_=selb[:, 0:8], axis=AX.X, op=ALU.min), R=[selb], W=[selb])
            P.op("dve", lambda e: e.tensor_scalar(out=imp[:, 32:32 + NSL], in0=imp[:, 0:NSL], scalar1=selb[:, 8:9], scalar2=None, op0=ALU.is_ge), R=[imp, selb], W=[imp])
            gv = gs[:, 0:12].rearrange("p (h c) -> p h c", c=3)
            P.op("dve", lambda e: e.tensor_tensor(out=sm[:, 8:12], in0=sm[:, 4:8], in1=gv[:, :, 0], op=ALU.mult), R=[sm, gs], W=[sm])
            ydv = yd[:].rearrange("p (h d) -> p h d", h=4)
            yd2v = yd2[:].rearrange("p (h d) -> p h d", h=4)
            P.op("dve", lambda e: e.tensor_tensor(out=ydv, in0=pOv[:, :, 0:64], in1=sm[:, 8:12].unsqueeze(2).to_broadcast([128, 4, 64]), op=ALU.mult), R=[pO, sm], W=[yd])
            nb_ = 2 * (n + 1)
            P.op("dve", lambda e: e.tensor_copy(out=selb_[:, 0:nb_ * 64].rearrange("p (j s) -> p j s", s=64),
                                                 in_=imp[:, 32:32 + nb_].unsqueeze(2).to_broadcast([128, nb_, 64])), R=[imp], W=[selx])
            for br in range(2):
                kts = list(range(n + 1)) if br == 0 else [kt for kt in range(n - 4, n + 1) if kt >= 0]
                koff = 0 if br == 0 else T
                pOT = self.ps[6 + br]
                for kt in kts:
                    ks_ = slice(kt * 128, (kt + 1) * 128)
                    pS = self.ps[3 + (self.cnt % 2)]; pti = self.cnt % 3; self.cnt += 1
                    pt, ptv = self.pt[pti], ptb[pti][:, 0:512]
                    self.pe_seq([(0, lambda e, pS=pS, kt=kt, koff=koff: e.matmul(pS[:], lhsT=kb[0:64, koff + kt * 128:koff + (kt + 1) * 128], rhs=qt_n, start=True, stop=True))],
                                R=[self.big[2], qbuf], W=[pS])
                    if br == 0:
                        pM = self.ps[5]
                        P.op("pe", lambda e, kt=kt: e.matmul(pM[:, 0:128], lhsT=selb_[:, kt * 128:(kt + 1) * 128], rhs=self.ident_bf[:], start=True, stop=True), R=[selx], W=[pM])
                    P.op("act", lambda e, pS=pS, ptv=ptv: e.activation(out=ptv, in_=pS[:], func=AF.Exp, scale=0.125), R=[pS], W=[pt])
                    if br == 0:
                        P.op("dve", lambda e, ptv=ptv: e.tensor_tensor(out=ptv.rearrange("p (h t) -> p h t", h=4), in0=ptv.rearrange("p (h t) -> p h t", h=4),
                                                                     in1=pM[:, 0:128].unsqueeze(1).to_broadcast([128, 4, 128]), op=ALU.mult), R=[pt, pM], W=[pt])
                    if kt == n:
                        P.op("dve", lambda e, ptv=ptv: e.tensor_tensor(out=ptv, in0=ptv, in1=self.tri_ge[:], op=ALU.mult), R=[pt], W=[pt])
                    elif br == 1 and kt == n - 4:
                        P.op("dve", lambda e, ptv=ptv: e.tensor_tensor(out=ptv, in0=ptv, in1=self.tri_lt[:], op=ALU.mult), R=[pt], W=[pt])
                    P.op("pe", lambda e, ptv=ptv, kt=kt, br=br, pOT=pOT, kts=kts: e.matmul(pOT[0:65, :], lhsT=self.vaug_b[:, kt, br, :], rhs=ptv, start=(kt == kts[0]), stop=(kt == kts[-1])),
                         R=[pt, (self.vaug_b, kt)], W=[pOT])
                P.op("act", lambda e, pOT=pOT: e.activation(out=oT[:], in_=pOT[0:65, :], func=AF.Copy), R=[pOT], W=[oT])
                pOs = self.ps[5]
                def f(e):
                    ins = None
                    for h in range(4):
                        ins = e.transpose(out=pOs[:, h * 65:(h + 1) * 65], in_=oT[0:65, h * 128:(h + 1) * 128], identity=self.ident[0:65, 0:65])
                    return ins
                self.pe_seq([(0, f)], R=[oT], W=[pOs])
                pOsv = pOs[:, 0:260].rearrange("p (h c) -> p h c", h=4)
                o_ = 12 + 4 * br
                P.op("dve", lambda e, o_=o_: e.reciprocal(out=sm[:, o_:o_ + 4], in_=pOsv[:, :, 64]), R=[pOs], W=[sm])
                P.op("dve", lambda e, o_=o_, br=br: e.tensor_tensor(out=sm[:, o_:o_ + 4], in0=sm[:, o_:o_ + 4], in1=gv[:, :, 1 + br], op=ALU.mult), R=[sm, gs], W=[sm])
                P.op("dve", lambda e, o_=o_: e.tensor_tensor(out=yd2v, in0=pOsv[:, :, 0:64], in1=sm[:, o_:o_ + 4].unsqueeze(2).to_broadcast([128, 4, 64]), op=ALU.mult), R=[pOs, sm], W=[yd2])
                P.op("pool", lambda e: e.tensor_tensor(out=yd[:], in0=yd[:], in1=yd2[:], op=ALU.add), R=[yd, yd2], W=[yd])
            P.op("pool", lambda e: e.tensor_tensor(out=yd[:], in0=yd[:], in1=zt[:, 0:256], op=ALU.mult), R=[yd, zt], W=[yd])
            self.emit_y(yd, n, 6)

    def layer(self, b, l):
        P, T, NT = self.P, self.T, self.NT
        P.epoch(b * self.NL + l)
        for g in GL:
            if g in self.mixers:
                w = self.load_w(l, g, 'fm')
                getattr(self, "mixer_" + g)(b, l, w)
            else:
                gi = GL.index(g)
                for n in range(NT):
                    P.dma("sp", self.ybuf[n * 128:(n + 1) * 128, gi * 256:(gi + 1) * 256], self.zero256[:], W=[(self.ybuf, n * 4 + gi)])
        self.outproj(b, l)

    def outproj(self, b, l):
        P, T, NT = self.P, self.T, self.NT
        wt = self.wt[0]
        P.dma("sp", wt[:, :, 0:D], self.wout_bf[l].rearrange("(mc p) c -> p mc c", p=128), R=[(self.wout_bf, l)], W=[wt])
        P.dma("sp", self.lnbc[:, 0:2 * D].rearrange("p (a d) -> p a d", a=2), self.lng_d[l].partition_broadcast(128), R=[self.lng_d], W=[self.lnbc])
        last = (l == self.NL - 1)
        for n in range(NT):
            i = n % 2
            yt, xt = self.yt[i], self.xt[i]
            P.dma("sp", yt[:, 0:D], self.ybuf[n * 128:(n + 1) * 128, :], R=[(self.ybuf, range(n * 4, n * 4 + 4))], W=[yt])
            P.dma("sp", xt[:, 0:D], self.hbuf[n * 128:(n + 1) * 128, :], R=[(self.hbuf, n)], W=[xt])
            yTt = self.yTt[i]
            for half in range(2):
                ps = self.ps[half]
                def f(e, half=half, ps=ps):
                    ins = None
                    for j in range(4):
                        mc = half * 4 + j
                        ins = e.transpose(out=ps[:, j * 128:(j + 1) * 128], in_=yt[:, mc * 128:(mc + 1) * 128], identity=self.ident[:])
                    return ins
                P.op("pe", f, R=[yt], W=[ps])
                P.op("act", lambda e, half=half, ps=ps: e.activation(out=yTt[:, half * 4:(half + 1) * 4, :], in_=ps[:].rearrange("p (j t) -> p j t", j=4), func=AF.Copy),
                     R=[ps], W=[yTt])
            for half in range(2):
                ps = self.ps[2 + half]
                def f(e, half=half, ps=ps):
                    ins = None
                    for mc in range(8):
                        ins = e.matmul(ps[:], lhsT=yTt[:, mc, :], rhs=wt[:, mc, half * 512:(half + 1) * 512], start=(mc == 0), stop=(mc == 7))
                    return ins
                P.op("pe", f, R=[yTt, wt], W=[ps])
                P.op("dve", lambda e, half=half, ps=ps: e.scalar_tensor_tensor(out=xt[:, half * 512:(half + 1) * 512], in0=xt[:, half * 512:(half + 1) * 512],
                                                                              scalar=DN_ALPHA, in1=ps[:], op0=ALU.mult, op1=ALU.add), R=[xt, ps], W=[xt])
            self.layernorm(xt, i)
            if last:
                tok = P.dma("sp", self.out[b, n * 128:(n + 1) * 128, :], xt[:, 0:D], R=[xt], W=[self.out])
                self.out_toks.append(tok)
            else:
                P.dma("sp", self.hbuf[n * 128:(n + 1) * 128, :], xt[:, 0:D], R=[xt], W=[(self.hbuf, n)])
                self.to_hT(xt, n)


_CACHE = {}


def kernel(x, ln0_g, ln0_b, w_in, b_in, a_sinks, b_conv_w, b_conv_b, c_lb, c_norm_g, d_cmp_pe, d_cmp_w1, d_cmp_w2, w_out, ln_g, ln_b):
    from concourse.bass_utils import run_bass_kernel_spmd
    f32 = np.float32
    full = dict(x=np.asarray(x, f32), ln0_g=np.asarray(ln0_g, f32), ln0_b=np.asarray(ln0_b, f32), w_in=np.asarray(w_in, f32), b_in=np.asarray(b_in, f32),
                a_sinks=np.asarray(a_sinks, f32), b_conv_w=np.asarray(b_conv_w, f32), b_conv_b=np.asarray(b_conv_b, f32), c_lb=np.asarray(c_lb, f32),
                c_norm_g=np.asarray(c_norm_g, f32), d_cmp_pe=np.asarray(d_cmp_pe, f32), d_cmp_w1=np.asarray(d_cmp_w1, f32), d_cmp_w2=np.asarray(d_cmp_w2, f32),
                w_out=np.asarray(w_out, f32), ln_g=np.asarray(ln_g, f32), ln_b=np.asarray(ln_b, f32))
    B, T, _ = full['x'].shape
    NCORE = 8
    NB = B // NCORE
    NL = full['w_in'].shape[0]
    hc = host_consts(T)
    w_r, b_fm, b_tm, b_if = host_weights(full['w_in'], full['b_in'])
    common = dict(ln0=np.stack([full['ln0_g'], full['ln0_b']]), lng=np.stack([full['ln_g'], full['ln_b']], 1),
                  w_in=w_r, b_fm=b_fm, b_tm=b_tm, b_if=b_if, a_sinks=full['a_sinks'], w_out=full['w_out'], **hc)
    common.update(extra_inputs(full, T))
    k = K(T=T, NB=NB, NL=NL)
    k.build()
    common = {n: np.ascontiguousarray(common[n]) for n in k.in_names if n != 'x'}
    in_maps = []
    for c in range(NCORE):
        m = dict(common)
        m['x'] = np.ascontiguousarray(full['x'][c * NB:(c + 1) * NB])
        in_maps.append(m)
    res = run_bass_kernel_spmd(k.nc, in_maps, core_ids=list(range(NCORE)))
    out = np.concatenate([np.asarray(r['out']) for r in res.results], axis=0)
    return out.astype(np.float32)
```

```python
import contextlib
import numpy as np
import concourse.bass as bass
import concourse.mybir as mybir

F32 = mybir.dt.float32
BF16 = mybir.dt.bfloat16
AF = mybir.ActivationFunctionType
ALU = mybir.AluOpType
AX = mybir.AxisListType


class Buf:
    def __init__(self, name, t, slots=1):
        self.name = name
        self.t = t
        self.slots = slots

    def keys(self, slot=None):
        if slot is None:
            return [(self.name, s) for s in range(self.slots)]
        if isinstance(slot, (list, tuple, range)):
            return [(self.name, s) for s in slot]
        return [(self.name, slot)]

    def __getitem__(self, idx):
        return self.t[idx]


class Prog:
    NDMA = 4
    NPOOL = 3

    def __init__(self, nc, nepoch=1):
        self.nc = nc
        self.es = contextlib.ExitStack()
        self.eng = {"pe": nc.tensor, "act": nc.scalar, "dve": nc.vector, "pool": nc.gpsimd, "sp": nc.sync}
        self.esem = {}
        self.ecnt = {}
        self.seen = {}
        self.last_tok = {}
        self.finals = []
        self.esem_sets = []
        self.dsem_sets = []
        self.dpool_sets = []
        for ep in range(nepoch):
            self.esem_sets.append({k: self.es.enter_context(nc.semaphore("sem_%s_%d" % (k, ep))) for k in ("pe", "act", "dve", "pool")})
            self.dsem_sets.append([self.es.enter_context(nc.semaphore("dsem_sp%d_%d" % (i, ep))) for i in range(self.NDMA)])
            self.dpool_sets.append([self.es.enter_context(nc.semaphore("dsem_pl%d_%d" % (i, ep))) for i in range(self.NPOOL)])
        for k in self.eng:
            self.ecnt[k] = 0
            self.seen[k] = {}
            self.last_tok[k] = None
        self.esem = dict(self.esem_sets[0])
        self.dsem = {"sp": self.dsem_sets[0]}
        self.dval = {"sp": [0] * self.NDMA}
        self.dnext = {"sp": 0}
        self.dsem["pool"] = self.dpool_sets[0]
        self.dval["pool"] = [0] * self.NPOOL
        self.dnext["pool"] = 0
        self.cur_epoch = 0
        self.state = {}
        self.nbuf = 0
        self.excl = set()
        self.stop_ops = None
        self.ninstr = 0

    def epoch(self, ep):
        if ep == self.cur_epoch:
            return
        for k in ("pe", "act", "dve", "pool"):
            if self.ecnt[k] > 0:
                self.finals.append((self.esem[k], self.ecnt[k], k))
            self.esem[k] = self.esem_sets[ep][k]
            self.ecnt[k] = 0
        for j in range(self.NDMA):
            if self.dval["sp"][j] > 0:
                self.finals.append((self.dsem["sp"][j], self.dval["sp"][j], "dma"))
        self.dsem["sp"] = self.dsem_sets[ep]
        self.dval["sp"] = [0] * self.NDMA
        self.dnext["sp"] = 0
        for j in range(self.NPOOL):
            if self.dval["pool"][j] > 0:
                self.finals.append((self.dsem["pool"][j], self.dval["pool"][j], "dma"))
        self.dsem["pool"] = self.dpool_sets[ep]
        self.dval["pool"] = [0] * self.NPOOL
        self.dnext["pool"] = 0
        self.cur_epoch = ep

    def sbuf(self, name, shape, dtype=F32, slots=1):
        t = self.es.enter_context(self.nc.sbuf_tensor(name, list(shape), dtype))
        return Buf(name, t, slots)

    def psum(self, name, shape, dtype=F32, slots=1):
        t = self.es.enter_context(self.nc.psum_tensor(name, list(shape), dtype))
        self.excl.add(name)
        return Buf(name, t, slots)

    def dram(self, name, shape, dtype=F32, kind="Internal", slots=1):
        t = self.nc.dram_tensor(name, list(shape), dtype, kind=kind)
        return Buf(name, t.ap(), slots)

    def _keys(self, lst):
        out = []
        for x in lst:
            if isinstance(x, Buf):
                out += x.keys()
            elif isinstance(x, tuple) and isinstance(x[0], Buf):
                out += x[0].keys(x[1])
            else:
                raise TypeError(x)
        return out

    def _wait(self, e, tok):
        sem, val, owner = tok
        sid = id(sem)
        if self.seen[e].get(sid, 0) >= val:
            return
        self.eng[e].wait_ge(sem, val)
        self.seen[e][sid] = val

    def _deps(self, e, R, W):
        toks = []
        for k in R:
            st = self.state.get(k)
            if st and st[0] is not None:
                toks.append(st[0])
        for k in W:
            st = self.state.get(k)
            if st:
                if st[0] is not None and (st[0][2] != e or e != 'pe'):
                    toks.append(st[0])
                for re_, tok in st[1].items():
                    if re_ != e or e != 'pe':
                        toks.append(tok)
        for tok in toks:
            self._wait(e, tok)

    def _commit(self, e, tok, R, W):
        for k in R:
            st = self.state.setdefault(k, [None, {}])
            st[1][e] = tok
        for k in W:
            self.state[k] = [tok, {}]

    def op(self, e, fn, R=(), W=(), fence=False):
        Rk, Wk = self._keys(R), self._keys(W)
        Wk = Wk + [k_ for k_ in Rk if k_[0] in self.excl]
        Rk = [k_ for k_ in Rk if k_[0] not in self.excl]
        self._deps(e, Rk, Wk)
        if fence and self.last_tok[e] is not None:
            self._wait(e, self.last_tok[e])
        if self.stop_ops is not None and self.ninstr >= self.stop_ops:
            raise StopIteration("stop_ops")
        ins = fn(self.eng[e])
        self.ecnt[e] += 1
        ins.then_inc(self.esem[e], 1)
        tok = (self.esem[e], self.ecnt[e], e)
        self.last_tok[e] = tok
        self._commit(e, tok, Rk, Wk)
        self.ninstr += 1
        return tok

    def dma(self, q, out, in_, R=(), W=(), **kw):
        Rk, Wk = self._keys(R), self._keys(W)
        self._deps(q, Rk, Wk)
        j = self.dnext[q]
        self.dnext[q] = (j + 1) % len(self.dsem[q])
        sem = self.dsem[q][j]
        if self.dval[q][j] > 0:
            self._wait(q, (sem, self.dval[q][j], "dma"))
        ins = self.eng[q].dma_start(out=out, in_=in_, **kw)
        self.dval[q][j] += 16
        ins.then_inc(sem, 16)
        tok = (sem, self.dval[q][j], "dma_" + q)
        self._commit("dma_" + q, tok, Rk, Wk)
        self.ninstr += 1
        return tok

    def wait_tok(self, e, tok):
        self._wait(e, tok)

    def finish(self, toks):
        for tok in toks:
            self._wait("sp", tok)
        for tok in self.finals:
            self._wait("sp", tok)
        for e in ("pe", "act", "dve", "pool"):
            if self.ecnt[e] > 0:
                self._wait("sp", (self.esem[e], self.ecnt[e], e))
        for q in ("sp", "pool"):
            for j in range(len(self.dsem[q])):
                if self.dval[q][j] > 0:
                    self._wait("sp", (self.dsem[q][j], self.dval[q][j], "dma"))


import numpy as np
import concourse.bass as bass
import concourse.mybir as mybir

D = 1024
HD = 64
LN_EPS = 1e-5
DN_ALPHA = (2.0 * 2) ** 0.25

OFF = {}
_o = 0
for _n, _s in (('a_q', 256), ('a_k', 128), ('a_v', 128), ('a_z', 256), ('b_qk', 512), ('b_v', 256), ('b_if', 8),
               ('b_o', 256), ('b_z', 256), ('c_q', 256), ('c_f', 256), ('c_i', 256), ('c_z', 256), ('d_q', 256),
               ('d_kv', 384), ('d_g', 12), ('d_z', 256)):
    OFF[_n] = _o
    _o += _s
NCOLS = _o


def _head(base, h):
    return np.arange(base + h * 64, base + (h + 1) * 64)


def _swap(c):
    return np.concatenate([c[32:], c[:32]])


def col_layout():
    L = {}
    idx = []

    def add(name, cols):
        L[name] = (sum(len(x) for x in idx), len(cols))
        idx.append(np.asarray(cols))

    aq, ak = OFF['a_q'], OFF['a_k']
    c = np.concatenate([_head(aq, 0), _head(aq, 2)]); add('AQ0', c); add('AQ0s', np.concatenate([_swap(_head(aq, 0)), _swap(_head(aq, 2))]))
    add('AQ1', np.concatenate([_head(aq, 1), _head(aq, 3)])); add('AQ1s', np.concatenate([_swap(_head(aq, 1)), _swap(_head(aq, 3))]))
    add('AK', np.concatenate([_head(ak, 0), _head(ak, 1)])); add('AKs', np.concatenate([_swap(_head(ak, 0)), _swap(_head(ak, 1))]))
    add('ATM', np.concatenate([np.arange(OFF['a_v'], OFF['a_v'] + 128), np.arange(OFF['a_z'], OFF['a_z'] + 256)]))
    bq = OFF['b_qk']
    add('BQ0', np.arange(bq, bq + 128)); add('BQ1', np.arange(bq + 128, bq + 256))
    add('BK0', np.arange(bq + 256, bq + 384)); add('BK1', np.arange(bq + 384, bq + 512))
    add('BI', np.arange(OFF['b_if'], OFF['b_if'] + 4)); add('BF', np.arange(OFF['b_if'] + 4, OFF['b_if'] + 8))
    add('BTM', np.concatenate([np.arange(OFF['b_v'], OFF['b_v'] + 256), np.arange(OFF['b_o'], OFF['b_o'] + 256),
                               np.arange(OFF['b_z'], OFF['b_z'] + 256)]))
    add('CQ0', np.arange(OFF['c_q'], OFF['c_q'] + 128)); add('CQ1', np.arange(OFF['c_q'] + 128, OFF['c_q'] + 256))
    add('CF0', np.arange(OFF['c_f'], OFF['c_f'] + 128)); add('CF1', np.arange(OFF['c_f'] + 128, OFF['c_f'] + 256))
    add('CTM', np.concatenate([np.arange(OFF['c_i'], OFF['c_i'] + 256), np.arange(OFF['c_z'], OFF['c_z'] + 256)]))
    dq, dk = OFF['d_q'], OFF['d_kv']
    add('DQ0', _head(dq, 0)); add('DQ0s', _swap(_head(dq, 0)))
    for h in range(1, 4):
        add('DH%d' % h, _head(dq, h)); add('DH%ds' % h, _swap(_head(dq, h)))
    add('DKCV', np.concatenate([_head(dk, 0), _head(dk, 1)]))
    add('DKS', _head(dk, 2)); add('DKSs', _swap(_head(dk, 2)))
    add('DKW', _head(dk, 4)); add('DKWs', _swap(_head(dk, 4)))
    add('DTM', np.concatenate([_head(dk, 3), _head(dk, 5), np.arange(OFF['d_g'], OFF['d_g'] + 12),
                               np.arange(OFF['d_z'], OFF['d_z'] + 256)]))
    return L, np.concatenate(idx)


LAY, GIDX = col_layout()
NCR = len(GIDX)
GROUPS = {
    'A': ('AQ0', 'ATM'),
    'B': ('BQ0', 'BTM'),
    'C': ('CQ0', 'CTM'),
    'D': ('DQ0', 'DTM'),
}


def grange(g):
    a, b = GROUPS[g]
    s = LAY[a][0]
    e = LAY[b][0] + LAY[b][1]
    return s, e


FM = ['AQ0', 'AQ0s', 'AQ1', 'AQ1s', 'AK', 'AKs', 'BQ0', 'BQ1', 'BK0', 'BK1', 'CQ0', 'CQ1', 'CF0', 'CF1',
      'DQ0', 'DQ0s', 'DH1', 'DH1s', 'DH2', 'DH2s', 'DH3', 'DH3s', 'DKCV', 'DKS', 'DKSs', 'DKW', 'DKWs']
FMI = {n: i for i, n in enumerate(FM)}
TM = ['ATM', 'BTM', 'CTM', 'DTM']
TMO = {}
_o = 0
for _n in TM:
    TMO[_n] = _o
    _o += LAY[_n][1]
NTM = _o
GL = ['A', 'B', 'C', 'D']
WMAX = max(grange(g)[1] - grange(g)[0] for g in GL)


def host_weights(w_in, b_in):
    w_r = np.ascontiguousarray(w_in[:, :, GIDX])
    b_r = b_in[:, GIDX]
    NL = w_in.shape[0]
    b_fm = np.zeros((NL, 128, len(FM)), np.float32)
    for i, n in enumerate(FM):
        s0, c = LAY[n]
        b_fm[:, :c, i] = b_r[:, s0:s0 + c]
    b_tm = np.concatenate([b_r[:, LAY[n][0]:LAY[n][0] + LAY[n][1]] for n in TM], axis=1)
    b_if = np.stack([b_r[:, LAY['BI'][0]:LAY['BI'][0] + 4], b_r[:, LAY['BF'][0]:LAY['BF'][0] + 4]], axis=-1)
    return w_r, np.ascontiguousarray(b_fm), np.ascontiguousarray(b_tm), np.ascontiguousarray(b_if)


def host_consts(T):
    half = 32
    inv = (10000.0 ** (-np.arange(half, dtype=np.float32) / half)).astype(np.float32)
    pos = np.arange(T, dtype=np.float32)
    ang = pos[None, :] * inv[:, None]
    cos = np.cos(ang).astype(np.float32)
    sin = np.sin(ang).astype(np.float32)
    cos64 = np.concatenate([cos, cos], 0)
    sin64 = np.concatenate([-sin, sin], 0)
    cosT = np.concatenate([cos64, cos64], 0)
    sinT = np.concatenate([sin64, sin64], 0)
    ident = np.eye(128, dtype=np.float32)
    p = np.arange(128)[:, None]
    f = np.arange(128)[None, :]
    tri_ge = (f >= p).astype(np.float32)
    tri_lt = (f < p).astype(np.float32)
    tri_ge = np.tile(tri_ge, (1, 4)); tri_lt = np.tile(tri_lt, (1, 4))
    NCMP = (T - 32) // 16 + 1
    cosE = np.zeros((128, 128), np.float32); sinE = np.zeros((128, 128), np.float32)
    cosE[:, :NCMP] = cosT[:, 31::16][:, :NCMP]; sinE[:, :NCMP] = sinT[:, 31::16][:, :NCMP]
    return dict(cosT=cosT, sinT=sinT, ident=ident, tri_ge=tri_ge, tri_lt=tri_lt, cosE=cosE, sinE=sinE)


def extra_inputs(full, T):
    NL = full['w_in'].shape[0]
    cw = full['b_conv_w'].reshape(NL, 4, 4, 128).transpose(0, 3, 2, 1)
    cb = full['b_conv_b'].reshape(NL, 4, 128).transpose(0, 2, 1)
    sel = np.zeros((4, 4, 128), np.float32)
    for h in range(4):
        sel[h, h, :] = 1.0
    clb = full['c_lb'].reshape(NL, 2, 128).transpose(2, 1, 0)
    rmask = np.ones((128, T), np.float32); rmask[:, ::64] = 0.0
    NCMP = (T - 32) // 16 + 1
    NSL = T // 64
    NT = T // 128
    w1 = full['d_cmp_w1'].reshape(NL, 2, 32, 64, 64).transpose(0, 1, 3, 2, 4).reshape(NL, 128, 32 * 64)
    w2 = full['d_cmp_w2']
    w2s = np.zeros((NL, 128, 320), np.float32)
    sw = np.concatenate([np.arange(32, 64), np.arange(0, 32)])
    w2s[:, 0:64, 0:64] = w2[:, 0]; w2s[:, 0:64, 64:128] = w2[:, 0]
    w2s[:, 0:64, 128:192] = w2[:, 0][:, :, sw]; w2s[:, 0:64, 192:256] = w2[:, 0][:, :, sw]
    w2s[:, 64:128, 256:320] = w2[:, 1]
    peT = full['d_cmp_pe'].transpose(0, 1, 3, 2).reshape(NL, 128, 32)
    starts = np.arange(NCMP) * 16
    blk = np.arange(NSL)
    overlap = ((starts[:, None] < (blk[None, :] + 1) * 64) & (starts[:, None] + 32 > blk[None, :] * 64)).astype(np.float32)
    vca0 = np.zeros((128, 65 + NSL), np.float32)
    vca0[:NCMP, 64] = 1.0
    vca0[:NCMP, 65:] = overlap
    t = np.arange(T)
    maskC = np.zeros((128, T), np.float32)
    maskC[:NCMP] = ((starts + 31)[:, None] <= t[None, :]).astype(np.float32)
    cur = (t // 64)[:, None]
    forced = (blk[None, :] == 0) | (blk[None, :] == cur) | (blk[None, :] == cur - 1)
    valid = blk[None, :] <= cur
    keep = (valid & ~forced).astype(np.float32)
    add = np.where(valid, np.where(forced, 1e6, 0.0), -1e30).astype(np.float32)
    keep = keep.reshape(NT, 128, NSL).transpose(1, 0, 2)
    add = add.reshape(NT, 128, NSL).transpose(1, 0, 2)
    return dict(cw=np.ascontiguousarray(cw), cb=np.ascontiguousarray(cb), sel=sel, clb=np.ascontiguousarray(clb), rmask=rmask,
                c_norm_g=full['c_norm_g'], w1s=np.ascontiguousarray(w1), w2s=w2s, peT=np.ascontiguousarray(peT), vca0=vca0, maskC=maskC,
                ikeep=np.ascontiguousarray(keep), iadd=np.ascontiguousarray(add))


class StopBuild(Exception):
    pass


class K:
    stop = None

    def chk(self, tag):
        if self.stop == tag:
            raise StopBuild(tag)


    def __init__(self, T=2048, NB=4, NL=2, dbg=(), mixers="ABCD"):
        self.mixers = mixers
        self.T, self.NB, self.NL = T, NB, NL
        self.NT = T // 128
        self.dbg = set(dbg)
        self.dbg_out = {}
        nc = bass.Bass("TRN2", target_bir_lowering=False)
        self.nc = nc
        self.P = Prog(nc, nepoch=NB * NL)
        self.out_toks = []
        self.in_names = []
        self.last_kb = None
        self.ctoks = []

    def const_barrier(self):
        for e in ("pe", "act", "dve", "pool"):
            for tok in self.ctoks:
                self.P.wait_tok(e, tok)
        self.ctoks = []

    def pe_seq(self, specs, R, W):
        P = self.P
        groups = []
        for kb, fn in specs:
            if groups and (kb is None or groups[-1][0] is None or groups[-1][0] == kb):
                if groups[-1][0] is None:
                    groups[-1][0] = kb
                groups[-1][1].append(fn)
            else:
                groups.append([kb, [fn]])
        for gi, (kb, fns) in enumerate(groups):
            def f(e, fns=fns):
                ins = None
                for fn in fns:
                    ins = fn(e)
                return ins
            fence = (kb is not None and self.last_kb is not None and kb != self.last_kb)
            P.op("pe", f, R=R, W=W, fence=fence)
            if kb is not None:
                self.last_kb = kb

    def din(self, name, shape, dtype=F32):
        self.in_names.append(name)
        return self.P.dram(name, shape, dtype, kind="ExternalInput")

    def dump(self, name, buf, ap, shape, R=None):
        if name not in self.dbg:
            return
        P = self.P
        d = P.dram("dbg_" + name, shape, F32, kind="ExternalOutput")
        tok = P.dma("pool", d[:], ap, R=[buf] if R is None else R, W=[d])
        self.out_toks.append(tok)

    def build(self):
        P, nc, T, NT = self.P, self.nc, self.T, self.NT
        NB, NL = self.NB, self.NL
        x = self.din("x", [NB, T, D])
        out = P.dram("out", [NB, T, D], F32, kind="ExternalOutput")
        w_in = self.din("w_in", [NL, D, NCR])
        ln0 = self.din("ln0", [2, D])
        lng = self.din("lng", [NL, 2, D])
        cosT_d = self.din("cosT", [128, T]); sinT_d = self.din("sinT", [128, T])
        ident_d = self.din("ident", [128, 128]); trige_d = self.din("tri_ge", [128, 512]); trilt_d = self.din("tri_lt", [128, 512])
        b_fm_d = self.din("b_fm", [NL, 128, len(FM)]); b_tm_d = self.din("b_tm", [NL, NTM]); b_if_d = self.din("b_if", [NL, 4, 2])
        self.w_bf = P.dram("w_bf", [NL, D, NCR], BF16, slots=NL * 4)
        sinks_d = self.din("a_sinks", [NL, 4])
        hbuf = P.dram("hbuf", [T, D], F32, slots=NT)
        self.x, self.out, self.w_in, self.hbuf = x, out, w_in, hbuf
        self.sinks_d = sinks_d
        self.cosT_d, self.sinT_d = cosT_d, sinT_d
        cosE_d = self.din("cosE", [128, 128]); sinE_d = self.din("sinE", [128, 128])
        self.cosE = P.sbuf("cosE_s", [128, 128]); self.sinE = P.sbuf("sinE_s", [128, 128])
        self.cst = [P.sbuf("cst%d" % i, [128, 2, 512]) for i in range(2)]
        self.ident = P.sbuf("ident_s", [128, 128]); self.tri_ge = P.sbuf("trige_s", [128, 512]); self.tri_lt = P.sbuf("trilt_s", [128, 512])
        for sb, dr in ((self.cosE, cosE_d), (self.sinE, sinE_d), (self.ident, ident_d), (self.tri_ge, trige_d), (self.tri_lt, trilt_d)):
            self.ctoks.append(P.dma("sp", sb[:], dr[:], R=[dr], W=[sb]))
        self.ln0_d = ln0; self.lng_d = lng
        self.b_fm = P.sbuf("b_fm_s", [128, NL, len(FM)])
        self.ctoks.append(P.dma("sp", self.b_fm[:], b_fm_d[:].rearrange("l p f -> p l f"), R=[b_fm_d], W=[self.b_fm]))
        self.b_tm_d = b_tm_d
        self.b_tm_s = P.sbuf("b_tm_s", [128, 768])
        self.b_if = P.sbuf("b_if_s", [4, NL, 2])
        self.ctoks.append(P.dma("sp", self.b_if[:], b_if_d[:].rearrange("l p f -> p l f"), R=[b_if_d], W=[self.b_if]))
        cw_d = self.din("cw", [NL, 128, 4, 4]); cb_d = self.din("cb", [NL, 128, 4]); sel_d = self.din("sel", [4, 4, 128])
        self.cw = P.sbuf("cw_s", [128, NL, 4, 4]); self.cb = P.sbuf("cb_s", [128, NL, 4]); self.sel = P.sbuf("sel_s", [4, 4, 128])
        self.ctoks.append(P.dma("sp", self.cw[:], cw_d[:].rearrange("l p j t -> p l j t"), R=[cw_d], W=[self.cw]))
        self.ctoks.append(P.dma("sp", self.cb[:], cb_d[:].rearrange("l p j -> p l j"), R=[cb_d], W=[self.cb]))
        self.ctoks.append(P.dma("sp", self.sel[:], sel_d[:], R=[sel_d], W=[self.sel]))
        clb_d = self.din("clb", [128, 2, NL]); cng_d = self.din("c_norm_g", [NL, 256])
        self.clb = P.sbuf("clb_s", [128, 2, NL]); self.gbc = P.sbuf("gbc", [128, NL, 256])
        self.ctoks.append(P.dma("sp", self.clb[:], clb_d[:], R=[clb_d], W=[self.clb]))
        self.ctoks.append(P.dma("sp", self.gbc[:], cng_d[:].partition_broadcast(128), R=[cng_d], W=[self.gbc]))
        self.NCMP = (T - 32) // 16 + 1
        self.NSL = T // 64
        NSL = self.NSL
        self.w1_d = self.din("w1s", [NL, 128, 2048]); w2_d = self.din("w2s", [NL, 128, 320]); pe_d = self.din("peT", [NL, 128, 32])
        vca_d = self.din("vca0", [128, 65 + NSL]); mc_d = self.din("maskC", [128, T]); ik_d = self.din("ikeep", [128, NT, NSL]); ia_d = self.din("iadd", [128, NT, NSL])
        self.w2s = P.sbuf("w2s_s", [128, NL, 320]); self.peT = P.sbuf("peT_s", [128, NL, 32]); self.vca = P.sbuf("vca", [128, 65 + NSL])
        self.maskC_d = mc_d; self.mct = [P.sbuf("mct%d" % i, [128, 128]) for i in range(2)]; self.ikeep = P.sbuf("ikeep_s", [128, NT, NSL]); self.iadd = P.sbuf("iadd_s", [128, NT, NSL])
        self.ctoks.append(P.dma("sp", self.w2s[:], w2_d[:].rearrange("l p f -> p l f"), R=[w2_d], W=[self.w2s]))
        self.ctoks.append(P.dma("sp", self.peT[:], pe_d[:].rearrange("l p f -> p l f"), R=[pe_d], W=[self.peT]))
        for sb, dr in ((self.vca, vca_d), (self.ikeep, ik_d), (self.iadd, ia_d)):
            self.ctoks.append(P.dma("sp", sb[:], dr[:], R=[dr], W=[sb]))
        self.esink = P.sbuf("esink", [128, NL, 4])
        self.ctoks.append(P.dma("sp", self.esink[:], sinks_d[:].partition_broadcast(128), R=[sinks_d], W=[self.esink]))
        for l in range(NL):
            for gi, g in enumerate(GL):
                s0, e0 = grange(g)
                P.dma("pool", self.w_bf[l, :, s0:e0], w_in[l, :, s0:e0], R=[w_in], W=[(self.w_bf, l * 4 + gi)])
        self.wt = [P.sbuf("wt%d" % i, [128, 8, 1152], BF16) for i in range(1)]
        wout = self.din("w_out", [NL, D, D])
        self.wout_bf = P.dram("wout_bf", [NL, D, D], BF16, slots=NL)
        for l in range(NL):
            P.dma("pool", self.wout_bf[l], wout[l], R=[wout], W=[(self.wout_bf, l)])
        self.wsel = 0
        self.hT = P.sbuf("hT", [128, 8, T], BF16, slots=NT)
        self.ybuf = P.dram("ybuf", [T, D], F32, slots=NT * 4)
        self.ps = [P.psum("ps%d" % i, [128, 512]) for i in range(8)]
        self.stat = [P.sbuf("stat%d" % i, [128, 2, 6]) for i in range(2)]
        self.mv = [P.sbuf("mv%d" % i, [128, 4]) for i in range(2)]
        self.const_barrier()
        try:
            for b in range(NB):
                self.seq(b)
        except (StopBuild, StopIteration):
            pass
        P.finish(self.out_toks)

    def layernorm(self, xt, i):
        P = self.P
        st, mv = self.stat[i], self.mv[i]
        xv = xt[:, 0:D]
        for c in range(2):
            P.op("dve", lambda e, c=c: e.bn_stats(out=st[:, c, :], in_=xt[:, c * 512:(c + 1) * 512]), R=[xt], W=[st])
        P.op("dve", lambda e: e.bn_aggr(out=mv[:, 0:2], in_=st[:]), R=[st], W=[mv])
        P.op("dve", lambda e: e.tensor_scalar(out=mv[:, 2:3], in0=mv[:, 1:2], scalar1=LN_EPS, scalar2=None, op0=ALU.add), R=[mv], W=[mv])
        P.op("act", lambda e: e.activation(out=mv[:, 2:3], in_=mv[:, 2:3], func=AF.Sqrt), R=[mv], W=[mv])
        P.op("dve", lambda e: e.reciprocal(out=mv[:, 3:4], in_=mv[:, 2:3]), R=[mv], W=[mv])
        P.op("dve", lambda e: e.tensor_scalar(out=xv, in0=xv, scalar1=mv[:, 0:1], scalar2=mv[:, 3:4], op0=ALU.subtract, op1=ALU.mult), R=[xt, mv], W=[xt])
        P.op("pool", lambda e: e.tensor_tensor(out=xv, in0=xv, in1=self.lnbc[:, 0:D], op=ALU.mult), R=[xt, self.lnbc], W=[xt])
        P.op("pool", lambda e: e.tensor_tensor(out=xv, in0=xv, in1=self.lnbc[:, D:2 * D], op=ALU.add), R=[xt, self.lnbc], W=[xt])

    def to_hT(self, xt, n):
        P = self.P
        for half in range(2):
            ps = self.ps[half]
            def f(e, half=half, ps=ps):
                ins = None
                for j in range(4):
                    dc = half * 4 + j
                    ins = e.transpose(out=ps[:, j * 128:(j + 1) * 128], in_=xt[:, dc * 128:(dc + 1) * 128], identity=self.ident[:])
                return ins
            P.op("pe", f, R=[xt, self.ident], W=[ps])
            P.op("act", lambda e, half=half, ps=ps: e.activation(
                out=self.hT[:, half * 4:(half + 1) * 4, n * 128:(n + 1) * 128],
                in_=ps[:].rearrange("p (j t) -> p j t", j=4), func=AF.Copy), R=[ps], W=[(self.hT, n)])

    def publish(self, tok):
        for e in ("pe", "act", "dve", "pool"):
            self.P.wait_tok(e, tok)

    def seq(self, b):
        P, T, NT = self.P, self.T, self.NT
        if b == 0:
            tok = P.op("act", lambda e: e.activation(out=self.esink[:], in_=self.esink[:], func=AF.Exp), W=[self.esink])
            self.publish(tok)
            self.alloc_work()
            self.xt = [self.big[3], self.big[4]]
            self.yt = [self.big[0], self.big[1]]
            self.lnbc = self.big[2]
        P.dma("sp", self.lnbc[:, 0:2 * D].rearrange("p (a d) -> p a d", a=2), self.ln0_d[:].partition_broadcast(128), R=[self.ln0_d], W=[self.lnbc])
        for n in range(NT):
            xt = self.xt[n % 2]
            P.dma("sp", xt[:, 0:D], self.x[b, n * 128:(n + 1) * 128, :], R=[self.x], W=[xt])
            self.layernorm(xt, n % 2)
            P.dma("sp", self.hbuf[n * 128:(n + 1) * 128, :], xt[:, 0:D], R=[xt], W=[(self.hbuf, n)])
            self.to_hT(xt, n)
        if b == 0:
            self.dump("hT", self.hT, self.hT[:, :, :], [128, 8, T])
        for l in range(self.NL):
            self.layer(b, l)

    def load_w(self, l, g, part):
        P = self.P
        wt = self.wt[0]
        s0, e0 = grange(g)
        tm0 = LAY[g + 'TM'][0]
        if part == 'fm':
            a, b_ = s0, tm0
        else:
            a, b_ = tm0, e0
            nt = e0 - tm0
            P.dma("sp", self.b_tm_s[:, 0:nt], self.b_tm_d[l, TMO[g + 'TM']:TMO[g + 'TM'] + nt].partition_broadcast(128), R=[self.b_tm_d], W=[self.b_tm_s])
        self.wbase = a
        gi = GL.index(g)
        P.dma("sp", wt[:, :, 0:b_ - a], self.w_bf[l, :, a:b_].rearrange("(dc p) c -> p dc c", p=128),
              R=[(self.w_bf, l * 4 + gi)], W=[wt])
        return wt

    def wcols(self, g, name):
        a, c = LAY[name]
        return a - self.wbase, c

    def proj_fm(self, ps, wt, g, name, c0, cw, mrows=128):
        a, c = self.wcols(g, name)
        n0, n1 = c0 // 128, (c0 + cw - 1) // 128
        def f(e):
            ins = None
            for dc in range(8):
                ins = e.matmul(ps[0:c, 0:cw], lhsT=wt[:, dc, a:a + c], rhs=self.hT[:, dc, c0:c0 + cw], start=(dc == 0), stop=(dc == 7))
            return ins
        self.P.op("pe", f, R=[wt, (self.hT, range(n0, n1 + 1))], W=[ps])

    def proj_tm(self, pss, wt, g, name, n):
        a, c = self.wcols(g, name)
        def f(e):
            ins = None
            for j, ps in enumerate(pss):
                cc = min(512, c - j * 512)
                for dc in range(8):
                    ins = e.matmul(ps[:, 0:cc], lhsT=self.hT[:, dc, n * 128:(n + 1) * 128], rhs=wt[:, dc, a + j * 512:a + j * 512 + cc],
                                   start=(dc == 0), stop=(dc == 7))
            return ins
        self.P.op("pe", f, R=[wt, (self.hT, n)], W=list(pss))

    def alloc_work(self):
        P, T, NT = self.P, self.T, self.NT
        BW = max(T + 128, 2176)
        self.big = [P.sbuf("big%d" % i, [128, BW]) for i in range(6)]
        self.fmb = self.big[0:4]
        self.t1 = [P.sbuf("t1_%d" % i, [128, 512]) for i in range(2)]
        self.vaug = P.sbuf("vaug", [128, NT, 4, 65], slots=NT)
        P.op("pool", lambda e: e.memset(self.vaug[:], 1.0), W=[self.vaug])
        self.zt = [P.sbuf("zt%d" % i, [128, 768]) for i in range(2)]
        self.pt = [P.sbuf("pt%d" % i, [128, 512]) for i in range(3)]
        self.t2 = [self.pt[0], self.pt[1]]
        self.ya = [P.sbuf("ya%d" % i, [128, 256]) for i in range(2)]
        self.sm = [P.sbuf("sm%d" % i, [128, 16]) for i in range(2)]
        self.cnt = 0
        self.sc = self.big[4:6]
        self.gbuf = P.dram("gbuf", [4, 3, T], F32)
        self.gsl = [P.sbuf("gsl%d" % i, [4, 3, 128]) for i in range(2)]
        self.EAc = P.sbuf("EAc", [128, T // 64, 2])
        self.z4 = P.sbuf("z4", [4, 2])
        self.publish(P.op("pool", lambda e: e.memset(self.z4[:], 0.0), W=[self.z4]))
        self.nbif = P.sbuf("nbif", [4, self.NL, 2])
        self.publish(P.op("dve", lambda e: e.tensor_scalar(out=self.nbif[:], in0=self.b_if[:], scalar1=-1.0, scalar2=None, op0=ALU.mult), W=[self.nbif]))
        self.smb = [P.sbuf("smb%d" % i, [128, 32]) for i in range(2)]
        self.w1 = [self.pt[0], self.pt[1]]
        self.wib = [P.sbuf("wib%d" % i, [128, 512]) for i in range(2)]
        self.qtl = [P.sbuf("qtl%d" % i, [128, 2, 128]) for i in range(2)]
        self.kw = [P.sbuf("kw%d" % i, [128, 256]) for i in range(2)]
        self.caug = P.sbuf("caug", [128, 2, 65])
        self.hb = [P.sbuf("hb%d" % i, [128, 256]) for i in range(2)]
        self.lbt = P.sbuf("lbt", [128, self.NL, 2, 2])
        P.op("pool", lambda e: e.memset(self.lbt[:, :, :, 0], 0.0), W=[self.lbt])
        P.op("pool", lambda e: e.memset(self.lbt[:, :, :, 1], 1.0), W=[self.lbt])
        if self.NL > 1:
            P.op("dve", lambda e: e.tensor_tensor(out=self.lbt[:, 1, :, 0], in0=self.clb[:, :, 1], in1=self.clb[:, :, 0], op=ALU.subtract), W=[self.lbt])
            P.op("act", lambda e: e.activation(out=self.lbt[:, 1, :, 0], in_=self.lbt[:, 1, :, 0], func=AF.Sigmoid), R=[self.lbt], W=[self.lbt])
            P.op("dve", lambda e: e.tensor_scalar(out=self.lbt[:, 1, :, 1], in0=self.lbt[:, 1, :, 0], scalar1=-1.0, scalar2=1.0, op0=ALU.mult, op1=ALU.add), R=[self.lbt], W=[self.lbt])
        self.publish(P.op("dve", lambda e: e.tensor_copy(out=self.lbt[:, 0, :, 0], in_=self.lbt[:, 0, :, 0]), R=[self.lbt], W=[self.lbt]))
        self.S2 = P.sbuf("S2", [128, 64]); self.st = [P.sbuf("st%d" % i, [128, 64]) for i in range(2)]
        self.at = [P.sbuf("at%d" % i, [128, 128]) for i in range(2)]
        self.kh = [P.sbuf("kh%d" % i, [128, 128]) for i in range(2)]
        self.oc = [P.sbuf("oc%d" % i, [128, 128]) for i in range(2)]
        self.osq = [P.sbuf("osq%d" % i, [128, 128]) for i in range(2)]
        self.vz = [P.sbuf("vz%d" % i, [128, 256]) for i in range(2)]
        self.cbias = P.sbuf("cbias", [128, 2])
        self.h1 = P.sbuf("h1", [128, 128]); self.kcr = P.sbuf("kcr", [128, 128]); self.kct = P.sbuf("kct", [128, 128])
        self.pc = self.wib
        self.ocmp = [P.sbuf("ocmp%d" % i, [128, 256]) for i in range(2)]
        self.imp = [P.sbuf("imp%d" % i, [128, 64]) for i in range(2)]
        self.selb = [P.sbuf("selb%d" % i, [128, 32]) for i in range(2)]
        self.gs = [P.sbuf("gs%d" % i, [128, 16]) for i in range(2)]
        self.yd = [P.sbuf("yd%d" % i, [128, 256]) for i in range(2)]
        self.yd2 = [P.sbuf("yd2_%d" % i, [128, 256]) for i in range(2)]
        self.zero256 = P.sbuf("zero256", [128, 256])
        self.publish(P.op("pool", lambda e: e.memset(self.zero256[:], 0.0), W=[self.zero256]))
        self.ident_bf = P.sbuf("ident_bf", [128, 128], BF16)
        self.publish(P.op("dve", lambda e: e.tensor_copy(out=self.ident_bf[:], in_=self.ident[:]), W=[self.ident_bf]))
        self.vaug_b = P.sbuf("vaug_b", [128, NT, 2, 65], BF16, slots=NT)
        self.publish(P.op("dve", lambda e: e.memset(self.vaug_b[:], 1.0), W=[self.vaug_b]))
        self.kcb = P.sbuf("kcb", [64, 128], BF16)
        self.oT = [P.sbuf("oT%d" % i, [65, 512]) for i in range(2)]
        self.yTt = [P.sbuf("yTt%d" % i, [128, 8, 128], BF16) for i in range(2)]

    def rope_block(self, wt, g, blk, blks, dst, l):
        P, T = self.P, self.T
        SW = min(512, T)
        for s in range(T // SW):
            c0 = s * SW
            i = self.cnt % 2; self.cnt += 1
            pa, pb = self.ps[0 + i], self.ps[2 + i]
            t1, t2 = self.t1[i], self.t2[i]
            cst = self.cst[i]
            P.dma("sp", cst[:, 0, 0:SW], self.cosT_d[:, c0:c0 + SW], R=[self.cosT_d], W=[cst])
            P.dma("sp", cst[:, 1, 0:SW], self.sinT_d[:, c0:c0 + SW], R=[self.sinT_d], W=[cst])
            self.proj_fm(pa, wt, g, blk, c0, SW)
            self.proj_fm(pb, wt, g, blks, c0, SW)
            P.op("act", lambda e: e.activation(out=t1[:, 0:SW], in_=pa[:, 0:SW], func=AF.Identity, bias=self.b_fm[:, l, FMI[blk]:FMI[blk] + 1]), R=[pa], W=[t1])
            P.op("act", lambda e: e.activation(out=t2[:, 0:SW], in_=pb[:, 0:SW], func=AF.Identity, bias=self.b_fm[:, l, FMI[blks]:FMI[blks] + 1]), R=[pb], W=[t2])
            P.op("dve", lambda e: e.tensor_tensor(out=t1[:, 0:SW], in0=t1[:, 0:SW], in1=cst[:, 0, 0:SW], op=ALU.mult), R=[t1, cst], W=[t1])
            P.op("pool", lambda e: e.tensor_tensor(out=t2[:, 0:SW], in0=t2[:, 0:SW], in1=cst[:, 1, 0:SW], op=ALU.mult), R=[t2, cst], W=[t2])
            P.op("dve", lambda e: e.tensor_tensor(out=dst[:, c0:c0 + SW], in0=t1[:, 0:SW], in1=t2[:, 0:SW], op=ALU.add), R=[t1, t2], W=[dst])

    def emit_y(self, ya, n, m0):
        mi = m0 // 2
        self.P.dma("pool", self.ybuf[n * 128:(n + 1) * 128, m0 * 128:m0 * 128 + 256], ya[:], R=[ya], W=[(self.ybuf, n * 4 + mi)])

    def mixer_A(self, b, l, wt):
        P, T, NT = self.P, self.T, self.NT
        g = 'A'
        qr = [self.fmb[0], self.fmb[1]]
        kr = self.fmb[2]
        self.chk("A0")
        self.rope_block(wt, g, 'AQ0', 'AQ0s', qr[0], l)
        self.chk("A1")
        self.rope_block(wt, g, 'AQ1', 'AQ1s', qr[1], l)
        self.rope_block(wt, g, 'AK', 'AKs', kr, l)
        self.chk("A1c")
        if b == 0 and l == 0:
            self.dump("A_q0", qr[0], qr[0][:], [128, T]); self.dump("A_k", kr, kr[:], [128, T])
        tmo = 0
        self.load_w(l, g, 'tm')
        self.chk("A1d")
        for n in range(NT):
            i = n % 2
            ptm = self.ps[6]
            zt, ya, sm = self.zt[i], self.ya[i], self.sm[i]
            self.proj_tm([ptm], wt, g, 'ATM', n)
            self.chk("A1e")
            P.op("dve", lambda e: e.tensor_tensor(out=self.vaug[:, n, 0:2, 0:64], in0=ptm[:, 0:128].rearrange("p (g d) -> p g d", g=2),
                                                  in1=self.b_tm_s[:, tmo:tmo + 128].rearrange("p (g d) -> p g d", g=2), op=ALU.add),
                 R=[ptm, self.b_tm_s], W=[(self.vaug, n)])
            self.chk("A2a")
            P.op("dve", lambda e: e.tensor_tensor(out=zt[:, 0:256], in0=ptm[:, 128:384], in1=self.b_tm_s[:, tmo + 128:tmo + 384], op=ALU.add), R=[ptm, self.b_tm_s], W=[zt])
            self.chk("A2b")
            P.op("act", lambda e: e.activation(out=zt[:, 0:256], in_=zt[:, 0:256], func=AF.Silu), R=[zt], W=[zt])
            self.chk("A2")
            kts = [kt for kt in (n - 1, n) if kt >= 0]
            po = self.ps[7]
            pts = []
            for kt in kts:
                psn = self.ps[2 + (self.cnt % 2)]; pt = self.pt[self.cnt % 3]; self.cnt += 1
                specs = []
                for gg in range(2):
                    for r in range(2):
                        specs.append((gg * 64, lambda e, gg=gg, r=r, kt=kt, psn=psn: e.matmul(
                            psn[:, (gg * 2 + r) * 128:(gg * 2 + r + 1) * 128], lhsT=kr[gg * 64:(gg + 1) * 64, kt * 128:(kt + 1) * 128],
                            rhs=qr[r][gg * 64:(gg + 1) * 64, n * 128:(n + 1) * 128], start=True, stop=True)))
                self.pe_seq(specs, R=[kr, qr[0], qr[1]], W=[psn])
                self.chk("A3a")
                P.op("act", lambda e, psn=psn, pt=pt: e.activation(out=pt[:], in_=psn[:], func=AF.Exp, scale=0.125), R=[psn], W=[pt])
                self.chk("A3b")
                mask = self.tri_ge if kt == n else self.tri_lt
                P.op("dve", lambda e, pt=pt, mask=mask: e.tensor_tensor(out=pt[:], in0=pt[:], in1=mask[:], op=ALU.mult), R=[pt], W=[pt])
                pts.append((kt, pt))
            self.chk("A3")
            def f(e):
                ins = None
                for gg in range(2):
                    for r in range(2):
                        h = gg * 2 + r
                        for j, (kt, pt) in enumerate(pts):
                            ins = e.matmul(po[:, h * 65:(h + 1) * 65], lhsT=pt[:, h * 128:(h + 1) * 128], rhs=self.vaug[:, kt, gg, :],
                                           start=(j == 0), stop=(j == len(pts) - 1))
                return ins
            P.op("pe", f, R=[p for _, p in pts] + [(self.vaug, kts)], W=[po])
            pov = po[:, 0:260].rearrange("p (h c) -> p h c", h=4)
            P.op("dve", lambda e: e.tensor_tensor(out=sm[:, 0:4], in0=pov[:, :, 64], in1=self.esink[:, l, :], op=ALU.add), R=[po], W=[sm])
            P.op("dve", lambda e: e.reciprocal(out=sm[:, 4:8], in_=sm[:, 0:4]), R=[sm], W=[sm])
            yav = ya[:].rearrange("p (h d) -> p h d", h=4)
            P.op("dve", lambda e: e.tensor_tensor(out=yav, in0=pov[:, :, 0:64], in1=sm[:, 4:8].unsqueeze(2).to_broadcast([128, 4, 64]), op=ALU.mult), R=[po, sm], W=[ya])
            P.op("pool", lambda e: e.tensor_tensor(out=ya[:], in0=ya[:], in1=zt[:, 0:256], op=ALU.mult), R=[ya, zt], W=[ya])
            self.chk("A4")
            self.emit_y(ya, n, 0)

    def mixer_B(self, b, l, wt):
        P, T, NT = self.P, self.T, self.NT
        g = 'B'
        SW = min(512, T)
        raw = self.sc[0]
        X1, X2, X3, X4 = self.big[0:4]
        for s_ in range(T // SW):
            c0 = s_ * SW
            pi, pf = self.ps[0], self.ps[1]
            self.proj_fm(pi, wt, g, 'BI', c0, SW)
            self.proj_fm(pf, wt, g, 'BF', c0, SW)
            P.op("act", lambda e, c0=c0: e.activation(out=X1[0:4, c0:c0 + SW], in_=pi[0:4, 0:SW], func=AF.Identity, bias=self.b_if[:, l, 0:1]), R=[pi], W=[X1])
            P.op("act", lambda e, c0=c0: e.activation(out=X4[0:4, c0:c0 + SW], in_=pf[0:4, 0:SW], func=AF.Exp, scale=-1.0, bias=self.nbif[:, l, 1:2]), R=[pf], W=[X4])
        P.op("act", lambda e: e.activation(out=X4[0:4, 0:T], in_=X4[0:4, 0:T], func=AF.Ln, bias=1.0), R=[X4], W=[X4])
        P.op("dve", lambda e: e.tensor_tensor_scan(out=X3[0:4, 0:T], data0=X4[0:4, 0:T], data1=self.z4[:, 0:1].to_broadcast([4, T]), initial=0.0, op0=ALU.add, op1=ALU.add), R=[X4], W=[X3])
        P.op("dve", lambda e: e.tensor_tensor(out=X1[0:4, 0:T], in0=X1[0:4, 0:T], in1=X3[0:4, 0:T], op=ALU.add), R=[X1, X3], W=[X1])
        P.op("pool", lambda e: e.memset(X2[0:4, 0:128], 0.0), W=[X2])
        P.op("dve", lambda e: e.tensor_tensor_scan(out=X2[0:4, 128:128 + T], data0=X1[0:4, 0:T], data1=self.z4[:, 0:1].to_broadcast([4, T]), initial=0.0, op0=ALU.max, op1=ALU.add), R=[X1], W=[X2])
        P.op("dve", lambda e: e.tensor_tensor(out=X3[0:4, 0:T], in0=X3[0:4, 0:T], in1=X2[0:4, 128:128 + T], op=ALU.subtract), R=[X3, X2], W=[X3])
        P.op("act", lambda e: e.activation(out=X3[0:4, 0:T], in_=X3[0:4, 0:T], func=AF.Exp), R=[X3], W=[X3])
        gprev = X2[0:4, 0:T].rearrange("p (c t) -> p c t", t=128)[:, :, 127:128].to_broadcast([4, NT, 128])
        P.op("dve", lambda e: e.tensor_tensor(out=X4[0:4, 0:T].rearrange("p (c t) -> p c t", t=128), in0=X2[0:4, 128:128 + T].rearrange("p (c t) -> p c t", t=128),
                                              in1=gprev, op=ALU.subtract), R=[X2], W=[X4])
        P.op("dve", lambda e: e.tensor_tensor(out=X1[0:4, 0:T].rearrange("p (c t) -> p c t", t=128), in0=X1[0:4, 0:T].rearrange("p (c t) -> p c t", t=128),
                                              in1=gprev, op=ALU.subtract), R=[X1, X2], W=[X1])
        P.dma("sp", self.gbuf[:, 0, :], X4[0:4, 0:T], R=[X4], W=[self.gbuf])
        P.dma("sp", self.gbuf[:, 1, :], X1[0:4, 0:T], R=[X1], W=[self.gbuf])
        P.dma("sp", self.gbuf[:, 2, :], X3[0:4, 0:T], R=[X3], W=[self.gbuf])
        for j, blk in enumerate(['BQ0', 'BQ1', 'BK0', 'BK1']):
            dst = self.fmb[j]
            P.op("pool", lambda e: e.memset(raw[:, 0:3], 0.0), W=[raw])
            for s_ in range(T // SW):
                c0 = s_ * SW
                ps = self.ps[self.cnt % 2]; self.cnt += 1
                self.proj_fm(ps, wt, g, blk, c0, SW)
                P.op("act", lambda e, ps=ps, c0=c0: e.activation(out=raw[:, 3 + c0:3 + c0 + SW], in_=ps[:, 0:SW], func=AF.Identity,
                                                                 bias=self.b_fm[:, l, FMI[blk]:FMI[blk] + 1]), R=[ps], W=[raw])
            P.op("dve", lambda e: e.tensor_scalar(out=dst[:, 0:T], in0=raw[:, 3:3 + T], scalar1=self.cw[:, l, j, 3:4], scalar2=self.cb[:, l, j:j + 1],
                                                  op0=ALU.mult, op1=ALU.add), R=[raw], W=[dst])
            for tap in (2, 1, 0):
                P.op("dve", lambda e, tap=tap: e.scalar_tensor_tensor(out=dst[:, 0:T], in0=raw[:, tap:tap + T], scalar=self.cw[:, l, j, tap:tap + 1],
                                                                      in1=dst[:, 0:T], op0=ALU.mult, op1=ALU.add), R=[raw, dst], W=[dst])
            P.op("act", lambda e: e.activation(out=dst[:, 0:T], in_=dst[:, 0:T], func=AF.Silu), R=[dst], W=[dst])
        qT = [self.fmb[0], self.fmb[1]]
        kT = [self.fmb[2], self.fmb[3]]
        self.load_w(l, g, 'tm')
        tmo = 0
        caug = self.caug
        for c in range(NT):
            i = c % 2
            cs_ = slice(c * 128, (c + 1) * 128)
            zt, hb, smb, w1, wib, qtl, kw = self.zt[i], self.hb[i], self.smb[i], self.w1[i], self.wib[i], self.qtl[i], self.kw[i]
            ptm = [self.ps[6], self.ps[7]]
            self.proj_tm(ptm, wt, g, 'BTM', c)
            P.op("dve", lambda e: e.tensor_tensor(out=self.vaug[:, c, :, 0:64], in0=ptm[0][:, 0:256].rearrange("p (h d) -> p h d", h=4),
                                                  in1=self.b_tm_s[:, tmo:tmo + 256].rearrange("p (h d) -> p h d", h=4), op=ALU.add),
                 R=[ptm[0], self.b_tm_s], W=[(self.vaug, c)])
            P.op("dve", lambda e: e.tensor_tensor(out=zt[:, 0:256], in0=ptm[0][:, 256:512], in1=self.b_tm_s[:, tmo + 256:tmo + 512], op=ALU.add), R=[ptm[0], self.b_tm_s], W=[zt])
            P.op("dve", lambda e: e.tensor_tensor(out=zt[:, 256:512], in0=ptm[1][:, 0:256], in1=self.b_tm_s[:, tmo + 512:tmo + 768], op=ALU.add), R=[ptm[1], self.b_tm_s], W=[zt])
            P.op("act", lambda e: e.activation(out=zt[:, 0:256], in_=zt[:, 0:256], func=AF.Sigmoid), R=[zt], W=[zt])
            P.op("act", lambda e: e.activation(out=zt[:, 256:512], in_=zt[:, 256:512], func=AF.Silu), R=[zt], W=[zt])
            gsl = self.gsl[i]
            P.dma("sp", gsl[:], self.gbuf[:, :, cs_], R=[self.gbuf], W=[gsl])
            psT = self.ps[0]
            def f(e):
                e.transpose(out=psT[:, 0:4], in_=gsl[0:4, 1, :], identity=self.ident[0:4, 0:4])
                return e.transpose(out=psT[:, 4:8], in_=gsl[0:4, 2, :], identity=self.ident[0:4, 0:4])
            self.pe_seq([(0, f)], R=[gsl], W=[psT])
            P.op("dve", lambda e: e.tensor_copy(out=smb[:, 0:8], in_=psT[:, 0:8]), R=[psT], W=[smb])
            psG = self.ps[1]
            def f(e):
                ins = None
                for h in range(4):
                    ins = e.matmul(psG[:, h * 128:(h + 1) * 128], lhsT=self.sel[0:4, h, :], rhs=gsl[0:4, 0, :], start=True, stop=True)
                return ins
            self.pe_seq([(0, f)], R=[gsl], W=[psG])
            for h in range(4):
                P.op("act", lambda e, h=h: e.activation(out=w1[:, h * 128:(h + 1) * 128], in_=psG[:, h * 128:(h + 1) * 128], func=AF.Exp, scale=-1.0,
                                                        bias=smb[:, h:h + 1]), R=[psG, smb], W=[w1])
            P.op("act", lambda e: e.activation(out=wib[:], in_=psG[:], func=AF.Exp, scale=-1.0), R=[psG], W=[wib])
            P.op("pool", lambda e: e.tensor_tensor(out=w1[:], in0=w1[:], in1=self.tri_ge[:], op=ALU.mult), R=[w1], W=[w1])
            P.op("dve", lambda e: e.tensor_tensor(out=smb[:, 8:12], in0=smb[:, 0:4], in1=psG[:].rearrange("p (h t) -> p h t", h=4)[:, :, 127], op=ALU.subtract),
                 R=[smb, psG], W=[smb])
            P.op("act", lambda e: e.activation(out=smb[:, 8:12], in_=smb[:, 8:12], func=AF.Exp), R=[smb], W=[smb])
            psS = self.ps[2]
            specs = []
            for h in (0, 2, 1, 3):
                j, base = h // 2, (h % 2) * 64
                specs.append((base, lambda e, h=h, j=j, base=base: e.matmul(psS[:, h * 128:(h + 1) * 128], lhsT=kT[j][base:base + 64, cs_],
                                                                          rhs=qT[j][base:base + 64, cs_], start=True, stop=True)))
            self.pe_seq(specs, R=[kT[0], kT[1], qT[0], qT[1]], W=[psS])
            P.op("dve", lambda e: e.scalar_tensor_tensor(out=w1[:], in0=psS[:], scalar=0.125, in1=w1[:], op0=ALU.mult, op1=ALU.mult), R=[psS, w1], W=[w1])
            if c > 0:
                for h in range(4):
                    j, base = h // 2, (h % 2) * 64
                    P.op("dve", lambda e, h=h, j=j, base=base: e.scalar_tensor_tensor(
                        out=qtl[base:base + 64, j, :], in0=qT[j][base:base + 64, cs_], scalar=0.125, in1=wib[base:base + 64, h * 128:(h + 1) * 128],
                        op0=ALU.mult, op1=ALU.mult), R=[qT[j], wib], W=[qtl])
            po = self.ps[3]
            specs = []
            for h in (0, 2, 1, 3):
                j, base = h // 2, (h % 2) * 64
                specs.append((None, lambda e, h=h: e.matmul(po[:, h * 65:(h + 1) * 65], lhsT=w1[:, h * 128:(h + 1) * 128], rhs=self.vaug[:, c, h, :], start=True, stop=(c == 0))))
                if c > 0:
                    specs.append((base, lambda e, h=h, j=j, base=base: e.matmul(po[:, h * 65:(h + 1) * 65], lhsT=qtl[base:base + 64, j, :], rhs=caug[base:base + 64, j, :],
                                                                              start=False, stop=True)))
            self.pe_seq(specs, R=[w1, (self.vaug, c)] + ([qtl, caug] if c > 0 else []), W=[po])
            pov = po[:, 0:260].rearrange("p (h c) -> p h c", h=4)
            P.op("act", lambda e: e.activation(out=smb[:, 12:16], in_=pov[:, :, 64], func=AF.Abs), R=[po], W=[smb])
            P.op("dve", lambda e: e.tensor_tensor(out=smb[:, 12:16], in0=smb[:, 12:16], in1=smb[:, 4:8], op=ALU.max), R=[smb], W=[smb])
            P.op("dve", lambda e: e.reciprocal(out=smb[:, 16:20], in_=smb[:, 12:16]), R=[smb], W=[smb])
            hbv = hb[:].rearrange("p (h d) -> p h d", h=4)
            P.op("dve", lambda e: e.tensor_tensor(out=hbv, in0=pov[:, :, 0:64], in1=smb[:, 16:20].unsqueeze(2).to_broadcast([128, 4, 64]), op=ALU.mult), R=[po, smb], W=[hb])
            P.op("pool", lambda e: e.tensor_tensor(out=hb[:], in0=hb[:], in1=zt[:, 0:256], op=ALU.mult), R=[hb, zt], W=[hb])
            P.op("pool", lambda e: e.tensor_tensor(out=hb[:], in0=hb[:], in1=zt[:, 256:512], op=ALU.mult), R=[hb, zt], W=[hb])
            self.emit_y(hb, c, 2)
            if c < NT - 1:
                pk = self.ps[4]
                def f(e):
                    e.transpose(out=pk[:, 0:128], in_=kT[0][:, cs_], identity=self.ident[:])
                    return e.transpose(out=pk[:, 128:256], in_=kT[1][:, cs_], identity=self.ident[:])
                P.op("pe", f, R=[kT[0], kT[1]], W=[pk])
                P.op("dve", lambda e: e.tensor_tensor(out=kw[:].rearrange("p (h d) -> p h d", h=4), in0=pk[:, 0:256].rearrange("p (h d) -> p h d", h=4),
                                                      in1=smb[:, 8:12].unsqueeze(2).to_broadcast([128, 4, 64]), op=ALU.mult), R=[pk, smb], W=[kw])
                pu = self.ps[5]
                def f(e):
                    ins = None
                    for j in range(2):
                        ins = e.matmul(pu[:, j * 130:(j + 1) * 130], lhsT=kw[:, j * 128:(j + 1) * 128],
                                       rhs=self.vaug[:, c, 2 * j:2 * j + 2, :], start=True, stop=True)
                    return ins
                P.op("pe", f, R=[kw, (self.vaug, c)], W=[pu])
                for h in range(4):
                    j, base = h // 2, (h % 2) * 64
                    src = pu[base:base + 64, j * 130 + (h % 2) * 65:j * 130 + (h % 2) * 65 + 65]
                    if c == 0:
                        P.op("dve", lambda e, src=src, j=j, base=base: e.tensor_copy(out=caug[base:base + 64, j, :], in_=src), R=[pu], W=[caug])
                    else:
                        P.op("dve", lambda e, src=src, j=j, base=base, h=h: e.scalar_tensor_tensor(
                            out=caug[base:base + 64, j, :], in0=caug[base:base + 64, j, :], scalar=wib[base:base + 64, h * 128 + 127:h * 128 + 128], in1=src,
                            op0=ALU.mult, op1=ALU.add), R=[pu, caug, wib], W=[caug])

    def mixer_C(self, b, l, wt):
        P, T, NT = self.P, self.T, self.NT
        g = 'C'
        SW = min(512, T)
        NC = T // 64
        qs, fk, lfd, aa, tmp, kt_ = self.big
        qt_, kh_, EAc = qs, fk, self.EAc
        v3 = lambda t: t[:, 0:T].rearrange("p (c t) -> p c t", t=64)
        tmo = 0
        for j in range(2):
            bq, bf = 'CQ%d' % j, 'CF%d' % j
            if j > 0:
                wt = self.load_w(l, g, 'fm')
            for s_ in range(T // SW):
                c0 = s_ * SW
                pa, pb_ = self.ps[0], self.ps[1]
                self.proj_fm(pa, wt, g, bq, c0, SW)
                self.proj_fm(pb_, wt, g, bf, c0, SW)
                P.op("act", lambda e, c0=c0: e.activation(out=qs[:, c0:c0 + SW], in_=pa[:, 0:SW], func=AF.Silu, bias=self.b_fm[:, l, FMI[bq]:FMI[bq] + 1]), R=[pa], W=[qs])
                P.op("act", lambda e, c0=c0: e.activation(out=fk[:, c0:c0 + SW], in_=pb_[:, 0:SW], func=AF.Sigmoid, bias=self.b_fm[:, l, FMI[bf]:FMI[bf] + 1]), R=[pb_], W=[fk])
            P.op("dve", lambda e: e.tensor_scalar(out=fk[:, 0:T], in0=fk[:, 0:T], scalar1=self.lbt[:, l, j, 1:2], scalar2=self.lbt[:, l, j, 0:1], op0=ALU.mult, op1=ALU.add), R=[fk], W=[fk])
            P.op("act", lambda e: e.activation(out=lfd[:, 0:T], in_=fk[:, 0:T], func=AF.Ln), R=[fk], W=[lfd])
            P.op("dve", lambda e: e.tensor_scalar(out=fk[:, 0:T], in0=fk[:, 0:T], scalar1=-1.0, scalar2=1.0, op0=ALU.mult, op1=ALU.add), R=[fk, lfd], W=[fk])
            P.op("pool", lambda e: e.memset(kt_[:, 0:T], 1.0), W=[kt_])
            P.op("pool", lambda e: e.memset(v3(kt_)[:, :, 0:1], 0.0), W=[kt_])
            P.op("dve", lambda e: e.tensor_tensor_scan(out=aa[:, 0:T], data0=kt_[:, 0:T], data1=lfd[:, 0:T], initial=0.0, op0=ALU.mult, op1=ALU.add), R=[kt_, lfd], W=[aa])
            P.op("act", lambda e: e.activation(out=EAc[:, :, 0], in_=v3(aa)[:, :, 31], func=AF.Exp), R=[aa], W=[EAc])
            P.op("act", lambda e: e.activation(out=EAc[:, :, 1], in_=v3(aa)[:, :, 63], func=AF.Exp), R=[aa], W=[EAc])
            P.op("dve", lambda e: e.tensor_tensor(out=v3(lfd), in0=v3(aa), in1=v3(aa)[:, :, 31:32].to_broadcast([128, NC, 64]), op=ALU.subtract), R=[aa], W=[lfd])
            P.op("act", lambda e: e.activation(out=tmp[:, 0:T], in_=lfd[:, 0:T], func=AF.Exp), R=[lfd], W=[tmp])
            P.op("dve", lambda e: e.tensor_tensor(out=qt_[:, 0:T], in0=qs[:, 0:T], in1=tmp[:, 0:T], op=ALU.mult), R=[qs, tmp], W=[qt_])
            P.op("act", lambda e: e.activation(out=tmp[:, 0:T], in_=lfd[:, 0:T], func=AF.Exp, scale=-1.0), R=[lfd, qt_], W=[tmp])
            P.op("pool", lambda e: e.tensor_tensor(out=kt_[:, 0:T], in0=fk[:, 0:T], in1=tmp[:, 0:T], op=ALU.mult), R=[fk, tmp], W=[kt_])
            P.op("dve", lambda e: e.tensor_tensor(out=v3(tmp), in0=v3(lfd)[:, :, 63:64].to_broadcast([128, NC, 64]), in1=v3(lfd), op=ALU.subtract), R=[lfd, kt_], W=[tmp])
            P.op("act", lambda e: e.activation(out=tmp[:, 0:T], in_=tmp[:, 0:T], func=AF.Exp), R=[tmp], W=[tmp])
            P.op("pool", lambda e: e.tensor_tensor(out=kh_[:, 0:T], in0=fk[:, 0:T], in1=tmp[:, 0:T], op=ALU.mult), R=[fk, tmp], W=[kh_])
            S2 = self.S2
            self.load_w(l, g, 'tm')
            aC, cC = self.wcols(g, 'CTM')
            for n in range(NT):
                i = n % 2
                vz, oc, osq, sm = self.vz[i], self.oc[i], self.osq[i], self.sm[i]
                ptm = self.ps[6]
                def f(e):
                    ins = None
                    for r, off in enumerate((j * 128, 256 + j * 128)):
                        for dc in range(8):
                            ins = e.matmul(ptm[:, r * 128:(r + 1) * 128], lhsT=self.hT[:, dc, n * 128:(n + 1) * 128], rhs=wt[:, dc, aC + off:aC + off + 128],
                                           start=(dc == 0), stop=(dc == 7))
                    return ins
                P.op("pe", f, R=[wt, (self.hT, n)], W=[ptm])
                P.op("dve", lambda e: e.tensor_tensor(out=vz[:, 0:128], in0=ptm[:, 0:128], in1=self.b_tm_s[:, tmo + j * 128:tmo + (j + 1) * 128], op=ALU.add), R=[ptm, self.b_tm_s], W=[vz])
                P.op("dve", lambda e: e.tensor_tensor(out=vz[:, 128:256], in0=ptm[:, 128:256], in1=self.b_tm_s[:, tmo + 256 + j * 128:tmo + 256 + (j + 1) * 128], op=ALU.add), R=[ptm, self.b_tm_s], W=[vz])
                P.op("act", lambda e: e.activation(out=vz[:, 128:256], in_=vz[:, 128:256], func=AF.Silu), R=[vz], W=[vz])
                po = self.ps[4 + i]
                khb = self.kh[i]
                if n * 2 < NC - 1:
                    pk = self.ps[0]
                    P.op("pe", lambda e, pk=pk: e.transpose(out=pk[:, 0:128], in_=kh_[:, n * 128:(n + 1) * 128], identity=self.ident[:]), R=[kh_], W=[pk])
                    P.op("act", lambda e, pk=pk: e.activation(out=khb[:], in_=pk[:, 0:128], func=AF.Copy), R=[pk], W=[khb])
                for cp in range(2):
                    c = n * 2 + cp
                    pb = cp * 64
                    cs_ = slice(c * 64, (c + 1) * 64)
                    at, kh, st = self.at[cp], khb, self.st[cp]
                    psA = self.ps[2 + cp]
                    specs = []
                    for hh in range(2):
                        base = hh * 64
                        specs.append((base, lambda e, hh=hh, base=base, psA=psA, pb=pb, cs_=cs_: e.matmul(
                            psA[pb:pb + 64, hh * 64:(hh + 1) * 64], lhsT=kt_[base:base + 64, cs_], rhs=qt_[base:base + 64, cs_], start=True, stop=True)))
                    self.pe_seq(specs, R=[kt_, qt_], W=[psA])
                    pU = self.ps[1] if cp == 0 else self.ps[7]
                    if c < NC - 1:
                        self.pe_seq([(pb, lambda e, pU=pU, kh=kh, pb=pb: e.matmul(pU[:, 0:128], lhsT=kh[pb:pb + 64, :], rhs=vz[pb:pb + 64, 0:128], start=True, stop=True))],
                                    R=[kh, vz], W=[pU])
                    mask = self.tri_ge[pb:pb + 64, :].rearrange("p (r f) -> p r f", f=128)[:, 0:2, pb:pb + 64]
                    P.op("dve", lambda e, at=at, psA=psA, mask=mask, pb=pb: e.tensor_tensor(out=at[pb:pb + 64, :].rearrange("p (r f) -> p r f", f=64),
                                                                                          in0=psA[pb:pb + 64, 0:128].rearrange("p (r f) -> p r f", f=64), in1=mask, op=ALU.mult),
                         R=[psA], W=[at])
                    if c > 0:
                        P.op("dve", lambda e, st=st, c=c: e.tensor_scalar(out=st[:], in0=S2[:], scalar1=EAc[:, c, 0:1], scalar2=None, op0=ALU.mult), R=[S2, EAc], W=[st])
                    specs = []
                    for hh in range(2):
                        base = hh * 64
                        specs.append((pb, lambda e, hh=hh, at=at, pb=pb, c=c: e.matmul(
                            po[pb:pb + 64, hh * 64:(hh + 1) * 64], lhsT=at[pb:pb + 64, hh * 64:(hh + 1) * 64], rhs=vz[pb:pb + 64, hh * 64:(hh + 1) * 64],
                            start=True, stop=(c == 0))))
                        if c > 0:
                            specs.append((base, lambda e, hh=hh, base=base, st=st, pb=pb, cs_=cs_: e.matmul(
                                po[pb:pb + 64, hh * 64:(hh + 1) * 64], lhsT=qt_[base:base + 64, cs_], rhs=st[base:base + 64, :], start=False, stop=True)))
                    self.pe_seq(specs, R=[at, vz, qt_] + ([st] if c > 0 else []), W=[po])
                    if c < NC - 1:
                        for hh in range(2):
                            base = hh * 64
                            if c == 0:
                                P.op("dve", lambda e, base=base, hh=hh, pU=pU: e.tensor_copy(out=S2[base:base + 64, :], in_=pU[base:base + 64, hh * 64:(hh + 1) * 64]), R=[pU], W=[S2])
                            else:
                                P.op("dve", lambda e, base=base, hh=hh, pU=pU, c=c: e.scalar_tensor_tensor(
                                    out=S2[base:base + 64, :], in0=S2[base:base + 64, :], scalar=EAc[base:base + 64, c, 1:2],
                                    in1=pU[base:base + 64, hh * 64:(hh + 1) * 64], op0=ALU.mult, op1=ALU.add), R=[pU, S2, EAc], W=[S2])
                P.op("act", lambda e: e.activation(out=oc[:], in_=po[:, 0:128], func=AF.Copy), R=[po], W=[oc])
                P.op("dve", lambda e: e.tensor_tensor(out=osq[:], in0=oc[:], in1=oc[:], op=ALU.mult), R=[oc], W=[osq])
                P.op("dve", lambda e: e.tensor_reduce(out=sm[:, 0:2], in_=osq[:].rearrange("p (h d) -> p h d", h=2), axis=AX.X, op=ALU.add), R=[osq], W=[sm])
                P.op("dve", lambda e: e.tensor_scalar(out=sm[:, 2:4], in0=sm[:, 0:2], scalar1=1.0 / 64, scalar2=1e-6, op0=ALU.mult, op1=ALU.add), R=[sm], W=[sm])
                P.op("act", lambda e: e.activation(out=sm[:, 2:4], in_=sm[:, 2:4], func=AF.Sqrt), R=[sm], W=[sm])
                P.op("dve", lambda e: e.reciprocal(out=sm[:, 4:6], in_=sm[:, 2:4]), R=[sm], W=[sm])
                P.op("dve", lambda e: e.tensor_tensor(out=oc[:].rearrange("p (h d) -> p h d", h=2), in0=oc[:].rearrange("p (h d) -> p h d", h=2),
                                                      in1=sm[:, 4:6].unsqueeze(2).to_broadcast([128, 2, 64]), op=ALU.mult), R=[oc, sm], W=[oc])
                P.op("pool", lambda e: e.tensor_tensor(out=oc[:], in0=oc[:], in1=self.gbc[:, l, j * 128:(j + 1) * 128], op=ALU.mult), R=[oc], W=[oc])
                P.op("pool", lambda e: e.tensor_tensor(out=oc[:], in0=oc[:], in1=vz[:, 128:256], op=ALU.mult), R=[oc, vz], W=[oc])
                P.dma("pool", self.ybuf[n * 128:(n + 1) * 128, 512 + j * 128:512 + (j + 1) * 128], oc[:], R=[oc], W=[(self.ybuf, n * 4 + 2)])

    def rope64(self, wt, g, blk, blks, l, dst_of):
        P, T = self.P, self.T
        SW = min(512, T)
        for s_ in range(T // SW):
            c0 = s_ * SW
            i = self.cnt % 2; self.cnt += 1
            pa, pb = self.ps[0 + i], self.ps[2 + i]
            t1, t2 = self.t1[i], self.t2[i]
            cst = self.cst[i]
            P.dma("sp", cst[0:64, 0, 0:SW], self.cosT_d[0:64, c0:c0 + SW], R=[self.cosT_d], W=[cst])
            P.dma("sp", cst[0:64, 1, 0:SW], self.sinT_d[0:64, c0:c0 + SW], R=[self.sinT_d], W=[cst])
            self.proj_fm(pa, wt, g, blk, c0, SW)
            self.proj_fm(pb, wt, g, blks, c0, SW)
            P.op("act", lambda e: e.activation(out=t1[0:64, 0:SW], in_=pa[0:64, 0:SW], func=AF.Identity, bias=self.b_fm[0:64, l, FMI[blk]:FMI[blk] + 1]), R=[pa], W=[t1])
            P.op("act", lambda e: e.activation(out=t2[0:64, 0:SW], in_=pb[0:64, 0:SW], func=AF.Identity, bias=self.b_fm[0:64, l, FMI[blks]:FMI[blks] + 1]), R=[pb], W=[t2])
            P.op("dve", lambda e: e.tensor_tensor(out=t1[0:64, 0:SW], in0=t1[0:64, 0:SW], in1=cst[0:64, 0, 0:SW], op=ALU.mult), R=[t1, cst], W=[t1])
            P.op("pool", lambda e: e.tensor_tensor(out=t2[0:64, 0:SW], in0=t2[0:64, 0:SW], in1=cst[0:64, 1, 0:SW], op=ALU.mult), R=[t2, cst], W=[t2])
            dbuf, dap, t1v, t2v = dst_of(c0, SW, t1, t2)
            P.op("dve", lambda e: e.tensor_tensor(out=dap, in0=t1v, in1=t2v, op=ALU.add), R=[t1, t2], W=[dbuf])

    def mixer_D(self, b, l, wt):
        P, T, NT = self.P, self.T, self.NT
        g = 'D'
        SW = min(512, T)
        NCMP, NSL = self.NCMP, self.NSL
        HT = NT if NT <= 8 else NT // 2
        qb = [self.big[0][:, :].bitcast(BF16), self.big[1][:, :].bitcast(BF16)]
        def qtile(n):
            return qb[n // HT][0:64, (n % HT) * 512:(n % HT + 1) * 512]
        kb = self.big[2][:, :].bitcast(BF16)
        KCV = self.big[4]
        w1b = self.big[5]
        w1v = w1b[:, 0:2048].rearrange("p (j e) -> p j e", e=64)
        P.dma("sp", w1v, self.w1_d[l].rearrange("p (j e) -> p j e", e=64), R=[self.w1_d], W=[w1b])
        for h in range(4):
            blk = 'DQ0' if h == 0 else 'DH%d' % h
            def dst_of(c0, SW_, t1, t2, h=h):
                n0 = c0 // 128
                nt_ = SW_ // 128
                buf = self.big[n0 // HT]
                base = (n0 % HT) * 512
                dap = qb[n0 // HT][0:64, base:base + nt_ * 512].rearrange("p (n hh t) -> p n hh t", hh=4, t=128)[:, :, h, :]
                return buf, dap, t1[0:64, 0:SW_].rearrange("p (n t) -> p n t", t=128), t2[0:64, 0:SW_].rearrange("p (n t) -> p n t", t=128)
            self.rope64(wt, g, blk, blk + 's', l, dst_of)
        for name, off in (('DKS', 0), ('DKW', T)):
            def dst_of(c0, SW_, t1, t2, off=off):
                return self.big[2], kb[0:64, off + c0:off + c0 + SW_], t1[0:64, 0:SW_], t2[0:64, 0:SW_]
            self.rope64(wt, g, name, name + 's', l, dst_of)
        for s_ in range(T // SW):
            c0 = s_ * SW
            ps = self.ps[self.cnt % 2]; self.cnt += 1
            self.proj_fm(ps, wt, g, 'DKCV', c0, SW)
            P.op("act", lambda e, ps=ps, c0=c0: e.activation(out=KCV[:, c0:c0 + SW], in_=ps[:, 0:SW], func=AF.Identity,
                                                             bias=self.b_fm[:, l, FMI['DKCV']:FMI['DKCV'] + 1]), R=[ps], W=[KCV])
        pb_ = self.ps[5]
        specs = []
        for half in range(2):
            r = slice(half * 64, half * 64 + 64)
            for j in range(32):
                specs.append((half * 64, lambda e, r=r, j=j: e.matmul(pb_[r, 0:1], lhsT=w1v[r, j, :], rhs=self.peT[r, l, j:j + 1], start=(j == 0), stop=(j == 31))))
        self.pe_seq(specs, R=[w1b], W=[pb_])
        P.op("dve", lambda e: e.tensor_copy(out=self.cbias[:, 0:1], in_=pb_[:, 0:1]), R=[pb_], W=[self.cbias])
        pH = self.ps[6]
        span = 16 * (NCMP - 1) + 1
        specs = []
        for half in range(2):
            r = slice(half * 64, half * 64 + 64)
            for j in range(32):
                specs.append((half * 64, lambda e, r=r, j=j: e.matmul(pH[r, 0:NCMP], lhsT=w1v[r, j, :], rhs=KCV[r, j:j + span:16], start=(j == 0), stop=(j == 31))))
        self.pe_seq(specs, R=[w1b, KCV], W=[pH])
        h1, kcr, kct, kcb = self.h1, self.kcr, self.kct, self.kcb
        P.op("act", lambda e: e.activation(out=h1[:, 0:NCMP], in_=pH[:, 0:NCMP], func=AF.Silu, bias=self.cbias[:, 0:1]), R=[pH, self.cbias], W=[h1])
        pK = self.ps[7]
        self.pe_seq([
            (0, lambda e: e.matmul(pK[:, 0:NCMP], lhsT=self.w2s[0:64, l, 0:128], rhs=h1[0:64, 0:NCMP], start=True, stop=True)),
            (0, lambda e: e.matmul(pK[:, 128:128 + NCMP], lhsT=self.w2s[0:64, l, 128:256], rhs=h1[0:64, 0:NCMP], start=True, stop=True)),
            (64, lambda e: e.matmul(pK[0:NCMP, 256:320], lhsT=h1[64:128, 0:NCMP], rhs=self.w2s[64:128, l, 256:320], start=True, stop=True))], R=[h1], W=[pK])
        ce = self.cosE[:, 0:NCMP]
        se = self.sinE[:, 0:NCMP]
        P.op("dve", lambda e: e.tensor_tensor(out=kcr[:, 0:NCMP], in0=pK[:, 0:NCMP], in1=ce, op=ALU.mult), R=[pK], W=[kcr])
        P.op("dve", lambda e: e.tensor_tensor(out=kct[:, 0:NCMP], in0=pK[:, 128:128 + NCMP], in1=se, op=ALU.mult), R=[pK], W=[kct])
        P.op("dve", lambda e: e.tensor_tensor(out=kcb[0:64, 0:NCMP], in0=kcr[0:64, 0:NCMP], in1=kct[0:64, 0:NCMP], op=ALU.add), R=[kcr, kct], W=[kcb])
        P.op("act", lambda e: e.activation(out=self.vca[0:NCMP, 0:64], in_=pK[0:NCMP, 256:320], func=AF.Copy), R=[pK], W=[self.vca])
        tmo = 0
        self.load_w(l, g, 'tm')
        W97 = 65 + NSL
        selx = self.big[5]
        selb_ = selx[:, :].bitcast(BF16)
        ptb = [p_[:, :].bitcast(BF16) for p_ in self.pt]
        for n in range(NT):
            i = n % 2
            qs_ = slice(n * 128, (n + 1) * 128)
            qt_n = qtile(n)
            qbuf = self.big[n // HT]
            zt, pc, imp, selb, gs, yd, yd2, sm, oT = self.zt[i], self.pc[i], self.imp[i], self.selb[i], self.gs[i], self.yd[i], self.yd2[i], self.smb[i], self.oT[i]
            ptm = self.ps[0]
            self.proj_tm([ptm], wt, g, 'DTM', n)
            P.op("dve", lambda e: e.tensor_tensor(out=self.vaug_b[:, n, 0:2, 0:64], in0=ptm[:, 0:128].rearrange("p (h d) -> p h d", h=2),
                                                  in1=self.b_tm_s[:, tmo:tmo + 128].rearrange("p (h d) -> p h d", h=2), op=ALU.add),
                 R=[ptm, self.b_tm_s], W=[(self.vaug_b, n)])
            P.op("dve", lambda e: e.tensor_tensor(out=gs[:, 0:12], in0=ptm[:, 128:140], in1=self.b_tm_s[:, tmo + 128:tmo + 140], op=ALU.add), R=[ptm, self.b_tm_s], W=[gs])
            P.op("act", lambda e: e.activation(out=gs[:, 0:12], in_=gs[:, 0:12], func=AF.Sigmoid), R=[gs], W=[gs])
            P.op("dve", lambda e: e.tensor_tensor(out=zt[:, 0:256], in0=ptm[:, 140:396], in1=self.b_tm_s[:, tmo + 140:tmo + 396], op=ALU.add), R=[ptm, self.b_tm_s], W=[zt])
            P.op("act", lambda e: e.activation(out=zt[:, 0:256], in_=zt[:, 0:256], func=AF.Silu), R=[zt], W=[zt])
            mct = self.mct[i]
            P.dma("sp", mct[:], self.maskC_d[:, qs_], R=[self.maskC_d], W=[mct])
            pC = self.ps[1]
            self.pe_seq([(0, lambda e: e.matmul(pC[0:NCMP, :], lhsT=kcb[0:64, 0:NCMP], rhs=qt_n, start=True, stop=True))], R=[kcb, qbuf], W=[pC])
            P.op("act", lambda e: e.activation(out=pc[0:NCMP, :], in_=pC[0:NCMP, :], func=AF.Exp, scale=0.125), R=[pC], W=[pc])
            P.op("dve", lambda e: e.tensor_tensor(out=pc[0:NCMP, :].rearrange("p (h t) -> p h t", h=4), in0=pc[0:NCMP, :].rearrange("p (h t) -> p h t", h=4),
                                                  in1=mct[0:NCMP, :].unsqueeze(1).to_broadcast([NCMP, 4, 128]), op=ALU.mult), R=[pc, mct], W=[pc])
            pO = self.ps[2]
            def f(e):
                ins = None
                for h in range(4):
                    ins = e.matmul(pO[:, h * W97:(h + 1) * W97], lhsT=pc[0:NCMP, h * 128:(h + 1) * 128], rhs=self.vca[0:NCMP, 0:W97], start=True, stop=True)
                return ins
            self.pe_seq([(0, f)], R=[pc, self.vca], W=[pO])
            pOv = pO[:, 0:4 * W97].rearrange("p (h c) -> p h c", h=4)
            P.op("dve", lambda e: e.tensor_scalar(out=sm[:, 0:4], in0=pOv[:, :, 64], scalar1=1e-30, scalar2=None, op0=ALU.max), R=[pO], W=[sm])
            P.op("dve", lambda e: e.reciprocal(out=sm[:, 4:8], in_=sm[:, 0:4]), R=[sm], W=[sm])
            for h in range(4):
                if h == 0:
                    P.op("dve", lambda e: e.tensor_scalar(out=imp[:, 0:NSL], in0=pOv[:, 0, 65:W97], scalar1=sm[:, 4:5], scalar2=None, op0=ALU.mult), R=[pO, sm], W=[imp])
                else:
                    P.op("dve", lambda e, h=h: e.scalar_tensor_tensor(out=imp[:, 0:NSL], in0=pOv[:, h, 65:W97], scalar=sm[:, 4 + h:5 + h], in1=imp[:, 0:NSL],
                                                                      op0=ALU.mult, op1=ALU.add), R=[pO, sm, imp], W=[imp])
            P.op("dve", lambda e: e.tensor_tensor(out=imp[:, 0:NSL], in0=imp[:, 0:NSL], in1=self.ikeep[:, n, :], op=ALU.mult), R=[imp], W=[imp])
            P.op("dve", lambda e: e.tensor_tensor(out=imp[:, 0:NSL], in0=imp[:, 0:NSL], in1=self.iadd[:, n, :], op=ALU.add), R=[imp], W=[imp])
            P.op("dve", lambda e: e.max(out=selb[:, 0:8], in_=imp[:, 0:NSL]), R=[imp], W=[selb])
            P.op("dve", lambda e: e.tensor_reduce(out=selb[:, 8:9], in_=selb[:, 0:8], axis=AX.X, op=ALU.min), R=[selb], W=[selb])
            P.op("dve", lambda e: e.tensor_scalar(out=imp[:, 32:32 + NSL], in0=imp[:, 0:NSL], scalar1=selb[:, 8:9], scalar2=None, op0=ALU.is_ge), R=[imp, selb], W=[imp])
            gv = gs[:, 0:12].rearrange("p (h c) -> p h c", c=3)
            P.op("dve", lambda e: e.tensor_tensor(out=sm[:, 8:12], in0=sm[:, 4:8], in1=gv[:, :, 0], op=ALU.mult), R=[sm, gs], W=[sm])
            ydv = yd[:].rearrange("p (h d) -> p h d", h=4)
            yd2v = yd2[:].rearrange("p (h d) -> p h d", h=4)
            P.op("dve", lambda e: e.tensor_tensor(out=ydv, in0=pOv[:, :, 0:64], in1=sm[:, 8:12].unsqueeze(2).to_broadcast([128, 4, 64]), op=ALU.mult), R=[pO, sm], W=[yd])
            nb_ = 2 * (n + 1)
            P.op("dve", lambda e: e.tensor_copy(out=selb_[:, 0:nb_ * 64].rearrange("p (j s) -> p j s", s=64),
                                                 in_=imp[:, 32:32 + nb_].unsqueeze(2).to_broadcast([128, nb_, 64])), R=[imp], W=[selx])
            for br in range(2):
                kts = list(range(n + 1)) if br == 0 else [kt for kt in range(n - 4, n + 1) if kt >= 0]
                koff = 0 if br == 0 else T
                pOT = self.ps[6 + br]
                for kt in kts:
                    ks_ = slice(kt * 128, (kt + 1) * 128)
                    pS = self.ps[3 + (self.cnt % 2)]; pti = self.cnt % 3; self.cnt += 1
                    pt, ptv = self.pt[pti], ptb[pti][:, 0:512]
                    self.pe_seq([(0, lambda e, pS=pS, kt=kt, koff=koff: e.matmul(pS[:], lhsT=kb[0:64, koff + kt * 128:koff + (kt + 1) * 128], rhs=qt_n, start=True, stop=True))],
                                R=[self.big[2], qbuf], W=[pS])
                    if br == 0:
                        pM = self.ps[5]
                        P.op("pe", lambda e, kt=kt: e.matmul(pM[:, 0:128], lhsT=selb_[:, kt * 128:(kt + 1) * 128], rhs=self.ident_bf[:], start=True, stop=True), R=[selx], W=[pM])
                    P.op("act", lambda e, pS=pS, ptv=ptv: e.activation(out=ptv, in_=pS[:], func=AF.Exp, scale=0.125), R=[pS], W=[pt])
                    if br == 0:
                        P.op("dve", lambda e, ptv=ptv: e.tensor_tensor(out=ptv.rearrange("p (h t) -> p h t", h=4), in0=ptv.rearrange("p (h t) -> p h t", h=4),
                                                                     in1=pM[:, 0:128].unsqueeze(1).to_broadcast([128, 4, 128]), op=ALU.mult), R=[pt, pM], W=[pt])
                    if kt == n:
                        P.op("dve", lambda e, ptv=ptv: e.tensor_tensor(out=ptv, in0=ptv, in1=self.tri_ge[:], op=ALU.mult), R=[pt], W=[pt])
                    elif br == 1 and kt == n - 4:
                        P.op("dve", lambda e, ptv=ptv: e.tensor_tensor(out=ptv, in0=ptv, in1=self.tri_lt[:], op=ALU.mult), R=[pt], W=[pt])
                    P.op("pe", lambda e, ptv=ptv, kt=kt, br=br, pOT=pOT, kts=kts: e.matmul(pOT[0:65, :], lhsT=self.vaug_b[:, kt, br, :], rhs=ptv, start=(kt == kts[0]), stop=(kt == kts[-1])),
                         R=[pt, (self.vaug_b, kt)], W=[pOT])
                P.op("act", lambda e, pOT=pOT: e.activation(out=oT[:], in_=pOT[0:65, :], func=AF.Copy), R=[pOT], W=[oT])
                pOs = self.ps[5]
                def f(e):
                    ins = None
                    for h in range(4):
                        ins = e.transpose(out=pOs[:, h * 65:(h + 1) * 65], in_=oT[0:65, h * 128:(h + 1) * 128], identity=self.ident[0:65, 0:65])
                    return ins
                self.pe_seq([(0, f)], R=[oT], W=[pOs])
                pOsv = pOs[:, 0:260].rearrange("p (h c) -> p h c", h=4)
                o_ = 12 + 4 * br
                P.op("dve", lambda e, o_=o_: e.reciprocal(out=sm[:, o_:o_ + 4], in_=pOsv[:, :, 64]), R=[pOs], W=[sm])
                P.op("dve", lambda e, o_=o_, br=br: e.tensor_tensor(out=sm[:, o_:o_ + 4], in0=sm[:, o_:o_ + 4], in1=gv[:, :, 1 + br], op=ALU.mult), R=[sm, gs], W=[sm])
                P.op("dve", lambda e, o_=o_: e.tensor_tensor(out=yd2v, in0=pOsv[:, :, 0:64], in1=sm[:, o_:o_ + 4].unsqueeze(2).to_broadcast([128, 4, 64]), op=ALU.mult), R=[pOs, sm], W=[yd2])
                P.op("pool", lambda e: e.tensor_tensor(out=yd[:], in0=yd[:], in1=yd2[:], op=ALU.add), R=[yd, yd2], W=[yd])
            P.op("pool", lambda e: e.tensor_tensor(out=yd[:], in0=yd[:], in1=zt[:, 0:256], op=ALU.mult), R=[yd, zt], W=[yd])
            self.emit_y(yd, n, 6)

    def layer(self, b, l):
        P, T, NT = self.P, self.T, self.NT
        P.epoch(b * self.NL + l)
        for g in GL:
            if g in self.mixers:
                w = self.load_w(l, g, 'fm')
                getattr(self, "mixer_" + g)(b, l, w)
            else:
                gi = GL.index(g)
                for n in range(NT):
                    P.dma("sp", self.ybuf[n * 128:(n + 1) * 128, gi * 256:(gi + 1) * 256], self.zero256[:], W=[(self.ybuf, n * 4 + gi)])
        self.outproj(b, l)

    def outproj(self, b, l):
        P, T, NT = self.P, self.T, self.NT
        wt = self.wt[0]
        P.dma("sp", wt[:, :, 0:D], self.wout_bf[l].rearrange("(mc p) c -> p mc c", p=128), R=[(self.wout_bf, l)], W=[wt])
        P.dma("sp", self.lnbc[:, 0:2 * D].rearrange("p (a d) -> p a d", a=2), self.lng_d[l].partition_broadcast(128), R=[self.lng_d], W=[self.lnbc])
        last = (l == self.NL - 1)
        for n in range(NT):
            i = n % 2
            yt, xt = self.yt[i], self.xt[i]
            P.dma("sp", yt[:, 0:D], self.ybuf[n * 128:(n + 1) * 128, :], R=[(self.ybuf, range(n * 4, n * 4 + 4))], W=[yt])
            P.dma("sp", xt[:, 0:D], self.hbuf[n * 128:(n + 1) * 128, :], R=[(self.hbuf, n)], W=[xt])
            yTt = self.yTt[i]
            for half in range(2):
                ps = self.ps[half]
                def f(e, half=half, ps=ps):
                    ins = None
                    for j in range(4):
                        mc = half * 4 + j
                        ins = e.transpose(out=ps[:, j * 128:(j + 1) * 128], in_=yt[:, mc * 128:(mc + 1) * 128], identity=self.ident[:])
                    return ins
                P.op("pe", f, R=[yt], W=[ps])
                P.op("act", lambda e, half=half, ps=ps: e.activation(out=yTt[:, half * 4:(half + 1) * 4, :], in_=ps[:].rearrange("p (j t) -> p j t", j=4), func=AF.Copy),
                     R=[ps], W=[yTt])
            for half in range(2):
                ps = self.ps[2 + half]
                def f(e, half=half, ps=ps):
                    ins = None
                    for mc in range(8):
                        ins = e.matmul(ps[:], lhsT=yTt[:, mc, :], rhs=wt[:, mc, half * 512:(half + 1) * 512], start=(mc == 0), stop=(mc == 7))
                    return ins
                P.op("pe", f, R=[yTt, wt], W=[ps])
                P.op("dve", lambda e, half=half, ps=ps: e.scalar_tensor_tensor(out=xt[:, half * 512:(half + 1) * 512], in0=xt[:, half * 512:(half + 1) * 512],
                                                                              scalar=DN_ALPHA, in1=ps[:], op0=ALU.mult, op1=ALU.add), R=[xt, ps], W=[xt])
            self.layernorm(xt, i)
            if last:
                tok = P.dma("sp", self.out[b, n * 128:(n + 1) * 128, :], xt[:, 0:D], R=[xt], W=[self.out])
                self.out_toks.append(tok)
            else:
                P.dma("sp", self.hbuf[n * 128:(n + 1) * 128, :], xt[:, 0:D], R=[xt], W=[(self.hbuf, n)])
                self.to_hT(xt, n)


_CACHE = {}


def kernel(x, ln0_g, ln0_b, w_in, b_in, a_sinks, b_conv_w, b_conv_b, c_lb, c_norm_g, d_cmp_pe, d_cmp_w1, d_cmp_w2, w_out, ln_g, ln_b):
    from concourse.bass_utils import run_bass_kernel_spmd
    f32 = np.float32
    full = dict(x=np.asarray(x, f32), ln0_g=np.asarray(ln0_g, f32), ln0_b=np.asarray(ln0_b, f32), w_in=np.asarray(w_in, f32), b_in=np.asarray(b_in, f32),
                a_sinks=np.asarray(a_sinks, f32), b_conv_w=np.asarray(b_conv_w, f32), b_conv_b=np.asarray(b_conv_b, f32), c_lb=np.asarray(c_lb, f32),
                c_norm_g=np.asarray(c_norm_g, f32), d_cmp_pe=np.asarray(d_cmp_pe, f32), d_cmp_w1=np.asarray(d_cmp_w1, f32), d_cmp_w2=np.asarray(d_cmp_w2, f32),
                w_out=np.asarray(w_out, f32), ln_g=np.asarray(ln_g, f32), ln_b=np.asarray(ln_b, f32))
    B, T, _ = full['x'].shape
    NCORE = 8
    NB = B // NCORE
    NL = full['w_in'].shape[0]
    hc = host_consts(T)
    w_r, b_fm, b_tm, b_if = host_weights(full['w_in'], full['b_in'])
    common = dict(ln0=np.stack([full['ln0_g'], full['ln0_b']]), lng=np.stack([full['ln_g'], full['ln_b']], 1),
                  w_in=w_r, b_fm=b_fm, b_tm=b_tm, b_if=b_if, a_sinks=full['a_sinks'], w_out=full['w_out'], **hc)
    common.update(extra_inputs(full, T))
    k = K(T=T, NB=NB, NL=NL)
    k.build()
    common = {n: np.ascontiguousarray(common[n]) for n in k.in_names if n != 'x'}
    in_maps = []
    for c in range(NCORE):
        m = dict(common)
        m['x'] = np.ascontiguousarray(full['x'][c * NB:(c + 1) * NB])
        in_maps.append(m)
    res = run_bass_kernel_spmd(k.nc, in_maps, core_ids=list(range(NCORE)))
    out = np.concatenate([np.asarray(r['out']) for r in res.results], axis=0)
    return out.astype(np.float32)
```
